# Optimizing a Trainium2 kernel written in Bass

```python
import math
import jax, jax.numpy as jnp
from jax import lax
import numpy as np

D_MODEL = 1024
BATCH = 16
SEQ = 2048
DEPTH = 1
DEC_BATCH = 128
DEC_SEQ = 8
PAST_LEN = 8192
PAGE_SIZE = 128

SSM_CH = 16
SSM_WIDTH = D_MODEL
SSM_GROUPS = SSM_WIDTH // SSM_CH
SSM_STATE = 64
HEAD_DIM = 64
HEADS_PER_GROUP = D_MODEL // 128
WINDOWS = (128, 512, 2048)
DILATIONS = (1, 4, 16)
N_ATTN_GROUPS = len(WINDOWS)
ATTN_WIDTH = N_ATTN_GROUPS * HEADS_PER_GROUP * HEAD_DIM
ATTN_OUT = HEADS_PER_GROUP * HEAD_DIM
PROJ_WIDTH = SSM_WIDTH + 3 * ATTN_WIDTH + 2 * D_MODEL
PEER_HEADS = 8
PEER_TOPK = 16
N_KEYS = 128
N_EXPERTS = N_KEYS * N_KEYS
PEER_HALF = 128
TOKEN_BLOCK = 128
EPS = 1e-6

kernel_name = "hybrid_s5_dilated_peer_decode_step"


def rmsnorm(x, g):
    xf = x.astype(jnp.float32)
    xf = xf * lax.rsqrt(jnp.mean(xf * xf, axis=-1, keepdims=True) + EPS)
    return (xf * g.astype(jnp.float32)).astype(x.dtype)


def split_projection(xn, w_in):
    B, S = xn.shape[:2]
    p = jnp.einsum('bsd,df->bsf', xn, w_in)
    o1 = SSM_WIDTH
    o2 = o1 + ATTN_WIDTH
    o3 = o2 + ATTN_WIDTH
    o4 = o3 + ATTN_WIDTH
    o5 = o4 + D_MODEL
    u, q, k, v, ga, gb = jnp.split(p, [o1, o2, o3, o4, o5], axis=-1)
    heads = lambda t: t.reshape(B, S, N_ATTN_GROUPS, HEADS_PER_GROUP, HEAD_DIM)
    return u.reshape(B, S, SSM_GROUPS, SSM_CH), heads(q), heads(k), heads(v), ga, gb


def _scan_combine(e1, e2):
    a1r, a1i, b1r, b1i = e1
    a2r, a2i, b2r, b2i = e2
    return (a1r * a2r - a1i * a2i,
            a1r * a2i + a1i * a2r,
            a2r * b1r - a2i * b1i + b2r,
            a2r * b1i + a2i * b1r + b2i)


def ssm_branch(u, h0_re, h0_im, lam_re, lam_im, log_dt, b_re, b_im, c_re, c_im, d_skip):
    f32 = jnp.float32
    B, S = u.shape[:2]
    uf = u.astype(f32)
    lr, li = lam_re.astype(f32), lam_im.astype(f32)
    dt = jnp.exp(log_dt.astype(f32))[:, None]
    mag = jnp.exp(lr * dt)
    ang = li * dt
    ab_re, ab_im = mag * jnp.cos(ang), mag * jnp.sin(ang)
    den = lr * lr + li * li
    f_re = ((ab_re - 1.0) * lr + ab_im * li) / den
    f_im = (ab_im * lr - (ab_re - 1.0) * li) / den
    br, bi = b_re.astype(f32), b_im.astype(f32)
    bb_re = f_re[..., None] * br - f_im[..., None] * bi
    bb_im = f_re[..., None] * bi + f_im[..., None] * br
    x_re = jnp.einsum('bsgc,gnc->bsgn', uf, bb_re)
    x_im = jnp.einsum('bsgc,gnc->bsgn', uf, bb_im)
    if h0_re is not None:
        h0r, h0i = h0_re.astype(f32), h0_im.astype(f32)
        x_re = x_re.at[:, 0].add(ab_re * h0r - ab_im * h0i)
        x_im = x_im.at[:, 0].add(ab_re * h0i + ab_im * h0r)
    a_re = jnp.broadcast_to(ab_re, (1, S) + ab_re.shape)
    a_im = jnp.broadcast_to(ab_im, (1, S) + ab_im.shape)
    _, _, h_re, h_im = lax.associative_scan(_scan_combine, (a_re, a_im, x_re, x_im), axis=1)
    y = (jnp.einsum('bsgn,gcn->bsgc', h_re, c_re.astype(f32))
         - jnp.einsum('bsgn,gcn->bsgc', h_im, c_im.astype(f32))
         + d_skip.astype(f32) * uf)
    return y.astype(u.dtype), h_re[:, -1].astype(u.dtype), h_im[:, -1].astype(u.dtype)


def dilated_attn_prompt(q, k, v, dil, n_steps):
    B, S, H, E = q.shape
    L = S // dil
    blk = n_steps
    nb = -(-L // blk)
    Lp = nb * blk

    def sub(t):
        t = t.reshape(B, L, dil, H, E).transpose(0, 2, 1, 3, 4)
        t = jnp.pad(t, ((0, 0), (0, 0), (0, Lp - L), (0, 0), (0, 0)))
        return t.reshape(B, dil, nb, blk, H, E)

    qb, kb, vb = sub(q), sub(k), sub(v)

    def with_prev(t):
        prev = jnp.pad(t[:, :, :-1], ((0, 0), (0, 0), (1, 0), (0, 0), (0, 0), (0, 0)))
        return jnp.concatenate([prev, t], axis=3)

    kk, vv = with_prev(kb), with_prev(vb)
    s = jnp.einsum('brnqhe,brnkhe->brnhqk', qb, kk,
                   preferred_element_type=jnp.float32) * (E ** -0.5)
    qi = jnp.arange(blk)[:, None]
    ki = jnp.arange(2 * blk)[None, :]
    dist = qi + blk - ki
    rel = (dist >= 0) & (dist <= n_steps)
    exists = (jnp.arange(nb)[:, None, None] > 0) | (ki[None] >= blk)
    mask = rel[None] & exists
    s = jnp.where(mask[None, None, :, None], s, -jnp.inf)
    m = jnp.max(s, axis=-1, keepdims=True)
    p = jnp.exp(s - m)
    den = jnp.sum(p, axis=-1, keepdims=True)
    o = jnp.einsum('brnhqk,brnkhe->brnqhe', (p / den).astype(vv.dtype), vv)
    lse = (m + jnp.log(den))[..., 0]
    o = o.reshape(B, dil, Lp, H, E)[:, :, :L].transpose(0, 2, 1, 3, 4).reshape(B, S, H, E)
    lse = lse.transpose(0, 1, 2, 4, 3).reshape(B, dil, Lp, H)[:, :, :L]
    lse = lse.transpose(0, 2, 1, 3).reshape(B, S, H)
    return o, lse


def dilated_attn_sample(q, kv_all, dil, n_steps):
    B, T, H, E = q.shape
    WB = kv_all.shape[1] - T
    idx = WB + jnp.arange(T)[:, None] - dil * jnp.arange(n_steps + 1)[None, :]
    valid = idx >= 0
    g = jnp.take(kv_all, jnp.maximum(idx, 0), axis=1)
    s = jnp.einsum('bqhe,bqkhe->bhqk', q, g[:, :, :, 0],
                   preferred_element_type=jnp.float32) * (E ** -0.5)
    s = jnp.where(valid[None, None], s, -jnp.inf)
    m = jnp.max(s, axis=-1, keepdims=True)
    p = jnp.exp(s - m)
    den = jnp.sum(p, axis=-1, keepdims=True)
    o = jnp.einsum('bhqk,bqkhe->bqhe', (p / den).astype(kv_all.dtype), g[:, :, :, 1])
    lse = (m + jnp.log(den))[..., 0].transpose(0, 2, 1)
    return o, lse


def peer_ffn(x, w_qp, sub_keys, u_tab, v_tab):
    lead = x.shape[:-1]
    xt = x.reshape(-1, D_MODEL)
    n = xt.shape[0]
    n_blk = -(-n // TOKEN_BLOCK)
    xt = jnp.pad(xt, ((0, n_blk * TOKEN_BLOCK - n), (0, 0)))

    def block(xb):
        q = jnp.einsum('td,dq->tq', xb, w_qp).reshape(TOKEN_BLOCK, PEER_HEADS, 2, PEER_HALF)
        s = jnp.einsum('thpe,hpke->thpk', q, sub_keys, preferred_element_type=jnp.float32)
        sv, si = lax.top_k(s, PEER_TOPK)
        cand = (sv[:, :, 0, :, None] + sv[:, :, 1, None, :]).reshape(TOKEN_BLOCK, PEER_HEADS, PEER_TOPK * PEER_TOPK)
        cidx = (si[:, :, 0, :, None] * N_KEYS + si[:, :, 1, None, :]).reshape(TOKEN_BLOCK, PEER_HEADS, PEER_TOPK * PEER_TOPK)
        best, pos = lax.top_k(cand, PEER_TOPK)
        eidx = jnp.take_along_axis(cidx, pos, axis=-1).reshape(TOKEN_BLOCK, PEER_HEADS * PEER_TOPK)
        gate = jax.nn.softmax(best, axis=-1).reshape(TOKEN_BLOCK, PEER_HEADS * PEER_TOPK)
        ue = jnp.take(u_tab, eidx, axis=0)
        act = jax.nn.gelu(jnp.einsum('td,tkd->tk', xb, ue, preferred_element_type=jnp.float32))
        ve = jnp.take(v_tab, eidx, axis=0)
        return jnp.einsum('tk,tkd->td', (gate * act).astype(xb.dtype), ve)

    out = lax.map(block, xt.reshape(n_blk, TOKEN_BLOCK, D_MODEL))
    return out.reshape(-1, D_MODEL)[:n].reshape(*lead, D_MODEL)


def decoder_layer(x, h0_re, h0_im, kv_caches, g_mix, w_in, lam_re, lam_im, log_dt, b_re, b_im,
                  c_re, c_im, d_skip, w_glu_a, w_glu_b, w_attn_proj, w_out, g_ffn, w_qp,
                  sub_keys, u_tab, v_tab):
    B, S, _ = x.shape
    xn = rmsnorm(x, g_mix)
    u, q, k, v, ga, gb = split_projection(xn, w_in)
    y_ssm, hT_re, hT_im = ssm_branch(u, h0_re, h0_im, lam_re, lam_im, log_dt, b_re, b_im, c_re, c_im, d_skip)
    hs = jax.nn.gelu(y_ssm.reshape(B, S, SSM_WIDTH))
    branch_a = jnp.einsum('bsc,cd->bsd', hs, w_glu_a) * jax.nn.sigmoid(jnp.einsum('bsc,cd->bsd', hs, w_glu_b))
    outs, lses, kv_new = [], [], []
    for gi in range(N_ATTN_GROUPS):
        win, dil = WINDOWS[gi], DILATIONS[gi]
        qg, kg, vg = q[:, :, gi], k[:, :, gi], v[:, :, gi]
        kv = jnp.stack([kg, vg], axis=2)
        if kv_caches is None:
            o, l = dilated_attn_prompt(qg, kg, vg, dil, win // dil)
            kv_new.append(kv[:, S - min(win, S):])
        else:
            o, l = dilated_attn_sample(qg, jnp.concatenate([kv_caches[gi], kv], axis=1), dil, win // dil)
            kv_new.append(kv)
        outs.append(o)
        lses.append(l)
    wts = jax.nn.softmax(jnp.stack(lses, axis=0), axis=0)[..., None]
    attn = jnp.sum(wts * jnp.stack(outs, axis=0).astype(jnp.float32), axis=0)
    attn = attn.astype(x.dtype).reshape(B, S, ATTN_OUT)
    branch_b = jnp.einsum('bsc,cd->bsd', attn, w_attn_proj)
    mix = jax.nn.sigmoid(ga) * branch_a + jax.nn.sigmoid(gb) * branch_b
    x = x + jnp.einsum('bsd,de->bse', mix, w_out)
    x = x + peer_ffn(rmsnorm(x, g_ffn), w_qp, sub_keys, u_tab, v_tab)
    return x, hT_re, hT_im, kv_new


def setup_inputs(seed: int = 0) -> dict:
    key = jax.random.key(seed)
    ks = jax.random.split(key, 32)
    f32 = jnp.float32
    nrm = lambda kk, shape, scale: scale * jax.random.normal(kk, shape, f32)
    L = DEPTH
    wb = [min(w, PAST_LEN) for w in WINDOWS]
    lam_im = jnp.pi * jnp.arange(SSM_STATE, dtype=f32)[None, None, :] + nrm(ks[10], (L, SSM_GROUPS, SSM_STATE), 0.01)
    return {
        "x_prompt": nrm(ks[0], (BATCH, SEQ, D_MODEL), 1.0),
        "x_sample": nrm(ks[1], (DEC_BATCH, DEC_SEQ, D_MODEL), 1.0),
        "state_ssm_re": nrm(ks[2], (L, DEC_BATCH, SSM_GROUPS, SSM_STATE), 0.5),
        "state_ssm_im": nrm(ks[3], (L, DEC_BATCH, SSM_GROUPS, SSM_STATE), 0.5),
        "cache_kv_w128": nrm(ks[4], (L, DEC_BATCH, wb[0], 2, HEADS_PER_GROUP, HEAD_DIM), 1.0),
        "cache_kv_w512": nrm(ks[5], (L, DEC_BATCH, wb[1], 2, HEADS_PER_GROUP, HEAD_DIM), 1.0),
        "cache_kv_w2048": nrm(ks[6], (L, DEC_BATCH, wb[2], 2, HEADS_PER_GROUP, HEAD_DIM), 1.0),
        "g_mix": 1.0 + nrm(ks[7], (L, D_MODEL), 0.01),
        "w_in": nrm(ks[8], (L, D_MODEL, PROJ_WIDTH), D_MODEL ** -0.5),
        "lam_re": -0.5 + nrm(ks[9], (L, SSM_GROUPS, SSM_STATE), 0.01),
        "lam_im": lam_im,
        "log_dt": jax.random.uniform(ks[11], (L, SSM_GROUPS), f32, math.log(1e-3), math.log(1e-1)),
        "b_re": nrm(ks[12], (L, SSM_GROUPS, SSM_STATE, SSM_CH), (2 * SSM_CH) ** -0.5),
        "b_im": nrm(ks[13], (L, SSM_GROUPS, SSM_STATE, SSM_CH), (2 * SSM_CH) ** -0.5),
        "c_re": nrm(ks[14], (L, SSM_GROUPS, SSM_CH, SSM_STATE), (2 * SSM_STATE) ** -0.5),
        "c_im": nrm(ks[15], (L, SSM_GROUPS, SSM_CH, SSM_STATE), (2 * SSM_STATE) ** -0.5),
        "d_skip": nrm(ks[16], (L, SSM_GROUPS, SSM_CH), 1.0),
        "w_glu_a": nrm(ks[17], (L, SSM_WIDTH, D_MODEL), SSM_WIDTH ** -0.5),
        "w_glu_b": nrm(ks[18], (L, SSM_WIDTH, D_MODEL), SSM_WIDTH ** -0.5),
        "w_attn_proj": nrm(ks[19], (L, ATTN_OUT, D_MODEL), ATTN_OUT ** -0.5),
        "w_out": nrm(ks[20], (L, D_MODEL, D_MODEL), D_MODEL ** -0.5),
        "g_ffn": 1.0 + nrm(ks[21], (L, D_MODEL), 0.01),
        "w_qp": nrm(ks[22], (L, D_MODEL, PEER_HEADS * 2 * PEER_HALF), D_MODEL ** -0.5),
        "sub_keys": nrm(ks[23], (L, PEER_HEADS, 2, N_KEYS, PEER_HALF), PEER_HALF ** -0.5),
        "u_tab": nrm(ks[24], (L, N_EXPERTS, D_MODEL), D_MODEL ** -0.5),
        "v_tab": nrm(ks[25], (L, N_EXPERTS, D_MODEL), 0.3),
        "g_final": 1.0 + nrm(ks[26], (D_MODEL,), 0.01),
    }


def reference(x_prompt, x_sample, state_ssm_re, state_ssm_im, cache_kv_w128, cache_kv_w512,
              cache_kv_w2048, g_mix, w_in, lam_re, lam_im, log_dt, b_re, b_im, c_re, c_im, d_skip,
              w_glu_a, w_glu_b, w_attn_proj, w_out, g_ffn, w_qp, sub_keys, u_tab, v_tab, g_final):
    xp, xs = x_prompt, x_sample
    sre_p, sim_p, sre_s, sim_s = [], [], [], []
    kvp = [[] for _ in range(N_ATTN_GROUPS)]
    kvs = [[] for _ in range(N_ATTN_GROUPS)]
    for li in range(DEPTH):
        lp = (g_mix[li], w_in[li], lam_re[li], lam_im[li], log_dt[li], b_re[li], b_im[li],
              c_re[li], c_im[li], d_skip[li], w_glu_a[li], w_glu_b[li], w_attn_proj[li], w_out[li],
              g_ffn[li], w_qp[li], sub_keys[li], u_tab[li], v_tab[li])
        xp, hr, hi, kv_new_p = decoder_layer(xp, None, None, None, *lp)
        sre_p.append(hr)
        sim_p.append(hi)
        caches = (cache_kv_w128[li], cache_kv_w512[li], cache_kv_w2048[li])
        xs, hr, hi, kv_new_s = decoder_layer(xs, state_ssm_re[li], state_ssm_im[li], caches, *lp)
        sre_s.append(hr)
        sim_s.append(hi)
        for gi in range(N_ATTN_GROUPS):
            kvp[gi].append(kv_new_p[gi])
            kvs[gi].append(kv_new_s[gi])
    y_prompt = rmsnorm(xp, g_final)
    y_sample = rmsnorm(xs, g_final)
    return (y_prompt, y_sample,
            jnp.stack(sre_p), jnp.stack(sim_p),
            jnp.stack(kvp[0]), jnp.stack(kvp[1]), jnp.stack(kvp[2]),
            jnp.stack(sre_s), jnp.stack(sim_s),
            jnp.stack(kvs[0]), jnp.stack(kvs[1]), jnp.stack(kvs[2]))
```

```python
from contextlib import ExitStack
import math
import numpy as np
import concourse.bass as bass
import concourse.mybir as mybir
from concourse.bass_utils import run_bass_kernel_spmd

F32 = mybir.dt.float32
BF16 = mybir.dt.bfloat16
I32 = mybir.dt.int32
U32 = mybir.dt.uint32
ALU = mybir.AluOpType
AF = mybir.ActivationFunctionType
AX = mybir.AxisListType

NCORES = 8
D = 1024
SEQ = 2048
NPS = 2
NSS = 16
DEC = 8
TOKP = NPS * SEQ
TOK = TOKP + 128
NTILE = TOK // 128
PROJ = 7680
OFF_U, OFF_Q, OFF_K, OFF_V, OFF_GA, OFF_GB = 0, 1024, 2560, 4096, 5632, 6656
WINS = (128, 512, 2048)
DILS = (1, 4, 16)
EPS = 1e-6
NEXP = 16384
DBG = {}


class Op:
    __slots__ = ("eng", "fn", "reads", "writes", "dma", "deps", "needs_inc", "sem", "val")

    def __init__(self, eng, fn, reads, writes, dma):
        self.eng = eng
        self.fn = fn
        self.reads = reads
        self.writes = writes
        self.dma = dma
        self.deps = ()
        self.needs_inc = False
        self.sem = None
        self.val = 0


class Sync:
    def __init__(self, nc, stack, dma_pool=None):
        self.nc = nc
        self.engs = {"pe": nc.tensor, "act": nc.scalar, "dve": nc.vector,
                     "pool": nc.gpsimd, "sp": nc.sync}
        dma_pool = dma_pool or {"sp": 24, "pool": 8, "act": 4}
        self.csem = {}
        self.ccount = {}
        for e in ("pe", "act", "dve", "pool"):
            self.csem[e] = stack.enter_context(nc.semaphore("cs_" + e))
            self.ccount[e] = 0
        self.pools = {}
        for q, n in dma_pool.items():
            self.pools[q] = {
                "sems": [stack.enter_context(nc.semaphore(f"ds_{q}_{i}")) for i in range(n)],
                "vals": [0] * n, "next": 0}
        self.waited = {e: {} for e in self.engs}
        self.n_inst = 0

    def wait(self, eng_name, sem, val):
        w = self.waited[eng_name]
        key = id(sem)
        if w.get(key, 0) >= val:
            return
        self.engs[eng_name].wait_ge(sem, val)
        w[key] = val

    def barrier(self, engines=("pe", "act", "dve", "pool", "sp")):
        for E in engines:
            for q, p in self.pools.items():
                for sem, v in zip(p["sems"], p["vals"]):
                    if v > 0:
                        self.wait(E, sem, v)
            for e in ("pe", "act", "dve", "pool"):
                if self.ccount[e] > 0 and e != E:
                    self.wait(E, self.csem[e], self.ccount[e])


class Prog:
    def __init__(self, sync):
        self.S = sync
        self.ops = []

    def op(self, eng, fn, reads=(), writes=(), dma=False):
        self.ops.append(Op(eng, fn, tuple(reads), tuple(writes), dma))

    def dma(self, eng, out, in_, reads=(), writes=(), **kw):
        self.op(eng, lambda e: e.dma_start(out=out, in_=in_, **kw), reads, writes, dma=True)

    def emit(self, barrier=True):
        S = self.S
        ops = self.ops
        last_w = {}
        readers = {}
        last_on_eng = {}
        for i, o in enumerate(ops):
            deps = set()
            for k in o.reads:
                if k in last_w:
                    deps.add(last_w[k])
            for k in o.writes:
                if k in last_w:
                    deps.add(last_w[k])
                deps.update(readers.get(k, ()))
            deps.discard(i)
            for k in o.reads:
                readers.setdefault(k, []).append(i)
            for k in o.writes:
                last_w[k] = i
                readers[k] = []
            o.deps = sorted(deps)
            for j in o.deps:
                ops[j].needs_inc = True
            if not o.dma:
                last_on_eng[o.eng] = i
        for i in last_on_eng.values():
            ops[i].needs_inc = True
        for o in ops:
            E = o.eng
            eng = S.engs[E]
            for j in o.deps:
                d = ops[j]
                if (not d.dma) and d.eng == "pe" and E == "pe" and not o.dma:
                    continue
                S.wait(E, d.sem, d.val)
            if o.dma:
                p = S.pools[E]
                k = p["next"]
                p["next"] = (k + 1) % len(p["sems"])
                sem = p["sems"][k]
                if p["vals"][k] > 0:
                    S.wait(E, sem, p["vals"][k])
                p["vals"][k] += 16
                ins = o.fn(eng)
                ins.then_inc(sem, 16)
                o.sem = sem
                o.val = p["vals"][k]
            else:
                ins = o.fn(eng)
                if o.needs_inc:
                    S.ccount[E] += 1
                    ins.then_inc(S.csem[E], 1)
                    o.sem = S.csem[E]
                    o.val = S.ccount[E]
            S.n_inst += 1
        if barrier:
            S.barrier()


class Ctx:
    pass


def ss(start, n, step):
    return slice(start, start + (n - 1) * step + 1, step)


def declare_io(nc):
    T = Ctx()
    di = lambda n, s, dt=F32: nc.dram_tensor(n, list(s), dt, kind="ExternalInput")
    do = lambda n, s, dt=F32: nc.dram_tensor(n, list(s), dt, kind="ExternalOutput")
    ds = lambda n, s, dt: nc.dram_tensor(n, list(s), dt, kind=("ExternalOutput" if n in DBG.get("expose", ()) else ("ExternalInput" if n in DBG.get("inject", ()) else "Internal")))
    T.x = di("x", [TOK, D])
    T.st_re = di("st_re", [NSS, 4096])
    T.st_im = di("st_im", [NSS, 4096])
    nss = 1 if DBG.get("small") else NSS
    nexp = 512 if DBG.get("small_tab") else NEXP
    T.c128 = di("c128", [nss, 128, 2, 512])
    T.c512 = di("c512", [nss, 512, 2, 512])
    T.c2048 = di("c2048", [nss, 2048, 2, 512])
    T.g_mix = di("g_mix", [D])
    T.w_in = di("w_in", [D, PROJ])
    T.lam_re = di("lam_re", [64, 64])
    T.lam_im = di("lam_im", [64, 64])
    T.log_dt = di("log_dt", [64])
    T.b_re = di("b_re", [4096, 16])
    T.b_im = di("b_im", [4096, 16])
    T.c_re = di("c_re", [1024, 64])
    T.c_im = di("c_im", [1024, 64])
    T.d_skip = di("d_skip", [1024])
    T.w_glu_a = di("w_glu_a", [D, D])
    T.w_glu_b = di("w_glu_b", [D, D])
    T.w_attn = di("w_attn", [512, D])
    T.w_out = di("w_out", [D, D])
    T.g_ffn = di("g_ffn", [D])
    T.w_qp = di("w_qp", [D, 2048])
    T.sub_keys = di("sub_keys", [16, 128, 128])
    T.u_tab = di("u_tab", [nexp, D])
    T.v_tab = di("v_tab", [nexp, D])
    T.g_final = di("g_final", [D])
    T.y = do("y", [TOK, D])
    T.ssm_p_re = do("ssm_p_re", [NPS, 4096])
    T.ssm_p_im = do("ssm_p_im", [NPS, 4096])
    T.ssm_s_re = do("ssm_s_re", [NSS, 4096])
    T.ssm_s_im = do("ssm_s_im", [NSS, 4096])
    T.kvp = [do(f"kvp{w}", [NPS, w, 1024]) for w in WINS]
    T.kvs = [do(f"kvs{w}", [128, 1024]) for w in WINS]
    T.uT_s = ds("uT_s", [1024, TOK], BF16)
    T.qT_s = ds("qT_s", [1536, TOK], BF16)
    T.kT_s = ds("kT_s", [1536, TOK], BF16)
    T.sga_s = ds("sga_s", [1024, TOK], F32)
    T.sgb_s = ds("sgb_s", [1024, TOK], F32)
    T.vtok_s = ds("vtok_s", [TOK, 1536], BF16)
    T.hsT_s = ds("hsT_s", [1024, TOK], BF16)
    T.x1_s = ds("x1_s", [TOK, D], F32)
    T.uv_s = ds("uv_s", [nexp, 2, D], BF16)
    T.U_s = [ds(f"U_s{g}", [TOK, 512], F32) for g in range(3)]
    T.md_s = [ds(f"md_s{g}", [TOK, 16], F32) for g in range(3)]
    return T


def phase_consts(nc, S, T, C, st):
    sb = lambda n, shape, dt: st.enter_context(nc.sbuf_tensor(n, shape, dt))
    P = Prog(S)
    C.ident_b = sb("ident_b", [128, 128], BF16)
    C.ident_f = sb("ident_f", [128, 128], F32)
    C.maskadd = sb("maskadd", [128, 256], BF16)
    iot = sb("c_iot", [128, 256], I32)
    t1 = sb("c_t1", [128, 256], F32)
    t2 = sb("c_t2", [128, 256], F32)
    P.op("pool", lambda e: e.iota(iot[:], [[1, 256]], base=0, channel_multiplier=-1), writes=["iot"])
    P.op("dve", lambda e: e.tensor_scalar(C.ident_b[:], iot[:, 0:128], 0.0, None, ALU.is_equal),
         reads=["iot"], writes=["ident_b"])
    P.op("dve", lambda e: e.tensor_scalar(C.ident_f[:], iot[:, 0:128], 0.0, None, ALU.is_equal),
         reads=["iot"], writes=["ident_f"])
    P.op("dve", lambda e: e.tensor_scalar(t1[:], iot[:], 0.0, None, ALU.is_ge), reads=["iot"], writes=["t1"])
    P.op("dve", lambda e: e.tensor_scalar(t2[:], iot[:], 128.0, None, ALU.is_le), reads=["iot"], writes=["t2"])
    P.op("dve", lambda e: e.tensor_tensor(t1[:], t1[:], t2[:], ALU.mult), reads=["t1", "t2"], writes=["t1"])
    P.op("dve", lambda e: e.tensor_scalar(C.maskadd[:], t1[:], -1.0, 30000.0, ALU.add, ALU.mult),
         reads=["t1"], writes=["maskadd"])
    P.emit()


def phase1(nc, S, T, C):
    with ExitStack() as st:
        sb = lambda n, shape, dt: st.enter_context(nc.sbuf_tensor(n, shape, dt))
        ps = lambda n, shape, dt: st.enter_context(nc.psum_tensor(n, shape, dt))
        P = Prog(S)
        win_b = sb("win_b", [128, 8, PROJ], BF16)
        wst = [sb(f"wst{i}", [128, 8, 256], F32) for i in range(2)]
        gmix = sb("gmix", [128, 8], F32)
        xs = [sb(f"xs{i}", [128, D], F32) for i in range(2)]
        junk = sb("junk", [128, D], BF16)
        ss = [sb(f"ss{i}", [128, 1], F32) for i in range(2)]
        rstd = [sb(f"rstd{i}", [128, 1], F32) for i in range(2)]
        xnb = [sb(f"xnb{i}", [128, D], BF16) for i in range(2)]
        xnT = [sb(f"xnT{i}", [128, 8, 512], BF16) for i in range(2)]
        kvst = [sb(f"kvst{i}", [128, 1024], F32) for i in range(3)]
        vb = [sb(f"vb{i}", [128, 512], BF16) for i in range(3)]
        fsb = [sb(f"fsb{i}", [128, 512], BF16) for i in range(4)]
        fsf = [sb(f"fsf{i}", [128, 512], F32) for i in range(3)]
        pT = [ps(f"pT{i}", [128, 8, 128], BF16) for i in range(2)]
        pM = [ps(f"pM{i}", [128, 512], F32) for i in range(5)]

        x = T.x.ap()
        P.dma("sp", gmix[:], T.g_mix.ap().rearrange("(k p) -> p k", p=128), writes=["gmix"],
              allow_slow_non_contiguous=True)
        w_in = T.w_in.ap().rearrange("(k p) f -> p k f", p=128)
        for c in range(PROJ // 256):
            b = c % 2
            P.dma("sp", wst[b][:], w_in[:, :, c * 256:(c + 1) * 256], writes=[("wst", b)])
            for k in range(8):
                eng = "dve" if k % 2 == 0 else "pool"
                P.op(eng, lambda e, b=b, k=k, c=c: e.tensor_scalar(
                    win_b[:, k, c * 256:(c + 1) * 256], wst[b][:, k, :], gmix[:, k:k + 1], None, ALU.mult),
                    reads=[("wst", b), "gmix"], writes=[("win", c)])
        wkeys = [("win", c) for c in range(PROJ // 256)]

        cnt = {"pm": 0, "kv": 0, "fsb": 0, "fsf": 0, "ev": 0}

        def next_pm():
            i = cnt["pm"] % len(pM)
            cnt["pm"] += 1
            return i

        nchunks = 9
        for ch in DBG.get("chunks", range(nchunks)):
            ntl = 4 if ch < 8 else 1
            cb = ch % 2
            ntok = ntl * 128
            for j in range(ntl):
                ti = ch * 4 + j
                b = ti % 2
                t0 = ti * 128
                P.dma("sp", xs[b][:], x[t0:t0 + 128, :], writes=[("xs", b)])
                P.op("act", lambda e, b=b: e.activation(junk[:], xs[b][:], AF.Square, accum_out=ss[b][:]),
                     reads=[("xs", b)], writes=["junk", ("ss", b)])
                P.op("dve", lambda e, b=b: e.tensor_scalar(rstd[b][:], ss[b][:], 1.0 / D, EPS, ALU.mult, ALU.add),
                     reads=[("ss", b)], writes=[("rstd", b)])
                P.op("act", lambda e, b=b: e.activation(rstd[b][:], rstd[b][:], AF.Sqrt),
                     reads=[("rstd", b)], writes=[("rstd", b)])
                P.op("dve", lambda e, b=b: e.reciprocal(rstd[b][:], rstd[b][:]),
                     reads=[("rstd", b)], writes=[("rstd", b)])
                P.op("dve", lambda e, b=b: e.tensor_scalar(xnb[b][:], xs[b][:], rstd[b][:], None, ALU.mult),
                     reads=[("xs", b), ("rstd", b)], writes=[("xnb", b)])
                for k in range(8):
                    P.op("pe", lambda e, b=b, k=k: e.transpose(pT[b][:, k, :], xnb[b][:, k * 128:(k + 1) * 128],
                                                               C.ident_b[:]),
                         reads=[("xnb", b), "ident_b"], writes=[("pT", b)])
                P.op("dve", lambda e, b=b, cb=cb, j=j: e.tensor_copy(xnT[cb][:, :, j * 128:(j + 1) * 128], pT[b][:]),
                     writes=[("pT", b), ("xnT", cb, j)])
                if ti < 32:
                    seq = ti // 16
                    tin = (ti % 16) * 128
                else:
                    seq, tin = None, 0
                for g in range(3):
                    need_k = True
                    if seq is not None and tin < SEQ - WINS[g]:
                        need_k = False
                    kb = cnt["kv"] % 3
                    cnt["kv"] += 1
                    for part, off in ((0, OFF_K + 512 * g), (1, OFF_V + 512 * g)):
                        if part == 0 and not need_k:
                            continue
                        pi = next_pm()
                        for k in range(8):
                            P.op("pe", lambda e, pi=pi, k=k, cb=cb, j=j, off=off: e.matmul(
                                pM[pi][:], xnT[cb][:, k, j * 128:(j + 1) * 128], win_b[:, k, off:off + 512],
                                start=(k == 0), stop=(k == 7)),
                                reads=[("xnT", cb, j)] + wkeys[off // 256: off // 256 + 2], writes=[("pM", pi)])
                        P.op("act", lambda e, pi=pi, kb=kb, part=part: e.activation(
                            kvst[kb][:, part * 512:(part + 1) * 512], pM[pi][:], AF.Copy),
                            writes=[("pM", pi), ("kvst", kb, part)])
                        if part == 1:
                            P.op("pool", lambda e, kb=kb: e.tensor_copy(vb[kb][:], kvst[kb][:, 512:1024]),
                                 reads=[("kvst", kb, 1)], writes=[("vb", kb)])
                    P.dma("sp", T.vtok_s.ap()[t0:t0 + 128, g * 512:(g + 1) * 512], vb[kb][:],
                          reads=[("vb", kb)], writes=[("vtok_s", ti, g)])
                    if need_k:
                        if seq is None:
                            dst = T.kvs[g].ap()[:, :]
                        else:
                            r0 = tin - (SEQ - WINS[g])
                            dst = T.kvp[g].ap()[seq, r0:r0 + 128, :]
                        P.dma("sp", dst, kvst[kb][:], reads=[("kvst", kb, 0), ("kvst", kb, 1)],
                              writes=[("kvout", ti, g)])
            tok0 = ch * 512
            xkeys = [("xnT", cb, j) for j in range(ntl)]
            jobs = []
            for f in range(8):
                jobs.append(("u", OFF_U + 128 * f, T.uT_s, f))
            for f in range(12):
                jobs.append(("q", OFF_Q + 128 * f, T.qT_s, f))
            for f in range(12):
                jobs.append(("k", OFF_K + 128 * f, T.kT_s, f))
            for f in range(8):
                jobs.append(("ga", OFF_GA + 128 * f, T.sga_s, f))
            for f in range(8):
                jobs.append(("gb", OFF_GB + 128 * f, T.sgb_s, f))
            for kind, off, dst_t, f in jobs:
                pi = next_pm()
                for k in range(8):
                    P.op("pe", lambda e, pi=pi, k=k, cb=cb, off=off, ntok=ntok: e.matmul(
                        pM[pi][:, 0:ntok], win_b[:, k, off:off + 128], xnT[cb][:, k, 0:ntok],
                        start=(k == 0), stop=(k == 7)),
                        reads=xkeys + [wkeys[off // 256]], writes=[("pM", pi)])
                dst = dst_t.ap()[f * 128:(f + 1) * 128, tok0:tok0 + ntok]
                if kind in ("ga", "gb"):
                    bi = cnt["fsf"] % len(fsf)
                    cnt["fsf"] += 1
                    P.op("act", lambda e, pi=pi, bi=bi, ntok=ntok: e.activation(
                        fsf[bi][:, 0:ntok], pM[pi][:, 0:ntok], AF.Sigmoid),
                        writes=[("pM", pi), ("fsf", bi)])
                    P.dma("sp", dst, fsf[bi][:, 0:ntok], reads=[("fsf", bi)], writes=[(kind, f, ch)])
                else:
                    bi = cnt["fsb"] % len(fsb)
                    cnt["fsb"] += 1
                    eng = "dve" if cnt["ev"] % 2 == 0 else "act"
                    cnt["ev"] += 1
                    qs = 0.125 if kind == "q" else 1.0
                    if eng == "dve":
                        P.op("dve", lambda e, pi=pi, bi=bi, ntok=ntok, qs=qs: e.tensor_scalar(
                            fsb[bi][:, 0:ntok], pM[pi][:, 0:ntok], qs, None, ALU.mult),
                            writes=[("pM", pi), ("fsb", bi)])
                    else:
                        P.op("act", lambda e, pi=pi, bi=bi, ntok=ntok, qs=qs: e.activation(
                            fsb[bi][:, 0:ntok], pM[pi][:, 0:ntok], AF.Copy, scale=qs),
                            writes=[("pM", pi), ("fsb", bi)])
                    P.dma("sp", dst, fsb[bi][:, 0:ntok], reads=[("fsb", bi)], writes=[(kind, f, ch)])
        P.emit()


def build_program(upto=99):
    nc = bass.Bass("TRN2", target_bir_lowering=False)
    T = declare_io(nc)
    C = Ctx()
    with ExitStack() as gst:
        S = Sync(nc, gst)
        phase_consts(nc, S, T, C, gst)
        only = DBG.get("only")
        run = lambda i: (upto >= i) if only is None else (i in only)
        if run(1):
            phase1(nc, S, T, C)
        if run(2) and not DBG.get("skip2"):
            phase2(nc, S, T, C)
        if run(3):
            phase3(nc, S, T, C)
        if run(4):
            phase4(nc, S, T, C)
        if run(5):
            phase5a(nc, S, T, C)
            phase5(nc, S, T, C)
        print("instructions:", S.n_inst, "counts:", S.ccount)
    return nc


def make_in_maps(inputs):
    f = lambda a: np.ascontiguousarray(np.asarray(a, dtype=np.float32))
    xp = f(inputs["x_prompt"])
    xsm = f(inputs["x_sample"])
    shared = {
        "g_mix": f(inputs["g_mix"]).reshape(D),
        "w_in": f(inputs["w_in"]).reshape(D, PROJ),
        "lam_re": f(inputs["lam_re"]).reshape(64, 64),
        "lam_im": f(inputs["lam_im"]).reshape(64, 64),
        "log_dt": f(inputs["log_dt"]).reshape(64),
        "b_re": f(inputs["b_re"]).reshape(4096, 16),
        "b_im": f(inputs["b_im"]).reshape(4096, 16),
        "c_re": f(inputs["c_re"]).reshape(1024, 64),
        "c_im": f(inputs["c_im"]).reshape(1024, 64),
        "d_skip": f(inputs["d_skip"]).reshape(1024),
        "w_glu_a": f(inputs["w_glu_a"]).reshape(D, D),
        "w_glu_b": f(inputs["w_glu_b"]).reshape(D, D),
        "w_attn": f(inputs["w_attn_proj"]).reshape(512, D),
        "w_out": f(inputs["w_out"]).reshape(D, D),
        "g_ffn": f(inputs["g_ffn"]).reshape(D),
        "w_qp": f(inputs["w_qp"]).reshape(D, 2048),
        "sub_keys": f(inputs["sub_keys"]).reshape(16, 128, 128),
        "u_tab": f(inputs["u_tab"]).reshape(NEXP, D),
        "v_tab": f(inputs["v_tab"]).reshape(NEXP, D),
        "g_final": f(inputs["g_final"]).reshape(D),
    }
    st_re = f(inputs["state_ssm_re"]).reshape(128, 4096)
    st_im = f(inputs["state_ssm_im"]).reshape(128, 4096)
    c128 = f(inputs["cache_kv_w128"]).reshape(128, 128, 2, 512)
    c512 = f(inputs["cache_kv_w512"]).reshape(128, 512, 2, 512)
    c2048 = f(inputs["cache_kv_w2048"]).reshape(128, 2048, 2, 512)
    maps = []
    for c in range(NCORES):
        m = dict(shared)
        m["x"] = np.concatenate([xp[NPS * c:NPS * (c + 1)].reshape(TOKP, D),
                                 xsm[NSS * c:NSS * (c + 1)].reshape(128, D)], axis=0)
        sl = slice(NSS * c, NSS * (c + 1))
        m["st_re"] = st_re[sl]
        m["st_im"] = st_im[sl]
        m["c128"] = c128[sl]
        m["c512"] = c512[sl]
        m["c2048"] = c2048[sl]
        maps.append(m)
    return maps


def gather_outputs(results):
    cat = lambda name: np.concatenate([np.asarray(r[name]) for r in results], axis=0)
    y = np.stack([np.asarray(r["y"]) for r in results], axis=0)
    y_prompt = y[:, :TOKP].reshape(16, SEQ, D)
    y_sample = y[:, TOKP:].reshape(128, DEC, D)
    outs = [y_prompt, y_sample,
            cat("ssm_p_re").reshape(1, 16, 64, 64), cat("ssm_p_im").reshape(1, 16, 64, 64)]
    for w in WINS:
        outs.append(cat(f"kvp{w}").reshape(1, 16, w, 2, 8, 64))
    outs.append(cat("ssm_s_re").reshape(1, 128, 64, 64))
    outs.append(cat("ssm_s_im").reshape(1, 128, 64, 64))
    for w in WINS:
        outs.append(cat(f"kvs{w}").reshape(1, 128, DEC, 2, 8, 64))
    return tuple(np.ascontiguousarray(o, dtype=np.float32) for o in outs)


def kernel(**inputs):
    nc = build_program()
    maps = make_in_maps(inputs)
    res = run_bass_kernel_spmd(nc, maps, core_ids=list(range(NCORES)))
    return gather_outputs(res.results)


TWO_PI = 2.0 * math.pi


def phase2(nc, S, T, C):
    with ExitStack() as st:
        sb = lambda n, shape, dt: st.enter_context(nc.sbuf_tensor(n, shape, dt))
        ps = lambda n, shape, dt: st.enter_context(nc.psum_tensor(n, shape, dt))
        P = Prog(S)
        V = "dve"

        def small(name):
            return sb("p2_" + name, [128, 32], F32)

        lr, li, ldt, dtt, rmag, ang = (small(n) for n in ("lr", "li", "ldt", "dt", "rmag", "ang"))
        a1, kq, red, m1 = (small(n) for n in ("a1", "kq", "red", "m1"))
        kqi = sb("p2_kqi", [128, 32], I32)
        sin_t, cos_t, abre, abim, am1 = (small(n) for n in ("sin", "cos", "abre", "abim", "am1"))
        den, fre, fim, tq = (small(n) for n in ("den", "fre", "fim", "tq"))
        Wre = sb("p2_Wre", [128, 11, 32], F32)
        Wim = sb("p2_Wim", [128, 11, 32], F32)
        bre = sb("p2_bre", [128, 32, 16], F32)
        bim = sb("p2_bim", [128, 32, 16], F32)
        bbre = sb("p2_bbre", [128, 32, 16], F32)
        bbim = sb("p2_bbim", [128, 32, 16], F32)
        btmp = sb("p2_btmp", [128, 32, 16], F32)
        bbpad = [[sb(f"p2_bbpad{c}{j}", [128, 128], BF16) for j in range(4)] for c in range(2)]
        Cn = [sb(f"p2_Cn{c}", [128, 8, 64], F32) for c in range(2)]
        CT = [sb(f"p2_CT{c}", [128, 128], BF16) for c in range(2)]
        xl = [sb(f"p2_xl{c}", [128, 128], BF16) for c in range(2)]
        yl = [[sb(f"p2_yl{c}{j}", [128, 128], BF16) for j in range(4)] for c in range(2)]
        bmi = sb("p2_bmi", [128, 8], I32)
        bm = sb("p2_bm", [128, 8], F32)
        bm2 = sb("p2_bm2", [128, 8], F32)
        nbm = sb("p2_nbm", [128, 8], F32)
        dsk = sb("p2_dsk", [128, 8], F32)
        h0st = [sb(f"p2_h0st{i}", [16, 1024], F32) for i in range(2)]
        h0T = [sb(f"p2_h0T{c}", [128, 32, 16], F32) for c in range(2)]
        Dt = [sb(f"p2_D{c}", [128, 2048], F32) for c in range(2)]
        dtmp = [sb(f"p2_dtmp{i}", [128, 1024], F32) for i in range(2)]
        uT = [sb(f"p2_uT{i}", [128, TOK], BF16) for i in range(2)]
        xt = [sb(f"p2_xt{c}", [128, 2048], F32) for c in range(2)]
        gg = [sb(f"p2_g{c}", [128, 2048], F32) for c in range(2)]
        tmp = [sb(f"p2_tmp{i}", [128, 512], F32) for i in range(2)]
        hbuf = sb("p2_hbuf", [128, 4, 2, TOK], BF16)
        hsT = sb("p2_hsT", [128, TOK], BF16)
        ysb = [sb(f"p2_ysb{i}", [128, 512], F32) for i in range(2)]
        yw = [sb(f"p2_yw{i}", [128, 512], F32) for i in range(2)]
        pat = sb("p2_pat", [128, 16, 8], F32)
        decs = sb("p2_decs", [128, 128], F32)
        hfp = [sb(f"p2_hfp{c}", [128, 2, 32], F32) for c in range(2)]
        hfs = [sb(f"p2_hfs{c}", [128, 16, 32], F32) for c in range(2)]
        hfo = sb("p2_hfo", [128, 128], F32)

        xps = [[ps(f"p2_xps{i}{c}", [128, 512], F32) for c in range(2)] for i in range(2)]
        tps = ps("p2_tps", [128, 128], BF16)
        yps = [ps(f"p2_yps{i}", [128, 512], F32) for i in range(2)]
        mps = ps("p2_mps", [128, 32, 16], F32)

        P.dma("sp", lr[:], T.lam_re.ap().rearrange("(k a) n -> (a n) k", a=2), writes=["lr"],
              allow_slow_non_contiguous=True)
        P.dma("sp", li[:], T.lam_im.ap().rearrange("(k a) n -> (a n) k", a=2), writes=["li"],
              allow_slow_non_contiguous=True)
        for a in range(2):
            P.dma("sp", ldt[a * 64:(a + 1) * 64, :], bass.AP(T.log_dt, a, [[0, 64], [2, 32]]), writes=["ldt"],
                  allow_slow_non_contiguous=True)
        P.dma("sp", bre[:], T.b_re.ap().rearrange("(k p) c -> p k c", p=128), writes=["bre"])
        P.dma("sp", bim[:], T.b_im.ap().rearrange("(k p) c -> p k c", p=128), writes=["bim"])
        P.dma("sp", Cn[0][:], T.c_re.ap().rearrange("(f r) n -> r f n", r=128), writes=["Cn0"])
        P.dma("sp", Cn[1][:], T.c_im.ap().rearrange("(f r) n -> r f n", r=128), writes=["Cn1"])
        P.dma("sp", dsk[:], T.d_skip.ap().rearrange("(f r) -> r f", r=128), writes=["dsk"],
              allow_slow_non_contiguous=True)

        def tt(out, a, b, op, reads, writes, eng=V):
            P.op(eng, lambda e: e.tensor_tensor(out, a, b, op), reads=reads, writes=writes)

        def ts(out, a, s1, s2, op0, op1, reads, writes, eng=V):
            if op1 is None:
                P.op(eng, lambda e: e.tensor_scalar(out, a, s1, None, op0), reads=reads, writes=writes)
            else:
                P.op(eng, lambda e: e.tensor_scalar(out, a, s1, s2, op0, op1), reads=reads, writes=writes)

        def stt(out, a, sc, b, op0, op1, reads, writes, eng=V):
            P.op(eng, lambda e: e.scalar_tensor_tensor(out, a, sc, b, op0, op1), reads=reads, writes=writes)

        def act(out, a, func, reads, writes, **kw):
            P.op("act", lambda e: e.activation(out, a, func, **kw), reads=reads, writes=writes)

        act(dtt[:], ldt[:], AF.Exp, ["ldt"], ["dt"])
        tt(a1[:], lr[:], dtt[:], ALU.mult, ["lr", "dt"], ["a1"])
        act(rmag[:], a1[:], AF.Exp, ["a1"], ["rmag"])
        tt(ang[:], li[:], dtt[:], ALU.mult, ["li", "dt"], ["ang"])
        for off, dst, nm in ((0.0, sin_t, "sin"), (math.pi / 2, cos_t, "cos")):
            ts(a1[:], ang[:], off, None, ALU.add, None, ["ang"], ["a1"])
            ts(kq[:], a1[:], 1.0 / TWO_PI, None, ALU.mult, None, ["a1"], ["kq"])
            P.op(V, lambda e: e.tensor_copy(kqi[:], kq[:]), reads=["kq"], writes=["kqi"])
            P.op(V, lambda e: e.tensor_copy(kq[:], kqi[:]), reads=["kqi"], writes=["kq"])
            stt(red[:], kq[:], -TWO_PI, a1[:], ALU.mult, ALU.add, ["kq", "a1"], ["red"])
            ts(m1[:], red[:], math.pi, None, ALU.is_gt, None, ["red"], ["m1"])
            stt(red[:], m1[:], -TWO_PI, red[:], ALU.mult, ALU.add, ["m1", "red"], ["red"])
            ts(m1[:], red[:], -math.pi, None, ALU.is_lt, None, ["red"], ["m1"])
            stt(red[:], m1[:], TWO_PI, red[:], ALU.mult, ALU.add, ["m1", "red"], ["red"])
            ts(red[:], red[:], -math.pi, math.pi, ALU.max, ALU.min, ["red"], ["red"])
            act(dst[:], red[:], AF.Sin, ["red"], [nm])
        tt(abre[:], rmag[:], cos_t[:], ALU.mult, ["rmag", "cos"], ["abre"])
        tt(abim[:], rmag[:], sin_t[:], ALU.mult, ["rmag", "sin"], ["abim"])
        P.op(V, lambda e: e.tensor_copy(Wre[:, 0, :], cos_t[:]), reads=["cos"], writes=["W"])
        P.op(V, lambda e: e.tensor_copy(Wim[:, 0, :], sin_t[:]), reads=["sin"], writes=["W"])
        for L in range(10):
            tt(a1[:], Wre[:, L, :], Wre[:, L, :], ALU.mult, ["W"], ["a1"])
            tt(kq[:], Wim[:, L, :], Wim[:, L, :], ALU.mult, ["W"], ["kq"])
            tt(red[:], Wre[:, L, :], Wim[:, L, :], ALU.mult, ["W"], ["red"])
            tt(Wre[:, L + 1, :], a1[:], kq[:], ALU.subtract, ["a1", "kq"], ["W"])
            ts(Wim[:, L + 1, :], red[:], 2.0, None, ALU.mult, None, ["red"], ["W"])
        tt(den[:], lr[:], lr[:], ALU.mult, ["lr"], ["den"])
        tt(tq[:], li[:], li[:], ALU.mult, ["li"], ["tq"])
        tt(den[:], den[:], tq[:], ALU.add, ["den", "tq"], ["den"])
        P.op(V, lambda e: e.reciprocal(den[:], den[:]), reads=["den"], writes=["den"])
        ts(am1[:], abre[:], -1.0, None, ALU.add, None, ["abre"], ["am1"])
        tt(fre[:], am1[:], lr[:], ALU.mult, ["am1", "lr"], ["fre"])
        tt(tq[:], abim[:], li[:], ALU.mult, ["abim", "li"], ["tq"])
        tt(fre[:], fre[:], tq[:], ALU.add, ["fre", "tq"], ["fre"])
        tt(fre[:], fre[:], den[:], ALU.mult, ["fre", "den"], ["fre"])
        tt(fim[:], abim[:], lr[:], ALU.mult, ["abim", "lr"], ["fim"])
        tt(tq[:], am1[:], li[:], ALU.mult, ["am1", "li"], ["tq"])
        tt(fim[:], fim[:], tq[:], ALU.subtract, ["fim", "tq"], ["fim"])
        tt(fim[:], fim[:], den[:], ALU.mult, ["fim", "den"], ["fim"])
        fre_b = fre[:].unsqueeze(2).to_broadcast([128, 32, 16])
        fim_b = fim[:].unsqueeze(2).to_broadcast([128, 32, 16])
        tt(bbre[:], bre[:], fre_b, ALU.mult, ["bre", "fre"], ["bbre"])
        tt(btmp[:], bim[:], fim_b, ALU.mult, ["bim", "fim"], ["btmp"])
        tt(bbre[:], bbre[:], btmp[:], ALU.subtract, ["bbre", "btmp"], ["bbre"])
        tt(bbim[:], bim[:], fre_b, ALU.mult, ["bim", "fre"], ["bbim"])
        tt(btmp[:], bre[:], fim_b, ALU.mult, ["bre", "fim"], ["btmp"])
        tt(bbim[:], bbim[:], btmp[:], ALU.add, ["bbim", "btmp"], ["bbim"])
        P.op("pool", lambda e: e.iota(bmi[:], [[-16, 8]], base=0, channel_multiplier=1), writes=["bmi"])
        ts(bm[:], bmi[:], 0.0, None, ALU.is_ge, None, ["bmi"], ["bm"])
        ts(bm2[:], bmi[:], 15.0, None, ALU.is_le, None, ["bmi"], ["bm2"])
        tt(bm[:], bm[:], bm2[:], ALU.mult, ["bm", "bm2"], ["bm"])
        ts(nbm[:], bm[:], -1.0, None, ALU.mult, None, ["bm"], ["nbm"])
        for c in range(2):
            for j in range(4):
                P.op("pool", lambda e, c=c, j=j: e.memset(bbpad[c][j][:], 0.0), writes=[("bbpad", c, j)])
        P.op("pool", lambda e: e.memset(pat[:], 1.0), writes=["pat"])
        P.op("pool", lambda e: e.memset(pat[:, :, 0:1], 0.0), writes=["pat"])
        for c, src in ((0, T.st_re), (1, T.st_im)):
            for pc in range(4):
                hb = (c * 4 + pc) % 2
                P.dma("sp", h0st[hb][:], src.ap()[:, pc * 1024:(pc + 1) * 1024], writes=[("h0st", hb)])
                for kk in range(8):
                    k = pc * 8 + kk
                    P.op("pe", lambda e, hb=hb, kk=kk, k=k: e.transpose(
                        mps[:, k, :], h0st[hb][:, kk * 128:(kk + 1) * 128], C.ident_f[0:16, 0:16]),
                        reads=[("h0st", hb), "ident_f"], writes=["mps"])
            P.op("act", lambda e, c=c: e.activation(h0T[c][:], mps[:], AF.Copy), writes=["mps", ("h0T", c)])

        segs = [(0, SEQ), (SEQ, SEQ), (TOKP, 128)]
        cnt = {"x": 0, "y": 0, "ysb": 0}
        for k in range(32):
            F, j = divmod(k, 4)
            r0 = j * 32
            if j == 0:
                ub = F % 2
                P.dma("sp", uT[ub][:], T.uT_s.ap()[F * 128:(F + 1) * 128, :], reads=[("u", F, ch) for ch in range(9)],
                      writes=[("uT", ub)])
            for c, bb in ((0, bbre), (1, bbim)):
                for a in range(2):
                    P.op(V, lambda e, c=c, a=a, bb=bb, k=k, j=j, r0=r0: e.tensor_copy(
                        bbpad[c][j][a * 64:(a + 1) * 64, r0 + 16 * a:r0 + 16 * a + 16], bb[a * 64:(a + 1) * 64, k, :]),
                        reads=["bbre" if c == 0 else "bbim"], writes=[("bbpad", c, j)])
                P.op("pe", lambda e, c=c, j=j: e.transpose(tps[:], bbpad[c][j][:], C.ident_b[:]),
                     reads=[("bbpad", c, j), "ident_b"], writes=["tps"])
                P.op("act", lambda e, c=c: e.activation(xl[c][:], tps[:], AF.Copy), writes=["tps", ("xl", c)])
            for c in range(2):
                msk = bm if c == 0 else nbm
                for a in range(2):
                    P.op(V, lambda e, c=c, a=a, F=F, j=j, msk=msk: e.tensor_scalar(
                        CT[c][:, a * 64:(a + 1) * 64], Cn[c][:, F, :], msk[:, 2 * j + a:2 * j + a + 1], None, ALU.mult),
                        reads=[f"Cn{c}", "bm", "nbm"], writes=[("CT", c)])
                P.op("pe", lambda e, c=c: e.transpose(tps[:], CT[c][:], C.ident_b[:]),
                     reads=[("CT", c), "ident_b"], writes=["tps"])
                P.op("act", lambda e, c=c, j=j: e.activation(yl[c][j][:], tps[:], AF.Copy),
                     writes=["tps", ("yl", c, j)])
            P.op(V, lambda e, k=k: e.tensor_copy(Dt[0][:, 0:1], Wre[:, 0, k:k + 1]), reads=["W"], writes=["D"])
            P.op(V, lambda e, k=k: e.tensor_copy(Dt[1][:, 0:1], Wim[:, 0, k:k + 1]), reads=["W"], writes=["D"])
            for L in range(11):
                n = 1 << L
                wr = Wre[:, L, k:k + 1]
                wi = Wim[:, L, k:k + 1]
                ts(dtmp[0][:, 0:n], Dt[1][:, 0:n], wi, None, ALU.mult, None, ["D", "W"], ["dtmp0"])
                ts(dtmp[1][:, 0:n], Dt[1][:, 0:n], wr, None, ALU.mult, None, ["D", "W"], ["dtmp1"])
                stt(Dt[0][:, n:2 * n], Dt[0][:, 0:n], wr, dtmp[0][:, 0:n], ALU.mult, ALU.subtract,
                    ["D", "W", "dtmp0"], ["D"])
                stt(Dt[1][:, n:2 * n], Dt[0][:, 0:n], wi, dtmp[1][:, 0:n], ALU.mult, ALU.add,
                    ["D", "W", "dtmp1"], ["D"])
            ts(decs[:], pat[:].rearrange("p b t -> p (b t)"), rmag[:, k:k + 1], None, ALU.mult, None,
               ["pat", "rmag"], ["decs"])
            ub = F % 2
            for si, (t0, ln) in enumerate(segs):
                nch = max(1, ln // 512)
                cw = min(ln, 512)
                for ch in range(nch):
                    xi = cnt["x"] % 2
                    cnt["x"] += 1
                    for c in range(2):
                        P.op("pe", lambda e, xi=xi, c=c, ub=ub, t0=t0, ch=ch, cw=cw: e.matmul(
                            xps[xi][c][:, 0:cw], xl[c][:], uT[ub][:, t0 + ch * 512:t0 + ch * 512 + cw],
                            start=True, stop=True),
                            reads=[("xl", c), ("uT", ub)], writes=[("xps", xi, c)])
                    sl = slice(ch * 512, ch * 512 + cw)
                    if si < 2:
                        dre, dim = Dt[0][:, sl], Dt[1][:, sl]
                        xr, xim = xps[xi][0][:, 0:cw], xps[xi][1][:, 0:cw]
                        o_re, o_im = xt[0][:, sl], xt[1][:, sl]
                        t_a, t_b = tmp[0][:, 0:cw], tmp[1][:, 0:cw]
                    else:
                        v3 = lambda ap: ap.rearrange("p (b t) -> p b t", t=8)
                        dre = Dt[0][:, 0:8].unsqueeze(1).to_broadcast([128, 16, 8])
                        dim = Dt[1][:, 0:8].unsqueeze(1).to_broadcast([128, 16, 8])
                        xr, xim = v3(xps[xi][0][:, 0:128]), v3(xps[xi][1][:, 0:128])
                        o_re, o_im = v3(xt[0][:, 0:128]), v3(xt[1][:, 0:128])
                        t_a, t_b = v3(tmp[0][:, 0:128]), v3(tmp[1][:, 0:128])
                    kx = [("xps", xi, 0), ("xps", xi, 1)]
                    P.op(V, lambda e, t_a=t_a, dim=dim, xim=xim: e.tensor_tensor(t_a, dim, xim, ALU.mult),
                         reads=["D"], writes=["tmp0"] + kx)
                    P.op(V, lambda e, o_re=o_re, dre=dre, xr=xr: e.tensor_tensor(o_re, dre, xr, ALU.mult),
                         reads=["D"], writes=["xt0"] + kx)
                    P.op(V, lambda e, o_re=o_re, t_a=t_a: e.tensor_tensor(o_re, o_re, t_a, ALU.add),
                         reads=["tmp0"], writes=["xt0"])
                    P.op(V, lambda e, t_b=t_b, dim=dim, xr=xr: e.tensor_tensor(t_b, dim, xr, ALU.mult),
                         reads=["D"], writes=["tmp1"] + kx)
                    P.op(V, lambda e, o_im=o_im, dre=dre, xim=xim: e.tensor_tensor(o_im, dre, xim, ALU.mult),
                         reads=["D"], writes=["xt1"] + kx)
                    P.op(V, lambda e, o_im=o_im, t_b=t_b: e.tensor_tensor(o_im, o_im, t_b, ALU.subtract),
                         reads=["tmp1"], writes=["xt1"])
                if si == 2:
                    for c in range(2):
                        xv = xt[c][:, 0:128].rearrange("p (b t) -> p b t", t=8)[:, :, 0]
                        P.op(V, lambda e, c=c, k=k, xv=xv: e.scalar_tensor_tensor(
                            xv, h0T[c][:, k, :], rmag[:, k:k + 1], xv, ALU.mult, ALU.add),
                            reads=[("h0T", c), "rmag"], writes=[f"xt{c}"])
                    dec = decs[:]
                else:
                    dec = rmag[:, k:k + 1].to_broadcast([128, ln])
                for c in range(2):
                    P.op(V, lambda e, c=c, dec=dec, ln=ln: e.tensor_tensor_scan(
                        gg[c][:, 0:ln], dec, xt[c][:, 0:ln], 0.0, ALU.mult, ALU.add),
                        reads=[f"xt{c}", "rmag", "decs"], writes=[f"g{c}"])
                for ch in range(nch):
                    sl = slice(ch * 512, ch * 512 + cw)
                    if si < 2:
                        dre, dim = Dt[0][:, sl], Dt[1][:, sl]
                        g_re, g_im = gg[0][:, sl], gg[1][:, sl]
                        o_re, o_im = xt[0][:, sl], xt[1][:, sl]
                        t_a, t_b = tmp[0][:, 0:cw], tmp[1][:, 0:cw]
                    else:
                        v3 = lambda ap: ap.rearrange("p (b t) -> p b t", t=8)
                        dre = Dt[0][:, 0:8].unsqueeze(1).to_broadcast([128, 16, 8])
                        dim = Dt[1][:, 0:8].unsqueeze(1).to_broadcast([128, 16, 8])
                        g_re, g_im = v3(gg[0][:, 0:128]), v3(gg[1][:, 0:128])
                        o_re, o_im = v3(xt[0][:, 0:128]), v3(xt[1][:, 0:128])
                        t_a, t_b = v3(tmp[0][:, 0:128]), v3(tmp[1][:, 0:128])
                    P.op(V, lambda e, t_a=t_a, dim=dim, g_im=g_im: e.tensor_tensor(t_a, dim, g_im, ALU.mult),
                         reads=["D", "g1"], writes=["tmp0"])
                    P.op(V, lambda e, o_re=o_re, dre=dre, g_re=g_re: e.tensor_tensor(o_re, dre, g_re, ALU.mult),
                         reads=["D", "g0"], writes=["xt0"])
                    P.op(V, lambda e, o_re=o_re, t_a=t_a: e.tensor_tensor(o_re, o_re, t_a, ALU.subtract),
                         reads=["tmp0"], writes=["xt0"])
                    P.op(V, lambda e, t_b=t_b, dim=dim, g_re=g_re: e.tensor_tensor(t_b, dim, g_re, ALU.mult),
                         reads=["D", "g0"], writes=["tmp1"])
                    P.op(V, lambda e, o_im=o_im, dre=dre, g_im=g_im: e.tensor_tensor(o_im, dre, g_im, ALU.mult),
                         reads=["D", "g1"], writes=["xt1"])
                    P.op(V, lambda e, o_im=o_im, t_b=t_b: e.tensor_tensor(o_im, o_im, t_b, ALU.add),
                         reads=["tmp1"], writes=["xt1"])
                for c in range(2):
                    P.op("act", lambda e, c=c, j=j, t0=t0, ln=ln: e.activation(
                        hbuf[:, j, c, t0:t0 + ln], xt[c][:, 0:ln], AF.Copy),
                        reads=[f"xt{c}"], writes=[("hbuf", j, c, si)])
                    if si < 2:
                        P.op("act", lambda e, c=c, k=k, si=si: e.activation(
                            hfp[c][:, si, k:k + 1], xt[c][:, SEQ - 1:SEQ], AF.Copy),
                            reads=[f"xt{c}"], writes=[("hfp", c)])
                    else:
                        P.op("act", lambda e, c=c, k=k: e.activation(
                            hfs[c][:, :, k], xt[c][:, 0:128].rearrange("p (b t) -> p b t", t=8)[:, :, 7], AF.Copy),
                            reads=[f"xt{c}"], writes=[("hfs", c)])
            if j == 3:
                for ch in range(9):
                    t0 = ch * 512
                    cw = 512 if ch < 8 else 128
                    si = 0 if ch < 4 else (1 if ch < 8 else 2)
                    yi = cnt["y"] % 2
                    cnt["y"] += 1
                    n = 0
                    for jj in range(4):
                        for c in range(2):
                            P.op("pe", lambda e, yi=yi, jj=jj, c=c, t0=t0, cw=cw, n=n: e.matmul(
                                yps[yi][:, 0:cw], yl[c][jj][:], hbuf[:, jj, c, t0:t0 + cw],
                                start=(n == 0), stop=(n == 7)),
                                reads=[("yl", c, jj), ("hbuf", jj, c, si)], writes=[("yps", yi)])
                            n += 1
                    bi = cnt["ysb"] % 2
                    cnt["ysb"] += 1
                    P.op(V, lambda e, yi=yi, bi=bi, ub=ub, F=F, t0=t0, cw=cw: e.scalar_tensor_tensor(
                        ysb[bi][:, 0:cw], uT[ub][:, t0:t0 + cw], dsk[:, F:F + 1], yps[yi][:, 0:cw],
                        ALU.mult, ALU.add),
                        reads=[("uT", ub), "dsk"], writes=[("yps", yi), ("ysb", bi)])
                    P.op("pool", lambda e, bi=bi, cw=cw: e.tensor_tensor(yw[bi][:, 0:cw], ysb[bi][:, 0:cw], ysb[bi][:, 0:cw], ALU.mult),
                         reads=[("ysb", bi)], writes=[("yw", bi)])
                    P.op("pool", lambda e, bi=bi, cw=cw: e.tensor_scalar(yw[bi][:, 0:cw], yw[bi][:, 0:cw], 0.044715, 1.0, ALU.mult, ALU.add),
                         reads=[("yw", bi)], writes=[("yw", bi)])
                    P.op("pool", lambda e, bi=bi, cw=cw: e.tensor_tensor(yw[bi][:, 0:cw], yw[bi][:, 0:cw], ysb[bi][:, 0:cw], ALU.mult),
                         reads=[("yw", bi), ("ysb", bi)], writes=[("yw", bi)])
                    P.op("act", lambda e, bi=bi, cw=cw: e.activation(yw[bi][:, 0:cw], yw[bi][:, 0:cw], AF.Sigmoid, scale=1.5957691216057308),
                         reads=[("yw", bi)], writes=[("yw", bi)])
                    P.op("pool", lambda e, bi=bi, cw=cw, t0=t0: e.tensor_tensor(hsT[:, t0:t0 + cw], yw[bi][:, 0:cw], ysb[bi][:, 0:cw], ALU.mult),
                         reads=[("yw", bi), ("ysb", bi)], writes=["hsT"])
                P.dma("sp", T.hsT_s.ap()[F * 128:(F + 1) * 128, :], hsT[:], reads=["hsT"], writes=[("hsT_s", F)])
        for c, dstp, dsts in ((0, T.ssm_p_re, T.ssm_s_re), (1, T.ssm_p_im, T.ssm_s_im)):
            P.op("pe", lambda e, c=c: e.transpose(yps[0][0:64, 0:128], hfp[c][:].rearrange("p s k -> p (s k)"), C.ident_f[:]),
                 reads=[("hfp", c), "ident_f"], writes=[("yps", 0)])
            P.op("act", lambda e: e.activation(hfo[0:64, :], yps[0][0:64, 0:128], AF.Copy), writes=[("yps", 0), "hfo"])
            for s_ in range(2):
                P.dma("sp", dstp.ap()[s_].rearrange("(k p) -> k p", p=128), hfo[s_ * 32:(s_ + 1) * 32, :],
                      reads=["hfo"], writes=[("ssm_p", c, s_)])
            for q in range(4):
                P.op("pe", lambda e, c=c, q=q: e.transpose(
                    yps[1][:, 0:128], hfs[c][:, q * 4:(q + 1) * 4, :].rearrange("p b k -> p (b k)"), C.ident_f[:]),
                    reads=[("hfs", c), "ident_f"], writes=[("yps", 1)])
                P.op("act", lambda e: e.activation(hfo[:, :], yps[1][:, 0:128], AF.Copy), writes=[("yps", 1), "hfo"])
                for bl in range(4):
                    P.dma("sp", dsts.ap()[q * 4 + bl].rearrange("(k p) -> k p", p=128), hfo[bl * 32:(bl + 1) * 32, :],
                          reads=["hfo"], writes=[("ssm_s", c, q, bl)])
        P.emit()


def phase3(nc, S, T, C):
    with ExitStack() as st:
        sb = lambda n, shape, dt: st.enter_context(nc.sbuf_tensor(n, shape, dt))
        ps = lambda n, shape, dt: st.enter_context(nc.psum_tensor(n, shape, dt))
        P = Prog(S)
        qTg = [sb(f"p3_qT{i}", [128, 4, SEQ], BF16) for i in range(2)]
        kTg = [sb(f"p3_kT{i}", [128, 4, SEQ], BF16) for i in range(2)]
        Vg = [sb(f"p3_V{i}", [128, 16, 512], BF16) for i in range(2)]
        qTs = sb("p3_qTs", [128, 12, 128], BF16)
        kTs = sb("p3_kTs", [128, 12, 128], BF16)
        kc32 = [sb(f"p3_kc32{i}", [128, 512], F32) for i in range(2)]
        vc32 = [sb(f"p3_vc32{i}", [128, 512], F32) for i in range(2)]
        kcb = [sb(f"p3_kcb{i}", [128, 512], BF16) for i in range(2)]
        vcb = [sb(f"p3_vcb{i}", [128, 512], BF16) for i in range(2)]
        kcT = [sb(f"p3_kcT{i}", [128, 4, 128], BF16) for i in range(2)]
        vnew = [sb(f"p3_vnew{i}", [8, 512], BF16) for i in range(2)]
        Pb = [sb(f"p3_Pb{i}", [128, 256], BF16) for i in range(4)]
        PTs = [sb(f"p3_PTs{i}", [128, 2, 128], BF16) for i in range(4)]
        stat = [sb(f"p3_stat{i}", [128, 16], F32) for i in range(2)]
        Osb = [sb(f"p3_Osb{i}", [128, 512], F32) for i in range(2)]
        sps = [ps(f"p3_sps{i}", [128, 2, 256], F32) for i in range(2)]
        ptp = [ps(f"p3_ptp{i}", [128, 4, 2, 128], BF16) for i in range(2)]
        ops_ = [ps(f"p3_ops{i}", [128, 8, 64], F32) for i in range(2)]
        kps = ps("p3_kps", [128, 4, 128], BF16)

        cnt = {"unit": 0, "head": 0}
        scale = 0.125

        def unit(nq, blocks, q_of, out_rows_U, out_rows_md, extra_reads):
            ui = cnt["unit"] % 2
            cnt["unit"] += 1
            c_lo = blocks[0][0]
            c_hi = blocks[-1][0] + blocks[-1][1]
            def res(h):
                hi = hbase + h
                sp = sps[hi % 2]
                sslot = (hi // 2) % 2
                skey = ("sps", hi % 2)
                pb = hi % 4
                tp = ptp[hi % 2]
                tslot = (hi // 2) % 4
                tkey = ("ptp", hi % 2)
                return sp, sslot, skey, pb, tp, tslot, tkey

            def stage_s(h):
                sp, sslot, skey, pb, tp, tslot, tkey = res(h)
                first = True
                for (col0, n, kT_of, v_of, rk) in blocks:
                    P.op("pe", lambda e, sp=sp, sslot=sslot, col0=col0, n=n, kT_of=kT_of, h=h, first=first: e.matmul(
                        sp[0:nq, sslot, col0:col0 + n], q_of(h), kT_of(h), start=first, stop=False,
                        skip_group_check=True),
                        reads=list(extra_reads) + list(rk), writes=[skey])
                    first = False
                P.op("pe", lambda e, sp=sp, sslot=sslot: e.matmul(
                    sp[0:nq, sslot, c_lo:c_hi], C.ident_b[0:nq, 0:nq], C.maskadd[0:nq, c_lo:c_hi],
                    start=False, stop=True, skip_group_check=True),
                    reads=["ident_b", "maskadd"], writes=[skey])
                P.op("dve", lambda e, sp=sp, sslot=sslot, h=h: e.tensor_reduce(
                    stat[ui][0:nq, h:h + 1], sp[0:nq, sslot, c_lo:c_hi], AX.X, ALU.max, negate=True),
                    writes=[skey, ("stat", ui, h)])
                P.op("act", lambda e, sp=sp, sslot=sslot, h=h, pb=pb: e.activation(
                    Pb[pb][0:nq, c_lo:c_hi], sp[0:nq, sslot, c_lo:c_hi], AF.Exp,
                    bias=stat[ui][0:nq, h:h + 1], accum_out=stat[ui][0:nq, 8 + h:9 + h]),
                    writes=[skey, ("stat", ui, h), ("Pb", pb)])

            def stage_t(h):
                sp, sslot, skey, pb, tp, tslot, tkey = res(h)
                for bi, (col0, n, kT_of, v_of, rk) in enumerate(blocks):
                    P.op("pe", lambda e, tp=tp, tslot=tslot, bi=bi, col0=col0, n=n, pb=pb: e.transpose(
                        tp[0:n, tslot, bi, 0:nq], Pb[pb][0:nq, col0:col0 + n], C.ident_b[0:nq, 0:nq]),
                        reads=[("Pb", pb), "ident_b"], writes=[tkey])
                for bi, (col0, n, kT_of, v_of, rk) in enumerate(blocks):
                    if h % 2 == 0:
                        P.op("dve", lambda e, tp=tp, tslot=tslot, bi=bi, n=n, pb=pb: e.tensor_copy(
                            PTs[pb][0:n, bi, 0:nq], tp[0:n, tslot, bi, 0:nq]),
                            writes=[tkey, ("PTs", pb, bi)])
                    else:
                        P.op("act", lambda e, tp=tp, tslot=tslot, bi=bi, n=n, pb=pb: e.activation(
                            PTs[pb][0:n, bi, 0:nq], tp[0:n, tslot, bi, 0:nq], AF.Copy),
                            writes=[tkey, ("PTs", pb, bi)])

            def stage_v(h):
                sp, sslot, skey, pb, tp, tslot, tkey = res(h)
                for bi, (col0, n, kT_of, v_of, rk) in enumerate(blocks):
                    P.op("pe", lambda e, h=h, bi=bi, n=n, pb=pb, v_of=v_of: e.matmul(
                        ops_[ui][0:nq, h, :], PTs[pb][0:n, bi, 0:nq], v_of(h),
                        start=(bi == 0), stop=(bi == len(blocks) - 1), skip_group_check=True),
                        reads=[("PTs", pb, bi)] + list(rk), writes=[("ops", ui)])

            hbase = cnt["head"]
            cnt["head"] += 8
            for i in range(10):
                if i < 8:
                    stage_s(i)
                if 1 <= i <= 8:
                    stage_t(i - 1)
                if 2 <= i <= 9:
                    stage_v(i - 2)
            P.op("act", lambda e, ui=ui: e.activation(
                Osb[ui][0:nq, :], ops_[ui][0:nq, :, :].rearrange("p h e -> p (h e)"), AF.Copy),
                writes=[("ops", ui), ("Osb", ui)])
            P.dma("sp", out_rows_U, Osb[ui][0:nq, :], reads=[("Osb", ui)], writes=[("U_s", cnt["unit"])])
            P.dma("sp", out_rows_md, stat[ui][0:nq, :], reads=[("stat", ui, h) for h in range(8)], writes=[("md_s", cnt["unit"])])

        gi = 0
        for seq in DBG.get("p3_seqs", range(NPS)):
            for g in range(3):
                d = DILS[g]
                bsel = gi % 2
                gi += 1
                c0 = seq * SEQ
                P.dma("sp", qTg[bsel][:], T.qT_s.ap()[g * 512:(g + 1) * 512, c0:c0 + SEQ].rearrange("(hp p) t -> p hp t", p=128),
                      reads=[("q", 4 * g + f, ch) for f in range(4) for ch in range(8)], writes=[("qTg", bsel)])
                P.dma("sp", kTg[bsel][:], T.kT_s.ap()[g * 512:(g + 1) * 512, c0:c0 + SEQ].rearrange("(hp p) t -> p hp t", p=128),
                      reads=[("k", 4 * g + f, ch) for f in range(4) for ch in range(8)], writes=[("kTg", bsel)])
                nb = SEQ // d // 128
                for r in range(d):
                    for blk in range(nb):
                        ux = r * nb + blk
                        row0 = c0 + r + d * blk * 128
                        P.dma("sp", Vg[bsel][:, ux, :], T.vtok_s.ap()[ss(row0, 128, d), g * 512:(g + 1) * 512],
                              reads=[("vtok_s", ti, g) for ti in range(33)], writes=[("Vg", bsel, ux)])
                for r in range(d):
                    for blk in [b_ for b_ in DBG.get("p3_blks", range(nb)) if b_ < nb]:
                        ux = r * nb + blk
                        tq0 = r + d * blk * 128

                        def q_of(h, bsel=bsel, tq0=tq0, d=d):
                            return qTg[bsel][64 * (h % 2):64 * (h % 2) + 64, h // 2, ss(tq0, 128, d)]

                        blocks = []
                        if blk > 0:
                            tk0 = r + d * (blk - 1) * 128
                            blocks.append((0, 128,
                                           lambda h, bsel=bsel, tk0=tk0, d=d: kTg[bsel][64 * (h % 2):64 * (h % 2) + 64, h // 2, ss(tk0, 128, d)],
                                           lambda h, bsel=bsel, ux=ux: Vg[bsel][:, ux - 1, h * 64:(h + 1) * 64],
                                           [("kTg", bsel), ("Vg", bsel, ux - 1)]))
                        blocks.append((128, 128,
                                       lambda h, bsel=bsel, tq0=tq0, d=d: kTg[bsel][64 * (h % 2):64 * (h % 2) + 64, h // 2, ss(tq0, 128, d)],
                                       lambda h, bsel=bsel, ux=ux: Vg[bsel][:, ux, h * 64:(h + 1) * 64],
                                       [("kTg", bsel), ("Vg", bsel, ux)]))
                        rows = ss(c0 + tq0, 128, d)
                        unit(128, blocks, q_of, T.U_s[g].ap()[rows, :], T.md_s[g].ap()[rows, :], [("qTg", bsel)])
        P.dma("sp", qTs[:], T.qT_s.ap()[:, TOKP:TOK].rearrange("(f p) t -> p f t", p=128),
              reads=[("q", f, 8) for f in range(12)], writes=["qTs"])
        P.dma("sp", kTs[:], T.kT_s.ap()[:, TOKP:TOK].rearrange("(f p) t -> p f t", p=128),
              reads=[("k", f, 8) for f in range(12)], writes=["kTs"])
        caches = (T.c128, T.c512, T.c2048)
        ci = 0
        for b in DBG.get("p3_bs", range(NSS)):
            for g in range(3):
                d = DILS[g]
                nq = max(1, DEC // d)
                for r in range(min(d, DEC)):
                    cb = ci % 2
                    ci += 1
                    csrc = caches[g].ap()[b, ss(r, 128, d), :, :]
                    P.dma("sp", kc32[cb][:], csrc[:, 0, :], writes=[("kc32", cb)])
                    P.dma("sp", vc32[cb][:], csrc[:, 1, :], writes=[("vc32", cb)])
                    tok0 = TOKP + b * DEC + r
                    P.dma("sp", vnew[cb][0:nq, :], T.vtok_s.ap()[ss(tok0, nq, d), g * 512:(g + 1) * 512],
                          reads=[("vtok_s", 32, g)], writes=[("vnew", cb)])
                    P.op("pool", lambda e, cb=cb: e.tensor_copy(kcb[cb][:], kc32[cb][:]), reads=[("kc32", cb)], writes=[("kcb", cb)])
                    P.op("pool", lambda e, cb=cb: e.tensor_copy(vcb[cb][:], vc32[cb][:]), reads=[("vc32", cb)], writes=[("vcb", cb)])
                    for hp in range(4):
                        P.op("pe", lambda e, cb=cb, hp=hp: e.transpose(kps[:, hp, :], kcb[cb][:, hp * 128:(hp + 1) * 128], C.ident_b[:]),
                             reads=[("kcb", cb), "ident_b"], writes=["kps"])
                    P.op("dve", lambda e, cb=cb: e.tensor_copy(kcT[cb][:], kps[:]), writes=["kps", ("kcT", cb)])
                    col = b * DEC + r

                    def q_of(h, g=g, col=col, nq=nq, d=d):
                        return qTs[64 * (h % 2):64 * (h % 2) + 64, 4 * g + h // 2, ss(col, nq, d)]

                    blocks = [
                        (0, 128,
                         lambda h, cb=cb: kcT[cb][64 * (h % 2):64 * (h % 2) + 64, h // 2, :],
                         lambda h, cb=cb: vcb[cb][:, h * 64:(h + 1) * 64],
                         [("kcT", cb), ("vcb", cb)]),
                        (128, nq,
                         lambda h, g=g, col=col, nq=nq, d=d: kTs[64 * (h % 2):64 * (h % 2) + 64, 4 * g + h // 2, ss(col, nq, d)],
                         lambda h, cb=cb, nq=nq: vnew[cb][0:nq, h * 64:(h + 1) * 64],
                         ["kTs", ("vnew", cb)]),
                    ]
                    rows = ss(tok0, nq, d)
                    unit(nq, blocks, q_of, T.U_s[g].ap()[rows, :], T.md_s[g].ap()[rows, :], ["qTs"])
        P.emit()


def load_weight_bf16(P, nc, dst, src_ap, kch, ncols, wst, tag, scale_ap=None, engs=("dve", "pool")):
    src = src_ap.rearrange("(k p) f -> p k f", p=128)
    cw = 2048 // kch
    n = 0
    for c in range(0, ncols, cw):
        b = n % 2
        P.dma("sp", wst[b][:, 0:kch * cw].rearrange("p (k f) -> p k f", k=kch), src[:, :, c:c + cw],
              writes=[("wst", b)])
        for k in range(kch):
            eng = engs[n % len(engs)]
            n2 = n
            if scale_ap is None:
                P.op(eng, lambda e, b=b, k=k, c=c: e.tensor_copy(
                    dst[:, k, c:c + cw], wst[b][:, k * cw:(k + 1) * cw]),
                    reads=[("wst", b)], writes=[(tag, c)])
            else:
                P.op(eng, lambda e, b=b, k=k, c=c: e.tensor_scalar(
                    dst[:, k, c:c + cw], wst[b][:, k * cw:(k + 1) * cw], scale_ap[:, k:k + 1], None, ALU.mult),
                    reads=[("wst", b), tag + "_scale"], writes=[(tag, c)])
        n += 1
    return [(tag, c) for c in range(0, ncols, cw)]


def phase4(nc, S, T, C):
    with ExitStack() as st:
        sb = lambda n, shape, dt: st.enter_context(nc.sbuf_tensor(n, shape, dt))
        ps = lambda n, shape, dt: st.enter_context(nc.psum_tensor(n, shape, dt))
        P = Prog(S)
        Wa = sb("p4_Wa", [128, 8, D], BF16)
        Wb = sb("p4_Wb", [128, 8, D], BF16)
        Wp = sb("p4_Wp", [128, 4, D], BF16)
        Wo = sb("p4_Wo", [128, 8, D], BF16)
        wst = [sb(f"p4_wst{i}", [128, 2048], F32) for i in range(2)]
        U = [sb(f"p4_U{i}", [128, 3, 512], F32) for i in range(2)]
        md = [sb(f"p4_md{i}", [128, 3, 16], F32) for i in range(2)]
        nM = sb("p4_nM", [128, 8], F32)
        tdf = sb("p4_tdf", [128, 3, 8], F32)
        aw = sb("p4_aw", [128, 3, 8], F32)
        ad = sb("p4_ad", [128, 3, 8], F32)
        Z = sb("p4_Z", [128, 8], F32)
        acc = sb("p4_acc", [128, 512], F32)
        acc2 = sb("p4_acc2", [128, 512], F32)
        attn_b = sb("p4_attn_b", [128, 512], BF16)
        attnT = [sb(f"p4_attnT{i}", [128, 4, 128], BF16) for i in range(2)]
        hsT = [sb(f"p4_hsT{i}", [128, 8, 128], BF16) for i in range(2)]
        sga = [sb(f"p4_sga{i}", [128, 8, 128], F32) for i in range(2)]
        sgb = [sb(f"p4_sgb{i}", [128, 8, 128], F32) for i in range(2)]
        sig = [sb(f"p4_sig{i}", [128, 128], F32) for i in range(2)]
        t1 = [sb(f"p4_t1{i}", [128, 128], F32) for i in range(2)]
        t2 = [sb(f"p4_t2{i}", [128, 128], F32) for i in range(2)]
        mixT = [sb(f"p4_mixT{i}", [128, 8, 128], BF16) for i in range(2)]
        xs = [sb(f"p4_xs{i}", [128, D], F32) for i in range(2)]
        x1 = [sb(f"p4_x1{i}", [128, D], F32) for i in range(2)]
        ptp = ps("p4_ptp", [128, 4, 128], BF16)
        pabc = [ps(f"p4_pabc{i}", [128, 3, 128], F32) for i in range(2)]
        pout = [ps(f"p4_pout{i}", [128, 512], F32) for i in range(4)]

        ka = load_weight_bf16(P, nc, Wa, T.w_glu_a.ap(), 8, D, wst, "Wa")
        kb = load_weight_bf16(P, nc, Wb, T.w_glu_b.ap(), 8, D, wst, "Wb")
        kp = load_weight_bf16(P, nc, Wp, T.w_attn.ap(), 4, D, wst, "Wp")
        ko = load_weight_bf16(P, nc, Wo, T.w_out.ap(), 8, D, wst, "Wo")

        V = "dve"
        cnt = {"f": 0, "po": 0}
        for ti in DBG.get("p4_tiles", range(NTILE)):
            b = ti % 2
            t0 = ti * 128
            for g in range(3):
                P.dma("sp", U[b][:, g, :], T.U_s[g].ap()[t0:t0 + 128, :], writes=[("U", b)])
                P.dma("sp", md[b][:, g, :], T.md_s[g].ap()[t0:t0 + 128, :], writes=[("md", b)])
            P.dma("sp", hsT[b][:], T.hsT_s.ap()[:, t0:t0 + 128].rearrange("(k p) t -> p k t", p=128), writes=[("hsT", b)])
            P.dma("sp", sga[b][:], T.sga_s.ap()[:, t0:t0 + 128].rearrange("(k p) t -> p k t", p=128), writes=[("sga", b)])
            P.dma("sp", sgb[b][:], T.sgb_s.ap()[:, t0:t0 + 128].rearrange("(k p) t -> p k t", p=128), writes=[("sgb", b)])
            P.dma("sp", xs[b][:], T.x.ap()[t0:t0 + 128, :], writes=[("xs", b)])
            P.op(V, lambda e, b=b: e.tensor_tensor(nM[:], md[b][:, 0, 0:8], md[b][:, 1, 0:8], ALU.min), reads=[("md", b)], writes=["nM"])
            P.op(V, lambda e, b=b: e.tensor_tensor(nM[:], nM[:], md[b][:, 2, 0:8], ALU.min), reads=[("md", b), "nM"], writes=["nM"])
            P.op(V, lambda e, b=b: e.tensor_tensor(tdf[:], md[b][:, :, 0:8], nM[:].unsqueeze(1).to_broadcast([128, 3, 8]), ALU.subtract),
                 reads=[("md", b), "nM"], writes=["tdf"])
            P.op("act", lambda e: e.activation(aw[:], tdf[:], AF.Exp, scale=-1.0), reads=["tdf"], writes=["aw"])
            P.op(V, lambda e, b=b: e.tensor_tensor(ad[:], aw[:], md[b][:, :, 8:16], ALU.mult), reads=["aw", ("md", b)], writes=["ad"])
            P.op(V, lambda e: e.tensor_tensor(Z[:], ad[:, 0, :], ad[:, 1, :], ALU.add), reads=["ad"], writes=["Z"])
            P.op(V, lambda e: e.tensor_tensor(Z[:], Z[:], ad[:, 2, :], ALU.add), reads=["ad", "Z"], writes=["Z"])
            P.op(V, lambda e: e.reciprocal(Z[:], Z[:]), reads=["Z"], writes=["Z"])
            P.op(V, lambda e: e.tensor_tensor(aw[:], aw[:], Z[:].unsqueeze(1).to_broadcast([128, 3, 8]), ALU.mult),
                 reads=["aw", "Z"], writes=["aw"])
            wbs = [aw[:, g, :].unsqueeze(2).to_broadcast([128, 8, 64]) for g in range(3)]
            u3s = [U[b][:, g, :].rearrange("p (h e) -> p h e", e=64) for g in range(3)]
            acc3 = acc[:].rearrange("p (h e) -> p h e", e=64)
            acc23 = acc2[:].rearrange("p (h e) -> p h e", e=64)
            P.op(V, lambda e, u=u3s[0], w=wbs[0]: e.tensor_tensor(acc3, u, w, ALU.mult), reads=[("U", b), "aw"], writes=["acc"])
            P.op(V, lambda e, u=u3s[1], w=wbs[1]: e.tensor_tensor(acc23, u, w, ALU.mult), reads=[("U", b), "aw"], writes=["acc2"])
            P.op(V, lambda e: e.tensor_tensor(acc[:], acc[:], acc2[:], ALU.add), reads=["acc", "acc2"], writes=["acc"])
            P.op(V, lambda e, u=u3s[2], w=wbs[2]: e.tensor_tensor(acc23, u, w, ALU.mult), reads=[("U", b), "aw"], writes=["acc2"])
            P.op(V, lambda e: e.tensor_tensor(attn_b[:], acc[:], acc2[:], ALU.add), reads=["acc", "acc2"], writes=["attn_b"])
            for c in range(4):
                P.op("pe", lambda e, c=c: e.transpose(ptp[:, c, :], attn_b[:, c * 128:(c + 1) * 128], C.ident_b[:]),
                     reads=["attn_b", "ident_b"], writes=["ptp"])
            P.op("act", lambda e, b=b: e.activation(attnT[b][:], ptp[:], AF.Copy), writes=["ptp", ("attnT", b)])
            for F in range(8):
                fi = cnt["f"] % 2
                cnt["f"] += 1
                pk = ("pabc", fi)
                for k in range(8):
                    P.op("pe", lambda e, fi=fi, k=k, F=F, b=b: e.matmul(
                        pabc[fi][:, 0, :], Wa[:, k, F * 128:(F + 1) * 128], hsT[b][:, k, :], start=(k == 0), stop=(k == 7),
                        skip_group_check=True), reads=ka + [("hsT", b)], writes=[pk])
                for k in range(8):
                    P.op("pe", lambda e, fi=fi, k=k, F=F, b=b: e.matmul(
                        pabc[fi][:, 1, :], Wb[:, k, F * 128:(F + 1) * 128], hsT[b][:, k, :], start=False, stop=(k == 7),
                        skip_group_check=True), reads=kb + [("hsT", b)], writes=[pk])
                for k in range(4):
                    P.op("pe", lambda e, fi=fi, k=k, F=F, b=b: e.matmul(
                        pabc[fi][:, 2, :], Wp[:, k, F * 128:(F + 1) * 128], attnT[b][:, k, :], start=False, stop=(k == 3),
                        skip_group_check=True), reads=kp + [("attnT", b)], writes=[pk])
                P.op("act", lambda e, fi=fi: e.activation(sig[fi][:], pabc[fi][:, 1, :], AF.Sigmoid), writes=[pk, ("sig", fi)])
                P.op(V, lambda e, fi=fi: e.tensor_tensor(t1[fi][:], pabc[fi][:, 0, :], sig[fi][:], ALU.mult),
                     reads=[("sig", fi)], writes=[pk, ("t1", fi)])
                P.op(V, lambda e, fi=fi, F=F, b=b: e.tensor_tensor(t2[fi][:], pabc[fi][:, 2, :], sgb[b][:, F, :], ALU.mult),
                     reads=[("sgb", b)], writes=[pk, ("t2", fi)])
                P.op("pool", lambda e, fi=fi, F=F, b=b: e.tensor_tensor(t1[fi][:], t1[fi][:], sga[b][:, F, :], ALU.mult),
                     reads=[("sga", b)], writes=[("t1", fi)])
                P.op("pool", lambda e, fi=fi, F=F, b=b: e.tensor_tensor(mixT[b][:, F, :], t1[fi][:], t2[fi][:], ALU.add),
                     reads=[("t1", fi), ("t2", fi)], writes=[("mixT", b)])
            for half in range(2):
                po = cnt["po"] % 4
                cnt["po"] += 1
                for k in range(8):
                    P.op("pe", lambda e, po=po, k=k, half=half, b=b: e.matmul(
                        pout[po][:], mixT[b][:, k, :], Wo[:, k, half * 512:(half + 1) * 512], start=(k == 0), stop=(k == 7)),
                        reads=ko + [("mixT", b)], writes=[("pout", po)])
                P.op(V, lambda e, po=po, half=half, b=b: e.tensor_tensor(
                    x1[b][:, half * 512:(half + 1) * 512], pout[po][:], xs[b][:, half * 512:(half + 1) * 512], ALU.add),
                    reads=[("xs", b)], writes=[("pout", po), ("x1", b)])
            P.dma("sp", T.x1_s.ap()[t0:t0 + 128, :], x1[b][:], reads=[("x1", b)], writes=[("x1_s", ti)])
        P.emit()


def phase5a(nc, S, T, C):
    with ExitStack() as st:
        sb = lambda n, shape, dt: st.enter_context(nc.sbuf_tensor(n, shape, dt))
        P = Prog(S)
        R = 4
        NBUF = 4
        cin = [sb(f"p5a_in{i}", [128, R, D], F32) for i in range(NBUF)]
        cout = [sb(f"p5a_out{i}", [128, R, D], BF16) for i in range(NBUF)]
        nexp = T.u_tab.shape[0]
        nblk = nexp // (128 * R)
        engs = ("act", "dve", "pool")
        n = 0
        for blk in range(nblk):
            r0 = blk * 128 * R
            for c, tab in ((0, T.u_tab), (1, T.v_tab)):
                i = n % NBUF
                P.dma("sp", cin[i][:], tab.ap()[r0:r0 + 128 * R, :].rearrange("(p r) d -> p r d", r=R), writes=[("cin", i)])
                eng = engs[n % 3]
                if eng == "act":
                    P.op("act", lambda e, i=i: e.activation(cout[i][:], cin[i][:], AF.Copy), reads=[("cin", i)], writes=[("cout", i)])
                else:
                    P.op(eng, lambda e, i=i: e.tensor_copy(cout[i][:], cin[i][:]), reads=[("cin", i)], writes=[("cout", i)])
                P.dma("sp", T.uv_s.ap()[r0:r0 + 128 * R, c, :].rearrange("(p r) d -> p r d", r=R), cout[i][:],
                      reads=[("cout", i)], writes=[("uv_s", blk, c)])
                n += 1
        P.emit()


def phase5(nc, S, T, C):
    NEG = -1.0e30
    with ExitStack() as st:
        sb = lambda n, shape, dt: st.enter_context(nc.sbuf_tensor(n, shape, dt))
        ps = lambda n, shape, dt: st.enter_context(nc.psum_tensor(n, shape, dt))
        P = Prog(S)
        V = "dve"
        Wq = sb("p5_Wq", [128, 8, 2048], BF16)
        wst = [sb(f"p5_wst{i}", [128, 2048], F32) for i in range(2)]
        gk = sb("p5_gk", [128, 8], F32)
        skT = sb("p5_skT", [128, 16, 128], BF16)
        skb = [sb(f"p5_skb{i}", [128, 128], BF16) for i in range(2)]
        gffn = sb("p5_gffn", [128, D], F32)
        gfin = sb("p5_gfin", [128, D], F32)
        io16 = sb("p5_io16", [128, 256], I32)
        io256 = sb("p5_io256", [128, 256], F32)
        x1 = [sb(f"p5_x1{i}", [128, D], F32) for i in range(2)]
        xn2s = [sb(f"p5_xn2s{i}", [128, D], BF16) for i in range(2)]
        eidx_is = [sb(f"p5_eidx_is{i}", [128, 128], I32) for i in range(2)]
        gates = [sb(f"p5_gates{i}", [128, 128], F32) for i in range(2)]
        NYIELD = 2
        ss = sb("p5_ss", [128, 4], F32)
        xn2b = sb("p5_xn2b", [128, D], BF16)
        xn2T = sb("p5_xn2T", [128, 8, 128], BF16)
        qpT = sb("p5_qpT", [128, 16, 128], BF16)
        sc = sb("p5_sc", [128, 16, 128], F32)
        scw = sb("p5_scw", [128, 16, 128], F32)
        sv = sb("p5_sv", [128, 16, 16], F32)
        si = sb("p5_si", [128, 16, 16], U32)
        sif = sb("p5_sif", [128, 16, 16], F32)
        cand = sb("p5_cand", [128, 8, 256], F32)
        candw = sb("p5_candw", [128, 8, 256], F32)
        cidx = sb("p5_cidx", [128, 8, 256], F32)
        best = sb("p5_best", [128, 8, 16], F32)
        pos = sb("p5_pos", [128, 8, 16], U32)
        posf = sb("p5_posf", [128, 8, 16], F32)
        eqb = sb("p5_eqb", [128, 8, 256], F32)
        eidx = sb("p5_eidx", [128, 128], F32)
        ex = sb("p5_ex", [128, 8, 16], F32)
        gs = sb("p5_gs", [128, 8], F32)
        dots = sb("p5_dots", [128, 128], F32)
        gw = [sb(f"p5_gw{i}", [128, 8], F32) for i in range(2)]
        wgt = sb("p5_wgt", [128, 128], F32)
        NB = 16
        pairs = [sb(f"p5_gp{i}", [128, 2, 2 * D], BF16)[:] for i in range(4)]
        gb_extra = {}
        for i_ in range(2):
            pairs.append(wst[i_][:].bitcast(BF16).rearrange("p (s d) -> p s d", s=2))
            gb_extra[8 + 2 * i_] = ("wst", i_)
            gb_extra[9 + 2 * i_] = ("wst", i_)
        for i_ in range(2):
            pairs.append(sb(f"p5_gpx{i_}", [128, 2, 2 * D], BF16)[:])
        gbv = lambda i: pairs[i // 2][:, i % 2, :]
        gbk = lambda i: [("gb", i)] + ([gb_extra[i]] if i in gb_extra else [])
        prod = [sb(f"p5_prod{i}", [128, 2, D], BF16) for i in range(2)]
        junk = sb("p5_junk", [128, D], BF16)
        diag = [sb(f"p5_diag{i}", [128, 4, 128], BF16) for i in range(2)]
        yb = sb("p5_yb", [128, D], F32)
        pacc = [ps(f"p5_pacc{i}", [128, 512], F32) for i in range(2)]
        uv2d = T.uv_s.ap().rearrange("n c d -> n (c d)")
        ptp = ps("p5_ptp", [128, 8, 128], BF16)
        pq = [ps(f"p5_pq{i}", [128, 4, 128], F32) for i in range(2)]
        psc = [ps(f"p5_psc{i}", [128, 4, 128], F32) for i in range(2)]

        bc_reg = nc.gpsimd.alloc_register("p5_bc")
        nc.gpsimd.reg_mov(bc_reg, T.u_tab.shape[0] - 1)
        P.dma("sp", gk[:], T.g_ffn.ap().rearrange("(k p) -> p k", p=128), writes=["Wq_scale"], allow_slow_non_contiguous=True)
        kq = load_weight_bf16(P, nc, Wq, T.w_qp.ap(), 8, 2048, wst, "Wq", scale_ap=gk)
        P.dma("sp", gffn[:], T.g_ffn.ap().partition_broadcast(128), writes=["gffn"])
        P.dma("sp", gfin[:], T.g_final.ap().partition_broadcast(128), writes=["gfin"])
        P.op("pool", lambda e: e.iota(io16[:], [[1, 256]], base=0, channel_multiplier=0), writes=["io16"])
        P.op(V, lambda e: e.tensor_copy(io256[:], io16[:]), reads=["io16"], writes=["io256"])
        for hp in range(16):
            b = hp % 2
            P.dma("sp", wst[b][:, 0:128], T.sub_keys.ap()[hp], writes=[("wst", b)])
            P.op(V, lambda e, b=b: e.tensor_copy(skb[b][:], wst[b][:, 0:128]), reads=[("wst", b)], writes=[("skb", b)])
            P.op("pe", lambda e, b=b: e.transpose(ptp[:, 0, :], skb[b][:], C.ident_b[:]), reads=[("skb", b), "ident_b"], writes=["ptp"])
            P.op("act", lambda e, hp=hp: e.activation(skT[:, hp, :], ptp[:, 0, :], AF.Copy), writes=["ptp", "skT"])

        def rms(src, col, reads):
            P.op("act", lambda e: e.activation(junk[:], src, AF.Square, accum_out=ss[:, col:col + 1]),
                 reads=reads, writes=["junk", ("ss", col)])
            P.op(V, lambda e: e.tensor_scalar(ss[:, col:col + 1], ss[:, col:col + 1], 1.0 / D, EPS, ALU.mult, ALU.add),
                 reads=[("ss", col)], writes=[("ss", col)])
            P.op("act", lambda e: e.activation(ss[:, col:col + 1], ss[:, col:col + 1], AF.Sqrt), reads=[("ss", col)], writes=[("ss", col)])
            P.op(V, lambda e: e.reciprocal(ss[:, col:col + 1], ss[:, col:col + 1]), reads=[("ss", col)], writes=[("ss", col)])

        def route(ti):
            b = ti % 2
            t0 = ti * 128
            P.dma("sp", x1[b][:], T.x1_s.ap()[t0:t0 + 128, :], reads=[("x1_s", ti)], writes=[("x1", b)])
            rms(x1[b][:], 0, [("x1", b)])
            P.op(V, lambda e, b=b: e.scalar_tensor_tensor(xn2s[b][:], x1[b][:], ss[:, 0:1], gffn[:], ALU.mult, ALU.mult),
                 reads=[("x1", b), ("ss", 0), "gffn"], writes=[("xn2", b)])
            P.op("pool", lambda e, b=b: e.tensor_scalar(xn2b[:], x1[b][:], ss[:, 0:1], None, ALU.mult),
                 reads=[("x1", b), ("ss", 0)], writes=["xn2b"])
            for k in range(8):
                P.op("pe", lambda e, k=k: e.transpose(ptp[:, k, :], xn2b[:, k * 128:(k + 1) * 128], C.ident_b[:]),
                     reads=["xn2b", "ident_b"], writes=["ptp"])
            P.op("act", lambda e: e.activation(xn2T[:], ptp[:], AF.Copy), writes=["ptp", "xn2T"])
            yield
            for j in range(4):
                pj = j % 2
                for hh in range(4):
                    hp = 4 * j + hh
                    for k in range(8):
                        P.op("pe", lambda e, pj=pj, hh=hh, hp=hp, k=k: e.matmul(
                            pq[pj][:, hh, :], Wq[:, k, hp * 128:(hp + 1) * 128], xn2T[:, k, :],
                            start=(k == 0 and hh == 0), stop=(k == 7), skip_group_check=True),
                            reads=kq + ["xn2T"], writes=[("pq", pj)])
                eng = "act" if j % 2 == 0 else V
                if eng == "act":
                    P.op("act", lambda e, pj=pj, j=j: e.activation(qpT[:, 4 * j:4 * j + 4, :], pq[pj][:], AF.Copy),
                         writes=[("pq", pj), ("qpT", j)])
                else:
                    P.op(V, lambda e, pj=pj, j=j: e.tensor_copy(qpT[:, 4 * j:4 * j + 4, :], pq[pj][:]),
                         writes=[("pq", pj), ("qpT", j)])
                yield
            for j in range(4):
                pj = j % 2
                for hh in range(4):
                    hp = 4 * j + hh
                    P.op("pe", lambda e, pj=pj, hh=hh, hp=hp: e.matmul(
                        psc[pj][:, hh, :], qpT[:, hp, :], skT[:, hp, :], start=(hh == 0), stop=True, skip_group_check=True),
                        reads=[("qpT", j), "skT"], writes=[("psc", pj)])
                P.op("act", lambda e, pj=pj, j=j: e.activation(sc[:, 4 * j:4 * j + 4, :], psc[pj][:], AF.Copy),
                     writes=[("psc", pj), ("sc", j)])
                yield
            for step in range(5):
                for hp in range(16):
                    j = hp // 4
                    if step == 0:
                        P.op(V, lambda e, hp=hp: e.max(sv[:, hp, 0:8], sc[:, hp, :]), reads=[("sc", j)], writes=[("sv", hp)])
                    elif step == 1:
                        P.op(V, lambda e, hp=hp: e.max_index(si[:, hp, 0:8], sv[:, hp, 0:8], sc[:, hp, :]),
                             reads=[("sc", j), ("sv", hp)], writes=[("si", hp)])
                    elif step == 2:
                        P.op(V, lambda e, hp=hp: e.match_replace(scw[:, hp, :], sv[:, hp, 0:8], sc[:, hp, :], NEG),
                             reads=[("sc", j), ("sv", hp)], writes=[("scw", hp)])
                    elif step == 3:
                        P.op(V, lambda e, hp=hp: e.max(sv[:, hp, 8:16], scw[:, hp, :]), reads=[("scw", hp)], writes=[("sv", hp)])
                    else:
                        P.op(V, lambda e, hp=hp: e.max_index(si[:, hp, 8:16], sv[:, hp, 8:16], scw[:, hp, :]),
                             reads=[("scw", hp), ("sv", hp)], writes=[("si", hp)])
                    if hp % 4 == 3:
                        yield
            svk = [("sv", hp) for hp in range(16)]
            sik = [("si", hp) for hp in range(16)]
            P.op(V, lambda e: e.tensor_copy(sif[:], si[:]), reads=sik, writes=["sif"])
            sv4 = sv[:].rearrange("p (h two) k -> p h two k", two=2)
            sif4 = sif[:].rearrange("p (h two) k -> p h two k", two=2)
            c4 = lambda t: t[:].rearrange("p h (a b) -> p h a b", b=16)
            P.op(V, lambda e: e.tensor_tensor(c4(cand), sv4[:, :, 0, :].unsqueeze(3).to_broadcast([128, 8, 16, 16]),
                                              sv4[:, :, 1, :].unsqueeze(2).to_broadcast([128, 8, 16, 16]), ALU.add),
                 reads=svk, writes=["cand"])
            P.op(V, lambda e: e.tensor_scalar(c4(cidx), sif4[:, :, 0, :].unsqueeze(3).to_broadcast([128, 8, 16, 16]), 128.0, None, ALU.mult),
                 reads=["sif"], writes=["cidx"])
            P.op(V, lambda e: e.tensor_tensor(c4(cidx), c4(cidx), sif4[:, :, 1, :].unsqueeze(2).to_broadcast([128, 8, 16, 16]), ALU.add),
                 reads=["sif", "cidx"], writes=["cidx"])
            for step in range(5):
                for h in range(8):
                    if step == 0:
                        P.op(V, lambda e, h=h: e.max(best[:, h, 0:8], cand[:, h, :]), reads=["cand"], writes=[("best", h)])
                    elif step == 1:
                        P.op(V, lambda e, h=h: e.max_index(pos[:, h, 0:8], best[:, h, 0:8], cand[:, h, :]),
                             reads=["cand", ("best", h)], writes=[("pos", h)])
                    elif step == 2:
                        P.op(V, lambda e, h=h: e.match_replace(candw[:, h, :], best[:, h, 0:8], cand[:, h, :], NEG),
                             reads=["cand", ("best", h)], writes=[("candw", h)])
                    elif step == 3:
                        P.op(V, lambda e, h=h: e.max(best[:, h, 8:16], candw[:, h, :]), reads=[("candw", h)], writes=[("best", h)])
                    else:
                        P.op(V, lambda e, h=h: e.max_index(pos[:, h, 8:16], best[:, h, 8:16], candw[:, h, :]),
                             reads=[("candw", h), ("best", h)], writes=[("pos", h)])
                    if h % 4 == 3:
                        yield
            bk = [("best", h) for h in range(8)]
            pk = [("pos", h) for h in range(8)]
            P.op(V, lambda e: e.tensor_copy(posf[:], pos[:]), reads=pk, writes=["posf"])
            for h in range(8):
                for hk in range(2):
                    bkey = ["eqb"]
                    e3 = eqb[:]
                    ks = slice(hk * 8, hk * 8 + 8)
                    P.op(V, lambda e, h=h, e3=e3, ks=ks: e.tensor_tensor(
                        e3, io256[:].unsqueeze(1).to_broadcast([128, 8, 256]),
                        posf[:, h, ks].unsqueeze(2).to_broadcast([128, 8, 256]), ALU.is_equal),
                        reads=["io256", "posf"], writes=bkey)
                    P.op(V, lambda e, h=h, e3=e3: e.tensor_tensor(
                        e3, e3, cidx[:, h, :].unsqueeze(1).to_broadcast([128, 8, 256]), ALU.mult),
                        reads=["cidx"], writes=bkey)
                    P.op(V, lambda e, h=h, e3=e3, hk=hk: e.tensor_reduce(eidx[:, h * 16 + hk * 8:h * 16 + hk * 8 + 8], e3, AX.X, ALU.add),
                         reads=bkey, writes=[("eidx", h)])
                    yield
            ek = [("eidx", h) for h in range(8)]
            P.op(V, lambda e, b=b: e.tensor_copy(eidx_is[b][:], eidx[:]), reads=ek, writes=[("eidx_i", b)])
            P.op(V, lambda e: e.tensor_tensor(ex[:], best[:], best[:, :, 0:1].to_broadcast([128, 8, 16]), ALU.subtract),
                 reads=bk, writes=["ex"])
            P.op("act", lambda e: e.activation(ex[:], ex[:], AF.Exp), reads=["ex"], writes=["ex"])
            P.op(V, lambda e: e.tensor_reduce(gs[:], ex[:], AX.X, ALU.add), reads=["ex"], writes=["gs"])
            P.op(V, lambda e: e.reciprocal(gs[:], gs[:]), reads=["gs"], writes=["gs"])
            P.op(V, lambda e, b=b: e.tensor_tensor(gates[b][:].rearrange("p (h k) -> p h k", k=16), ex[:],
                                                   gs[:].unsqueeze(2).to_broadcast([128, 8, 16]), ALU.mult),
                 reads=["ex", "gs"], writes=[("gate", b)])
            yield

        def gather(ti, nxt):
            b = ti % 2
            t0 = ti * 128
            GS = 4
            for k0 in range(0, 128, GS):
                gpar = (k0 // GS) % 2
                for k in range(k0, k0 + GS):
                    gbi = k % NB
                    P.op("pool", lambda e, k=k, gbi=gbi, b=b: e.indirect_dma_start(
                        out=gbv(gbi), out_offset=None, in_=uv2d,
                        in_offset=bass.IndirectOffsetOnAxis(ap=eidx_is[b][:, k:k + 1], axis=0),
                        bounds_check=bc_reg, oob_is_err=False),
                        reads=[("eidx_i", b)], writes=gbk(gbi), dma=True)
                    if k % 2 == 1:
                        pj = (k % NB) // 2
                        pi = (k // 2) % 2
                        P.op(V, lambda e, pj=pj, pi=pi, b=b: e.tensor_tensor(
                            prod[pi][:], pairs[pj][:, :, 0:D], xn2s[b][:].unsqueeze(1).to_broadcast([128, 2, D]), ALU.mult),
                            reads=[("gb", 2 * pj), ("gb", 2 * pj + 1), ("xn2", b)], writes=[("prod", pi)])
                        for s_ in range(2):
                            kk = k - 1 + s_
                            P.op("act", lambda e, kk=kk, pi=pi, s_=s_: e.activation(
                                junk[:], prod[pi][:, s_, :], AF.Copy, accum_out=dots[:, kk:kk + 1]),
                                reads=[("prod", pi)], writes=["junk", ("dots", k0)])
                dk = [("dots", k0)]
                sl = slice(k0, k0 + GS)
                gk_ = ("gw", gpar)
                gwb = gw[gpar]
                P.op("act", lambda e, sl=sl, gwb=gwb: e.activation(gwb[:, 0:GS], dots[:, sl], AF.Gelu_apprx_tanh), reads=dk, writes=[gk_])
                P.op(V, lambda e, sl=sl, gwb=gwb, b=b: e.tensor_tensor(wgt[:, sl], gwb[:, 0:GS], gates[b][:, sl], ALU.mult), reads=[gk_, ("gate", b)], writes=[("wgt", k0)])
                P.op(V, lambda e, sl=sl, gpar=gpar: e.tensor_tensor(
                    diag[gpar][:], C.ident_b[:].unsqueeze(1).to_broadcast([128, GS, 128]),
                    wgt[:, sl].unsqueeze(2).to_broadcast([128, GS, 128]), ALU.mult),
                    reads=["ident_b", ("wgt", k0)], writes=[("diag", gpar)])
                for k in range(k0, k0 + GS):
                    gbi = k % NB
                    for hf in range(2):
                        P.op("pe", lambda e, k=k, gpar=gpar, gbi=gbi, hf=hf, k0=k0: e.matmul(
                            pacc[hf][:], diag[gpar][:, k - k0, :], gbv(gbi)[:, D + hf * 512:D + (hf + 1) * 512],
                            start=(k == 0), stop=(k == 127)),
                            reads=[("diag", gpar), ("gb", gbi)], writes=[("pacc", hf)])
                if nxt is not None:
                    for _ in range(NYIELD):
                        next(nxt, None)
            for hf in range(2):
                P.op(V, lambda e, hf=hf, b=b: e.tensor_tensor(
                    yb[:, hf * 512:(hf + 1) * 512], pacc[hf][:], x1[b][:, hf * 512:(hf + 1) * 512], ALU.add),
                    reads=[("x1", b)], writes=[("pacc", hf), "yb"])
            rms(yb[:], 1, ["yb"])
            P.op(V, lambda e: e.scalar_tensor_tensor(yb[:], yb[:], ss[:, 1:2], gfin[:], ALU.mult, ALU.mult),
                 reads=["yb", ("ss", 1), "gfin"], writes=["yb"])
            P.dma("sp", T.y.ap()[t0:t0 + 128, :], yb[:], reads=["yb"], writes=[("y", ti)])

        tiles = list(DBG.get("p5_tiles", range(NTILE)))
        gens = {ti: route(ti) for ti in tiles}
        for _ in gens[tiles[0]]:
            pass
        for n_, ti in enumerate(tiles):
            nxt = gens[tiles[n_ + 1]] if n_ + 1 < len(tiles) else None
            gather(ti, nxt)
            if nxt is not None:
                for _ in nxt:
                    pass
        P.emit()
```

```python
from contextlib import ExitStack
import math
import numpy as np
import concourse.bass as bass
import concourse.mybir as mybir
from concourse.bass_utils import run_bass_kernel_spmd

F32 = mybir.dt.float32
BF16 = mybir.dt.bfloat16
I32 = mybir.dt.int32
U32 = mybir.dt.uint32
ALU = mybir.AluOpType
AF = mybir.ActivationFunctionType
AX = mybir.AxisListType

NCORES = 8
D = 1024
SEQ = 2048
NPS = 2
NSS = 16
DEC = 8
TOKP = NPS * SEQ
TOK = TOKP + 128
NTILE = TOK // 128
PROJ = 7680
OFF_U, OFF_Q, OFF_K, OFF_V, OFF_GA, OFF_GB = 0, 1024, 2560, 4096, 5632, 6656
WINS = (128, 512, 2048)
DILS = (1, 4, 16)
EPS = 1e-6
NEXP = 16384
DBG = {}


class Op:
    __slots__ = ("eng", "fn", "reads", "writes", "dma", "deps", "needs_inc", "sem", "val")

    def __init__(self, eng, fn, reads, writes, dma):
        self.eng = eng
        self.fn = fn
        self.reads = reads
        self.writes = writes
        self.dma = dma
        self.deps = ()
        self.needs_inc = False
        self.sem = None
        self.val = 0


class Sync:
    def __init__(self, nc, stack, dma_pool=None):
        self.nc = nc
        self.engs = {"pe": nc.tensor, "act": nc.scalar, "dve": nc.vector,
                     "pool": nc.gpsimd, "sp": nc.sync}
        dma_pool = dma_pool or {"sp": 24, "pool": 16, "act": 4}
        self.csem = {}
        self.ccount = {}
        for e in ("pe", "act", "dve", "pool"):
            self.csem[e] = stack.enter_context(nc.semaphore("cs_" + e))
            self.ccount[e] = 0
        self.pools = {}
        for q, n in dma_pool.items():
            self.pools[q] = {
                "sems": [stack.enter_context(nc.semaphore(f"ds_{q}_{i}")) for i in range(n)],
                "vals": [0] * n, "next": 0}
        self.waited = {e: {} for e in self.engs}
        self.n_inst = 0

    def wait(self, eng_name, sem, val):
        w = self.waited[eng_name]
        key = id(sem)
        if w.get(key, 0) >= val:
            return
        self.engs[eng_name].wait_ge(sem, val)
        w[key] = val

    def barrier(self, engines=("pe", "act", "dve", "pool", "sp")):
        for E in engines:
            for q, p in self.pools.items():
                for sem, v in zip(p["sems"], p["vals"]):
                    if v > 0:
                        self.wait(E, sem, v)
            for e in ("pe", "act", "dve", "pool"):
                if self.ccount[e] > 0 and e != E:
                    self.wait(E, self.csem[e], self.ccount[e])


class Prog:
    def __init__(self, sync):
        self.S = sync
        self.ops = []

    def op(self, eng, fn, reads=(), writes=(), dma=False):
        self.ops.append(Op(eng, fn, tuple(reads), tuple(writes), dma))

    def dma(self, eng, out, in_, reads=(), writes=(), **kw):
        self.op(eng, lambda e: e.dma_start(out=out, in_=in_, **kw), reads, writes, dma=True)

    def emit(self, barrier=True):
        S = self.S
        ops = self.ops
        last_w = {}
        readers = {}
        last_on_eng = {}
        for i, o in enumerate(ops):
            deps = set()
            for k in o.reads:
                if k in last_w:
                    deps.add(last_w[k])
            for k in o.writes:
                if k in last_w:
                    deps.add(last_w[k])
                deps.update(readers.get(k, ()))
            deps.discard(i)
            for k in o.reads:
                readers.setdefault(k, []).append(i)
            for k in o.writes:
                last_w[k] = i
                readers[k] = []
            o.deps = sorted(deps)
            for j in o.deps:
                ops[j].needs_inc = True
            if not o.dma:
                last_on_eng[o.eng] = i
        for i in last_on_eng.values():
            ops[i].needs_inc = True
        for o in ops:
            E = o.eng
            eng = S.engs[E]
            for j in o.deps:
                d = ops[j]
                if (not d.dma) and d.eng == "pe" and E == "pe" and not o.dma:
                    continue
                S.wait(E, d.sem, d.val)
            if o.dma:
                p = S.pools[E]
                k = p["next"]
                p["next"] = (k + 1) % len(p["sems"])
                sem = p["sems"][k]
                if p["vals"][k] > 0:
                    S.wait(E, sem, p["vals"][k])
                p["vals"][k] += 16
                ins = o.fn(eng)
                ins.then_inc(sem, 16)
                o.sem = sem
                o.val = p["vals"][k]
            else:
                ins = o.fn(eng)
                if o.needs_inc:
                    S.ccount[E] += 1
                    ins.then_inc(S.csem[E], 1)
                    o.sem = S.csem[E]
                    o.val = S.ccount[E]
            S.n_inst += 1
        if barrier:
            S.barrier()


class Ctx:
    pass


def ss(start, n, step):
    return slice(start, start + (n - 1) * step + 1, step)


def declare_io(nc):
    T = Ctx()
    di = lambda n, s, dt=F32: nc.dram_tensor(n, list(s), dt, kind="ExternalInput")
    do = lambda n, s, dt=F32: nc.dram_tensor(n, list(s), dt, kind="ExternalOutput")
    ds = lambda n, s, dt: nc.dram_tensor(n, list(s), dt, kind=("ExternalOutput" if n in DBG.get("expose", ()) else ("ExternalInput" if n in DBG.get("inject", ()) else "Internal")))
    T.x = di("x", [TOK, D])
    T.st_re = di("st_re", [NSS, 4096])
    T.st_im = di("st_im", [NSS, 4096])
    nss = 1 if DBG.get("small") else NSS
    nexp = 512 if DBG.get("small_tab") else NEXP
    T.c128 = di("c128", [nss, 128, 2, 512])
    T.c512 = di("c512", [nss, 512, 2, 512])
    T.c2048 = di("c2048", [nss, 2048, 2, 512])
    T.g_mix = di("g_mix", [D])
    T.w_in = di("w_in", [D, PROJ])
    T.lam_re = di("lam_re", [64, 64])
    T.lam_im = di("lam_im", [64, 64])
    T.log_dt = di("log_dt", [64])
    T.b_re = di("b_re", [4096, 16])
    T.b_im = di("b_im", [4096, 16])
    T.c_re = di("c_re", [1024, 64])
    T.c_im = di("c_im", [1024, 64])
    T.d_skip = di("d_skip", [1024])
    T.w_glu_a = di("w_glu_a", [D, D])
    T.w_glu_b = di("w_glu_b", [D, D])
    T.w_attn = di("w_attn", [512, D])
    T.w_out = di("w_out", [D, D])
    T.g_ffn = di("g_ffn", [D])
    T.w_qp = di("w_qp", [D, 2048])
    T.sub_keys = di("sub_keys", [16, 128, 128])
    T.u_tab = di("u_tab", [nexp, D])
    T.v_tab = di("v_tab", [nexp, D])
    T.g_final = di("g_final", [D])
    T.y = do("y", [TOK, D])
    T.ssm_p_re = do("ssm_p_re", [NPS, 4096])
    T.ssm_p_im = do("ssm_p_im", [NPS, 4096])
    T.ssm_s_re = do("ssm_s_re", [NSS, 4096])
    T.ssm_s_im = do("ssm_s_im", [NSS, 4096])
    T.kvp = [do(f"kvp{w}", [NPS, w, 1024]) for w in WINS]
    T.kvs = [do(f"kvs{w}", [128, 1024]) for w in WINS]
    T.uT_s = ds("uT_s", [1024, TOK], BF16)
    T.qT_s = ds("qT_s", [1536, TOK], BF16)
    T.kT_s = ds("kT_s", [1536, TOK], BF16)
    T.sga_s = ds("sga_s", [1024, TOK], F32)
    T.sgb_s = ds("sgb_s", [1024, TOK], F32)
    T.vtok_s = ds("vtok_s", [TOK, 1536], BF16)
    T.hsT_s = ds("hsT_s", [1024, TOK], BF16)
    T.x1_s = ds("x1_s", [TOK, D], F32)
    T.uv_s = ds("uv_s", [nexp, 2, D], BF16)
    T.U_s = [ds(f"U_s{g}", [TOK, 512], F32) for g in range(3)]
    T.md_s = [ds(f"md_s{g}", [TOK, 16], F32) for g in range(3)]
    return T


def phase_consts(nc, S, T, C, st):
    sb = lambda n, shape, dt: st.enter_context(nc.sbuf_tensor(n, shape, dt))
    P = Prog(S)
    C.ident_b = sb("ident_b", [128, 128], BF16)
    C.ident_f = sb("ident_f", [128, 128], F32)
    C.maskadd = sb("maskadd", [128, 256], BF16)
    iot = sb("c_iot", [128, 256], I32)
    t1 = sb("c_t1", [128, 256], F32)
    t2 = sb("c_t2", [128, 256], F32)
    P.op("pool", lambda e: e.iota(iot[:], [[1, 256]], base=0, channel_multiplier=-1), writes=["iot"])
    P.op("dve", lambda e: e.tensor_scalar(C.ident_b[:], iot[:, 0:128], 0.0, None, ALU.is_equal),
         reads=["iot"], writes=["ident_b"])
    P.op("dve", lambda e: e.tensor_scalar(C.ident_f[:], iot[:, 0:128], 0.0, None, ALU.is_equal),
         reads=["iot"], writes=["ident_f"])
    P.op("dve", lambda e: e.tensor_scalar(t1[:], iot[:], 0.0, None, ALU.is_ge), reads=["iot"], writes=["t1"])
    P.op("dve", lambda e: e.tensor_scalar(t2[:], iot[:], 128.0, None, ALU.is_le), reads=["iot"], writes=["t2"])
    P.op("dve", lambda e: e.tensor_tensor(t1[:], t1[:], t2[:], ALU.mult), reads=["t1", "t2"], writes=["t1"])
    P.op("dve", lambda e: e.tensor_scalar(C.maskadd[:], t1[:], -1.0, 30000.0, ALU.add, ALU.mult),
         reads=["t1"], writes=["maskadd"])
    P.emit()


def phase1(nc, S, T, C):
    with ExitStack() as st:
        sb = lambda n, shape, dt: st.enter_context(nc.sbuf_tensor(n, shape, dt))
        ps = lambda n, shape, dt: st.enter_context(nc.psum_tensor(n, shape, dt))
        P = Prog(S)
        win_b = sb("win_b", [128, 8, PROJ], BF16)
        wst = [sb(f"wst{i}", [128, 8, 256], F32) for i in range(2)]
        gmix = sb("gmix", [128, 8], F32)
        xs = [sb(f"xs{i}", [128, D], F32) for i in range(2)]
        junk = sb("junk", [128, D], BF16)
        ss = [sb(f"ss{i}", [128, 1], F32) for i in range(2)]
        rstd = [sb(f"rstd{i}", [128, 1], F32) for i in range(2)]
        xnb = [sb(f"xnb{i}", [128, D], BF16) for i in range(2)]
        xnT = [sb(f"xnT{i}", [128, 8, 512], BF16) for i in range(2)]
        kvst = [sb(f"kvst{i}", [128, 1024], F32) for i in range(3)]
        vb = [sb(f"vb{i}", [128, 512], BF16) for i in range(3)]
        fsb = [sb(f"fsb{i}", [128, 512], BF16) for i in range(4)]
        fsf = [sb(f"fsf{i}", [128, 512], F32) for i in range(3)]
        pT = [ps(f"pT{i}", [128, 8, 128], BF16) for i in range(2)]
        pM = [ps(f"pM{i}", [128, 512], F32) for i in range(5)]

        x = T.x.ap()
        P.dma("sp", gmix[:], T.g_mix.ap().rearrange("(k p) -> p k", p=128), writes=["gmix"],
              allow_slow_non_contiguous=True)
        w_in = T.w_in.ap().rearrange("(k p) f -> p k f", p=128)
        for c in range(PROJ // 256):
            b = c % 2
            P.dma("sp", wst[b][:], w_in[:, :, c * 256:(c + 1) * 256], writes=[("wst", b)])
            for k in range(8):
                eng = "dve" if k % 2 == 0 else "pool"
                P.op(eng, lambda e, b=b, k=k, c=c: e.tensor_scalar(
                    win_b[:, k, c * 256:(c + 1) * 256], wst[b][:, k, :], gmix[:, k:k + 1], None, ALU.mult),
                    reads=[("wst", b), "gmix"], writes=[("win", c)])
        wkeys = [("win", c) for c in range(PROJ // 256)]

        cnt = {"pm": 0, "kv": 0, "fsb": 0, "fsf": 0, "ev": 0}

        def next_pm():
            i = cnt["pm"] % len(pM)
            cnt["pm"] += 1
            return i

        nchunks = 9
        for ch in DBG.get("chunks", range(nchunks)):
            ntl = 4 if ch < 8 else 1
            cb = ch % 2
            ntok = ntl * 128
            for j in range(ntl):
                ti = ch * 4 + j
                b = ti % 2
                t0 = ti * 128
                P.dma("sp", xs[b][:], x[t0:t0 + 128, :], writes=[("xs", b)])
                P.op("act", lambda e, b=b: e.activation(junk[:], xs[b][:], AF.Square, accum_out=ss[b][:]),
                     reads=[("xs", b)], writes=["junk", ("ss", b)])
                P.op("dve", lambda e, b=b: e.tensor_scalar(rstd[b][:], ss[b][:], 1.0 / D, EPS, ALU.mult, ALU.add),
                     reads=[("ss", b)], writes=[("rstd", b)])
                P.op("act", lambda e, b=b: e.activation(rstd[b][:], rstd[b][:], AF.Sqrt),
                     reads=[("rstd", b)], writes=[("rstd", b)])
                P.op("dve", lambda e, b=b: e.reciprocal(rstd[b][:], rstd[b][:]),
                     reads=[("rstd", b)], writes=[("rstd", b)])
                P.op("dve", lambda e, b=b: e.tensor_scalar(xnb[b][:], xs[b][:], rstd[b][:], None, ALU.mult),
                     reads=[("xs", b), ("rstd", b)], writes=[("xnb", b)])
                for k in range(8):
                    P.op("pe", lambda e, b=b, k=k: e.transpose(pT[b][:, k, :], xnb[b][:, k * 128:(k + 1) * 128],
                                                               C.ident_b[:]),
                         reads=[("xnb", b), "ident_b"], writes=[("pT", b)])
                P.op("dve", lambda e, b=b, cb=cb, j=j: e.tensor_copy(xnT[cb][:, :, j * 128:(j + 1) * 128], pT[b][:]),
                     writes=[("pT", b), ("xnT", cb, j)])
                if ti < 32:
                    seq = ti // 16
                    tin = (ti % 16) * 128
                else:
                    seq, tin = None, 0
                for g in range(3):
                    need_k = True
                    if seq is not None and tin < SEQ - WINS[g]:
                        need_k = False
                    kb = cnt["kv"] % 3
                    cnt["kv"] += 1
                    for part, off in ((0, OFF_K + 512 * g), (1, OFF_V + 512 * g)):
                        if part == 0 and not need_k:
                            continue
                        pi = next_pm()
                        for k in range(8):
                            P.op("pe", lambda e, pi=pi, k=k, cb=cb, j=j, off=off: e.matmul(
                                pM[pi][:], xnT[cb][:, k, j * 128:(j + 1) * 128], win_b[:, k, off:off + 512],
                                start=(k == 0), stop=(k == 7)),
                                reads=[("xnT", cb, j)] + wkeys[off // 256: off // 256 + 2], writes=[("pM", pi)])
                        P.op("act", lambda e, pi=pi, kb=kb, part=part: e.activation(
                            kvst[kb][:, part * 512:(part + 1) * 512], pM[pi][:], AF.Copy),
                            writes=[("pM", pi), ("kvst", kb, part)])
                        if part == 1:
                            P.op("pool", lambda e, kb=kb: e.tensor_copy(vb[kb][:], kvst[kb][:, 512:1024]),
                                 reads=[("kvst", kb, 1)], writes=[("vb", kb)])
                    P.dma("sp", T.vtok_s.ap()[t0:t0 + 128, g * 512:(g + 1) * 512], vb[kb][:],
                          reads=[("vb", kb)], writes=[("vtok_s", ti, g)])
                    if need_k:
                        if seq is None:
                            dst = T.kvs[g].ap()[:, :]
                        else:
                            r0 = tin - (SEQ - WINS[g])
                            dst = T.kvp[g].ap()[seq, r0:r0 + 128, :]
                        P.dma("sp", dst, kvst[kb][:], reads=[("kvst", kb, 0), ("kvst", kb, 1)],
                              writes=[("kvout", ti, g)])
            tok0 = ch * 512
            xkeys = [("xnT", cb, j) for j in range(ntl)]
            jobs = []
            for f in range(8):
                jobs.append(("u", OFF_U + 128 * f, T.uT_s, f))
            for f in range(12):
                jobs.append(("q", OFF_Q + 128 * f, T.qT_s, f))
            for f in range(12):
                jobs.append(("k", OFF_K + 128 * f, T.kT_s, f))
            for f in range(8):
                jobs.append(("ga", OFF_GA + 128 * f, T.sga_s, f))
            for f in range(8):
                jobs.append(("gb", OFF_GB + 128 * f, T.sgb_s, f))
            for kind, off, dst_t, f in jobs:
                pi = next_pm()
                for k in range(8):
                    P.op("pe", lambda e, pi=pi, k=k, cb=cb, off=off, ntok=ntok: e.matmul(
                        pM[pi][:, 0:ntok], win_b[:, k, off:off + 128], xnT[cb][:, k, 0:ntok],
                        start=(k == 0), stop=(k == 7)),
                        reads=xkeys + [wkeys[off // 256]], writes=[("pM", pi)])
                dst = dst_t.ap()[f * 128:(f + 1) * 128, tok0:tok0 + ntok]
                if kind in ("ga", "gb"):
                    bi = cnt["fsf"] % len(fsf)
                    cnt["fsf"] += 1
                    P.op("act", lambda e, pi=pi, bi=bi, ntok=ntok: e.activation(
                        fsf[bi][:, 0:ntok], pM[pi][:, 0:ntok], AF.Sigmoid),
                        writes=[("pM", pi), ("fsf", bi)])
                    P.dma("sp", dst, fsf[bi][:, 0:ntok], reads=[("fsf", bi)], writes=[(kind, f, ch)])
                else:
                    bi = cnt["fsb"] % len(fsb)
                    cnt["fsb"] += 1
                    eng = "dve" if cnt["ev"] % 2 == 0 else "act"
                    cnt["ev"] += 1
                    qs = 0.125 if kind == "q" else 1.0
                    if eng == "dve":
                        P.op("dve", lambda e, pi=pi, bi=bi, ntok=ntok, qs=qs: e.tensor_scalar(
                            fsb[bi][:, 0:ntok], pM[pi][:, 0:ntok], qs, None, ALU.mult),
                            writes=[("pM", pi), ("fsb", bi)])
                    else:
                        P.op("act", lambda e, pi=pi, bi=bi, ntok=ntok, qs=qs: e.activation(
                            fsb[bi][:, 0:ntok], pM[pi][:, 0:ntok], AF.Copy, scale=qs),
                            writes=[("pM", pi), ("fsb", bi)])
                    P.dma("sp", dst, fsb[bi][:, 0:ntok], reads=[("fsb", bi)], writes=[(kind, f, ch)])
        P.emit()


def build_program(upto=99):
    nc = bass.Bass("TRN2", target_bir_lowering=False)
    T = declare_io(nc)
    C = Ctx()
    with ExitStack() as gst:
        S = Sync(nc, gst)
        phase_consts(nc, S, T, C, gst)
        only = DBG.get("only")
        run = lambda i: (upto >= i) if only is None else (i in only)
        if run(1):
            phase1(nc, S, T, C)
        if run(2) and not DBG.get("skip2"):
            phase2(nc, S, T, C)
        if run(3):
            phase3(nc, S, T, C)
        if run(4):
            phase4(nc, S, T, C)
        if run(5):
            phase5a(nc, S, T, C)
            phase5(nc, S, T, C)
        print("instructions:", S.n_inst, "counts:", S.ccount)
    return nc


def make_in_maps(inputs):
    f = lambda a: np.ascontiguousarray(np.asarray(a, dtype=np.float32))
    xp = f(inputs["x_prompt"])
    xsm = f(inputs["x_sample"])
    shared = {
        "g_mix": f(inputs["g_mix"]).reshape(D),
        "w_in": f(inputs["w_in"]).reshape(D, PROJ),
        "lam_re": f(inputs["lam_re"]).reshape(64, 64),
        "lam_im": f(inputs["lam_im"]).reshape(64, 64),
        "log_dt": f(inputs["log_dt"]).reshape(64),
        "b_re": f(inputs["b_re"]).reshape(4096, 16),
        "b_im": f(inputs["b_im"]).reshape(4096, 16),
        "c_re": f(inputs["c_re"]).reshape(1024, 64),
        "c_im": f(inputs["c_im"]).reshape(1024, 64),
        "d_skip": f(inputs["d_skip"]).reshape(1024),
        "w_glu_a": f(inputs["w_glu_a"]).reshape(D, D),
        "w_glu_b": f(inputs["w_glu_b"]).reshape(D, D),
        "w_attn": f(inputs["w_attn_proj"]).reshape(512, D),
        "w_out": f(inputs["w_out"]).reshape(D, D),
        "g_ffn": f(inputs["g_ffn"]).reshape(D),
        "w_qp": f(inputs["w_qp"]).reshape(D, 2048),
        "sub_keys": f(inputs["sub_keys"]).reshape(16, 128, 128),
        "u_tab": f(inputs["u_tab"]).reshape(NEXP, D),
        "v_tab": f(inputs["v_tab"]).reshape(NEXP, D),
        "g_final": f(inputs["g_final"]).reshape(D),
    }
    st_re = f(inputs["state_ssm_re"]).reshape(128, 4096)
    st_im = f(inputs["state_ssm_im"]).reshape(128, 4096)
    c128 = f(inputs["cache_kv_w128"]).reshape(128, 128, 2, 512)
    c512 = f(inputs["cache_kv_w512"]).reshape(128, 512, 2, 512)
    c2048 = f(inputs["cache_kv_w2048"]).reshape(128, 2048, 2, 512)
    maps = []
    for c in range(NCORES):
        m = dict(shared)
        m["x"] = np.concatenate([xp[NPS * c:NPS * (c + 1)].reshape(TOKP, D),
                                 xsm[NSS * c:NSS * (c + 1)].reshape(128, D)], axis=0)
        sl = slice(NSS * c, NSS * (c + 1))
        m["st_re"] = st_re[sl]
        m["st_im"] = st_im[sl]
        m["c128"] = c128[sl]
        m["c512"] = c512[sl]
        m["c2048"] = c2048[sl]
        maps.append(m)
    return maps


def gather_outputs(results):
    cat = lambda name: np.concatenate([np.asarray(r[name]) for r in results], axis=0)
    y = np.stack([np.asarray(r["y"]) for r in results], axis=0)
    y_prompt = y[:, :TOKP].reshape(16, SEQ, D)
    y_sample = y[:, TOKP:].reshape(128, DEC, D)
    outs = [y_prompt, y_sample,
            cat("ssm_p_re").reshape(1, 16, 64, 64), cat("ssm_p_im").reshape(1, 16, 64, 64)]
    for w in WINS:
        outs.append(cat(f"kvp{w}").reshape(1, 16, w, 2, 8, 64))
    outs.append(cat("ssm_s_re").reshape(1, 128, 64, 64))
    outs.append(cat("ssm_s_im").reshape(1, 128, 64, 64))
    for w in WINS:
        outs.append(cat(f"kvs{w}").reshape(1, 128, DEC, 2, 8, 64))
    return tuple(np.ascontiguousarray(o, dtype=np.float32) for o in outs)


def kernel(**inputs):
    nc = build_program()
    maps = make_in_maps(inputs)
    res = run_bass_kernel_spmd(nc, maps, core_ids=list(range(NCORES)))
    return gather_outputs(res.results)


TWO_PI = 2.0 * math.pi


def phase2(nc, S, T, C):
    with ExitStack() as st:
        sb = lambda n, shape, dt: st.enter_context(nc.sbuf_tensor(n, shape, dt))
        ps = lambda n, shape, dt: st.enter_context(nc.psum_tensor(n, shape, dt))
        P = Prog(S)
        V = "dve"

        def small(name):
            return sb("p2_" + name, [128, 32], F32)

        lr, li, ldt, dtt, rmag, ang = (small(n) for n in ("lr", "li", "ldt", "dt", "rmag", "ang"))
        a1, kq, red, m1 = (small(n) for n in ("a1", "kq", "red", "m1"))
        kqi = sb("p2_kqi", [128, 32], I32)
        sin_t, cos_t, abre, abim, am1 = (small(n) for n in ("sin", "cos", "abre", "abim", "am1"))
        den, fre, fim, tq = (small(n) for n in ("den", "fre", "fim", "tq"))
        Wre = sb("p2_Wre", [128, 11, 32], F32)
        Wim = sb("p2_Wim", [128, 11, 32], F32)
        bre = sb("p2_bre", [128, 32, 16], F32)
        bim = sb("p2_bim", [128, 32, 16], F32)
        bbre = sb("p2_bbre", [128, 32, 16], F32)
        bbim = sb("p2_bbim", [128, 32, 16], F32)
        btmp = sb("p2_btmp", [128, 32, 16], F32)
        bbpad = [[sb(f"p2_bbpad{c}{j}", [128, 128], BF16) for j in range(4)] for c in range(2)]
        Cn = [sb(f"p2_Cn{c}", [128, 8, 64], F32) for c in range(2)]
        CT = [sb(f"p2_CT{c}", [128, 128], BF16) for c in range(2)]
        xl = [sb(f"p2_xl{c}", [128, 128], BF16) for c in range(2)]
        yl = [[sb(f"p2_yl{c}{j}", [128, 128], BF16) for j in range(4)] for c in range(2)]
        bmi = sb("p2_bmi", [128, 8], I32)
        bm = sb("p2_bm", [128, 8], F32)
        bm2 = sb("p2_bm2", [128, 8], F32)
        nbm = sb("p2_nbm", [128, 8], F32)
        dsk = sb("p2_dsk", [128, 8], F32)
        h0st = [sb(f"p2_h0st{i}", [16, 1024], F32) for i in range(2)]
        h0T = [sb(f"p2_h0T{c}", [128, 32, 16], F32) for c in range(2)]
        Dt = [sb(f"p2_D{c}", [128, 2048], F32) for c in range(2)]
        dtmp = [sb(f"p2_dtmp{i}", [128, 1024], F32) for i in range(2)]
        uT = [sb(f"p2_uT{i}", [128, TOK], BF16) for i in range(2)]
        xt = [sb(f"p2_xt{c}", [128, 2048], F32) for c in range(2)]
        gg = [sb(f"p2_g{c}", [128, 2048], F32) for c in range(2)]
        tmp = [sb(f"p2_tmp{i}", [128, 512], F32) for i in range(2)]
        hbuf = sb("p2_hbuf", [128, 4, 2, TOK], BF16)
        hsT = sb("p2_hsT", [128, TOK], BF16)
        ysb = [sb(f"p2_ysb{i}", [128, 512], F32) for i in range(2)]
        yw = [sb(f"p2_yw{i}", [128, 512], F32) for i in range(2)]
        pat = sb("p2_pat", [128, 16, 8], F32)
        decs = sb("p2_decs", [128, 128], F32)
        hfp = [sb(f"p2_hfp{c}", [128, 2, 32], F32) for c in range(2)]
        hfs = [sb(f"p2_hfs{c}", [128, 16, 32], F32) for c in range(2)]
        hfo = sb("p2_hfo", [128, 128], F32)

        xps = [[ps(f"p2_xps{i}{c}", [128, 512], F32) for c in range(2)] for i in range(2)]
        tps = ps("p2_tps", [128, 128], BF16)
        yps = [ps(f"p2_yps{i}", [128, 512], F32) for i in range(2)]
        mps = ps("p2_mps", [128, 32, 16], F32)

        P.dma("sp", lr[:], T.lam_re.ap().rearrange("(k a) n -> (a n) k", a=2), writes=["lr"],
              allow_slow_non_contiguous=True)
        P.dma("sp", li[:], T.lam_im.ap().rearrange("(k a) n -> (a n) k", a=2), writes=["li"],
              allow_slow_non_contiguous=True)
        for a in range(2):
            P.dma("sp", ldt[a * 64:(a + 1) * 64, :], bass.AP(T.log_dt, a, [[0, 64], [2, 32]]), writes=["ldt"],
                  allow_slow_non_contiguous=True)
        P.dma("sp", bre[:], T.b_re.ap().rearrange("(k p) c -> p k c", p=128), writes=["bre"])
        P.dma("sp", bim[:], T.b_im.ap().rearrange("(k p) c -> p k c", p=128), writes=["bim"])
        P.dma("sp", Cn[0][:], T.c_re.ap().rearrange("(f r) n -> r f n", r=128), writes=["Cn0"])
        P.dma("sp", Cn[1][:], T.c_im.ap().rearrange("(f r) n -> r f n", r=128), writes=["Cn1"])
        P.dma("sp", dsk[:], T.d_skip.ap().rearrange("(f r) -> r f", r=128), writes=["dsk"],
              allow_slow_non_contiguous=True)

        def tt(out, a, b, op, reads, writes, eng=V):
            P.op(eng, lambda e: e.tensor_tensor(out, a, b, op), reads=reads, writes=writes)

        def ts(out, a, s1, s2, op0, op1, reads, writes, eng=V):
            if op1 is None:
                P.op(eng, lambda e: e.tensor_scalar(out, a, s1, None, op0), reads=reads, writes=writes)
            else:
                P.op(eng, lambda e: e.tensor_scalar(out, a, s1, s2, op0, op1), reads=reads, writes=writes)

        def stt(out, a, sc, b, op0, op1, reads, writes, eng=V):
            P.op(eng, lambda e: e.scalar_tensor_tensor(out, a, sc, b, op0, op1), reads=reads, writes=writes)

        def act(out, a, func, reads, writes, **kw):
            P.op("act", lambda e: e.activation(out, a, func, **kw), reads=reads, writes=writes)

        act(dtt[:], ldt[:], AF.Exp, ["ldt"], ["dt"])
        tt(a1[:], lr[:], dtt[:], ALU.mult, ["lr", "dt"], ["a1"])
        act(rmag[:], a1[:], AF.Exp, ["a1"], ["rmag"])
        tt(ang[:], li[:], dtt[:], ALU.mult, ["li", "dt"], ["ang"])
        for off, dst, nm in ((0.0, sin_t, "sin"), (math.pi / 2, cos_t, "cos")):
            ts(a1[:], ang[:], off, None, ALU.add, None, ["ang"], ["a1"])
            ts(kq[:], a1[:], 1.0 / TWO_PI, None, ALU.mult, None, ["a1"], ["kq"])
            P.op(V, lambda e: e.tensor_copy(kqi[:], kq[:]), reads=["kq"], writes=["kqi"])
            P.op(V, lambda e: e.tensor_copy(kq[:], kqi[:]), reads=["kqi"], writes=["kq"])
            stt(red[:], kq[:], -TWO_PI, a1[:], ALU.mult, ALU.add, ["kq", "a1"], ["red"])
            ts(m1[:], red[:], math.pi, None, ALU.is_gt, None, ["red"], ["m1"])
            stt(red[:], m1[:], -TWO_PI, red[:], ALU.mult, ALU.add, ["m1", "red"], ["red"])
            ts(m1[:], red[:], -math.pi, None, ALU.is_lt, None, ["red"], ["m1"])
            stt(red[:], m1[:], TWO_PI, red[:], ALU.mult, ALU.add, ["m1", "red"], ["red"])
            ts(red[:], red[:], -math.pi, math.pi, ALU.max, ALU.min, ["red"], ["red"])
            act(dst[:], red[:], AF.Sin, ["red"], [nm])
        tt(abre[:], rmag[:], cos_t[:], ALU.mult, ["rmag", "cos"], ["abre"])
        tt(abim[:], rmag[:], sin_t[:], ALU.mult, ["rmag", "sin"], ["abim"])
        P.op(V, lambda e: e.tensor_copy(Wre[:, 0, :], cos_t[:]), reads=["cos"], writes=["W"])
        P.op(V, lambda e: e.tensor_copy(Wim[:, 0, :], sin_t[:]), reads=["sin"], writes=["W"])
        for L in range(10):
            tt(a1[:], Wre[:, L, :], Wre[:, L, :], ALU.mult, ["W"], ["a1"])
            tt(kq[:], Wim[:, L, :], Wim[:, L, :], ALU.mult, ["W"], ["kq"])
            tt(red[:], Wre[:, L, :], Wim[:, L, :], ALU.mult, ["W"], ["red"])
            tt(Wre[:, L + 1, :], a1[:], kq[:], ALU.subtract, ["a1", "kq"], ["W"])
            ts(Wim[:, L + 1, :], red[:], 2.0, None, ALU.mult, None, ["red"], ["W"])
        tt(den[:], lr[:], lr[:], ALU.mult, ["lr"], ["den"])
        tt(tq[:], li[:], li[:], ALU.mult, ["li"], ["tq"])
        tt(den[:], den[:], tq[:], ALU.add, ["den", "tq"], ["den"])
        P.op(V, lambda e: e.reciprocal(den[:], den[:]), reads=["den"], writes=["den"])
        ts(am1[:], abre[:], -1.0, None, ALU.add, None, ["abre"], ["am1"])
        tt(fre[:], am1[:], lr[:], ALU.mult, ["am1", "lr"], ["fre"])
        tt(tq[:], abim[:], li[:], ALU.mult, ["abim", "li"], ["tq"])
        tt(fre[:], fre[:], tq[:], ALU.add, ["fre", "tq"], ["fre"])
        tt(fre[:], fre[:], den[:], ALU.mult, ["fre", "den"], ["fre"])
        tt(fim[:], abim[:], lr[:], ALU.mult, ["abim", "lr"], ["fim"])
        tt(tq[:], am1[:], li[:], ALU.mult, ["am1", "li"], ["tq"])
        tt(fim[:], fim[:], tq[:], ALU.subtract, ["fim", "tq"], ["fim"])
        tt(fim[:], fim[:], den[:], ALU.mult, ["fim", "den"], ["fim"])
        fre_b = fre[:].unsqueeze(2).to_broadcast([128, 32, 16])
        fim_b = fim[:].unsqueeze(2).to_broadcast([128, 32, 16])
        tt(bbre[:], bre[:], fre_b, ALU.mult, ["bre", "fre"], ["bbre"])
        tt(btmp[:], bim[:], fim_b, ALU.mult, ["bim", "fim"], ["btmp"])
        tt(bbre[:], bbre[:], btmp[:], ALU.subtract, ["bbre", "btmp"], ["bbre"])
        tt(bbim[:], bim[:], fre_b, ALU.mult, ["bim", "fre"], ["bbim"])
        tt(btmp[:], bre[:], fim_b, ALU.mult, ["bre", "fim"], ["btmp"])
        tt(bbim[:], bbim[:], btmp[:], ALU.add, ["bbim", "btmp"], ["bbim"])
        P.op("pool", lambda e: e.iota(bmi[:], [[-16, 8]], base=0, channel_multiplier=1), writes=["bmi"])
        ts(bm[:], bmi[:], 0.0, None, ALU.is_ge, None, ["bmi"], ["bm"])
        ts(bm2[:], bmi[:], 15.0, None, ALU.is_le, None, ["bmi"], ["bm2"])
        tt(bm[:], bm[:], bm2[:], ALU.mult, ["bm", "bm2"], ["bm"])
        ts(nbm[:], bm[:], -1.0, None, ALU.mult, None, ["bm"], ["nbm"])
        for c in range(2):
            for j in range(4):
                P.op("pool", lambda e, c=c, j=j: e.memset(bbpad[c][j][:], 0.0), writes=[("bbpad", c, j)])
        P.op("pool", lambda e: e.memset(pat[:], 1.0), writes=["pat"])
        P.op("pool", lambda e: e.memset(pat[:, :, 0:1], 0.0), writes=["pat"])
        for c, src in ((0, T.st_re), (1, T.st_im)):
            for pc in range(4):
                hb = (c * 4 + pc) % 2
                P.dma("sp", h0st[hb][:], src.ap()[:, pc * 1024:(pc + 1) * 1024], writes=[("h0st", hb)])
                for kk in range(8):
                    k = pc * 8 + kk
                    P.op("pe", lambda e, hb=hb, kk=kk, k=k: e.transpose(
                        mps[:, k, :], h0st[hb][:, kk * 128:(kk + 1) * 128], C.ident_f[0:16, 0:16]),
                        reads=[("h0st", hb), "ident_f"], writes=["mps"])
            P.op("act", lambda e, c=c: e.activation(h0T[c][:], mps[:], AF.Copy), writes=["mps", ("h0T", c)])

        segs = [(0, SEQ), (SEQ, SEQ), (TOKP, 128)]
        cnt = {"x": 0, "y": 0, "ysb": 0}
        for k in range(32):
            F, j = divmod(k, 4)
            r0 = j * 32
            if j == 0:
                ub = F % 2
                P.dma("sp", uT[ub][:], T.uT_s.ap()[F * 128:(F + 1) * 128, :], reads=[("u", F, ch) for ch in range(9)],
                      writes=[("uT", ub)])
            for c, bb in ((0, bbre), (1, bbim)):
                for a in range(2):
                    P.op(V, lambda e, c=c, a=a, bb=bb, k=k, j=j, r0=r0: e.tensor_copy(
                        bbpad[c][j][a * 64:(a + 1) * 64, r0 + 16 * a:r0 + 16 * a + 16], bb[a * 64:(a + 1) * 64, k, :]),
                        reads=["bbre" if c == 0 else "bbim"], writes=[("bbpad", c, j)])
                P.op("pe", lambda e, c=c, j=j: e.transpose(tps[:], bbpad[c][j][:], C.ident_b[:]),
                     reads=[("bbpad", c, j), "ident_b"], writes=["tps"])
                P.op("act", lambda e, c=c: e.activation(xl[c][:], tps[:], AF.Copy), writes=["tps", ("xl", c)])
            for c in range(2):
                msk = bm if c == 0 else nbm
                for a in range(2):
                    P.op(V, lambda e, c=c, a=a, F=F, j=j, msk=msk: e.tensor_scalar(
                        CT[c][:, a * 64:(a + 1) * 64], Cn[c][:, F, :], msk[:, 2 * j + a:2 * j + a + 1], None, ALU.mult),
                        reads=[f"Cn{c}", "bm", "nbm"], writes=[("CT", c)])
                P.op("pe", lambda e, c=c: e.transpose(tps[:], CT[c][:], C.ident_b[:]),
                     reads=[("CT", c), "ident_b"], writes=["tps"])
                P.op("act", lambda e, c=c, j=j: e.activation(yl[c][j][:], tps[:], AF.Copy),
                     writes=["tps", ("yl", c, j)])
            P.op(V, lambda e, k=k: e.tensor_copy(Dt[0][:, 0:1], Wre[:, 0, k:k + 1]), reads=["W"], writes=["D"])
            P.op(V, lambda e, k=k: e.tensor_copy(Dt[1][:, 0:1], Wim[:, 0, k:k + 1]), reads=["W"], writes=["D"])
            for L in range(11):
                n = 1 << L
                wr = Wre[:, L, k:k + 1]
                wi = Wim[:, L, k:k + 1]
                ts(dtmp[0][:, 0:n], Dt[1][:, 0:n], wi, None, ALU.mult, None, ["D", "W"], ["dtmp0"])
                ts(dtmp[1][:, 0:n], Dt[1][:, 0:n], wr, None, ALU.mult, None, ["D", "W"], ["dtmp1"])
                stt(Dt[0][:, n:2 * n], Dt[0][:, 0:n], wr, dtmp[0][:, 0:n], ALU.mult, ALU.subtract,
                    ["D", "W", "dtmp0"], ["D"])
                stt(Dt[1][:, n:2 * n], Dt[0][:, 0:n], wi, dtmp[1][:, 0:n], ALU.mult, ALU.add,
                    ["D", "W", "dtmp1"], ["D"])
            ts(decs[:], pat[:].rearrange("p b t -> p (b t)"), rmag[:, k:k + 1], None, ALU.mult, None,
               ["pat", "rmag"], ["decs"])
            ub = F % 2
            for si, (t0, ln) in enumerate(segs):
                nch = max(1, ln // 512)
                cw = min(ln, 512)
                for ch in range(nch):
                    xi = cnt["x"] % 2
                    cnt["x"] += 1
                    for c in range(2):
                        P.op("pe", lambda e, xi=xi, c=c, ub=ub, t0=t0, ch=ch, cw=cw: e.matmul(
                            xps[xi][c][:, 0:cw], xl[c][:], uT[ub][:, t0 + ch * 512:t0 + ch * 512 + cw],
                            start=True, stop=True),
                            reads=[("xl", c), ("uT", ub)], writes=[("xps", xi, c)])
                    sl = slice(ch * 512, ch * 512 + cw)
                    if si < 2:
                        dre, dim = Dt[0][:, sl], Dt[1][:, sl]
                        xr, xim = xps[xi][0][:, 0:cw], xps[xi][1][:, 0:cw]
                        o_re, o_im = xt[0][:, sl], xt[1][:, sl]
                        t_a, t_b = tmp[0][:, 0:cw], tmp[1][:, 0:cw]
                    else:
                        v3 = lambda ap: ap.rearrange("p (b t) -> p b t", t=8)
                        dre = Dt[0][:, 0:8].unsqueeze(1).to_broadcast([128, 16, 8])
                        dim = Dt[1][:, 0:8].unsqueeze(1).to_broadcast([128, 16, 8])
                        xr, xim = v3(xps[xi][0][:, 0:128]), v3(xps[xi][1][:, 0:128])
                        o_re, o_im = v3(xt[0][:, 0:128]), v3(xt[1][:, 0:128])
                        t_a, t_b = v3(tmp[0][:, 0:128]), v3(tmp[1][:, 0:128])
                    kx = [("xps", xi, 0), ("xps", xi, 1)]
                    P.op(V, lambda e, t_a=t_a, dim=dim, xim=xim: e.tensor_tensor(t_a, dim, xim, ALU.mult),
                         reads=["D"], writes=["tmp0"] + kx)
                    P.op(V, lambda e, o_re=o_re, dre=dre, xr=xr: e.tensor_tensor(o_re, dre, xr, ALU.mult),
                         reads=["D"], writes=["xt0"] + kx)
                    P.op(V, lambda e, o_re=o_re, t_a=t_a: e.tensor_tensor(o_re, o_re, t_a, ALU.add),
                         reads=["tmp0"], writes=["xt0"])
                    P.op(V, lambda e, t_b=t_b, dim=dim, xr=xr: e.tensor_tensor(t_b, dim, xr, ALU.mult),
                         reads=["D"], writes=["tmp1"] + kx)
                    P.op(V, lambda e, o_im=o_im, dre=dre, xim=xim: e.tensor_tensor(o_im, dre, xim, ALU.mult),
                         reads=["D"], writes=["xt1"] + kx)
                    P.op(V, lambda e, o_im=o_im, t_b=t_b: e.tensor_tensor(o_im, o_im, t_b, ALU.subtract),
                         reads=["tmp1"], writes=["xt1"])
                if si == 2:
                    for c in range(2):
                        xv = xt[c][:, 0:128].rearrange("p (b t) -> p b t", t=8)[:, :, 0]
                        P.op(V, lambda e, c=c, k=k, xv=xv: e.scalar_tensor_tensor(
                            xv, h0T[c][:, k, :], rmag[:, k:k + 1], xv, ALU.mult, ALU.add),
                            reads=[("h0T", c), "rmag"], writes=[f"xt{c}"])
                    dec = decs[:]
                else:
                    dec = rmag[:, k:k + 1].to_broadcast([128, ln])
                for c in range(2):
                    P.op(V, lambda e, c=c, dec=dec, ln=ln: e.tensor_tensor_scan(
                        gg[c][:, 0:ln], dec, xt[c][:, 0:ln], 0.0, ALU.mult, ALU.add),
                        reads=[f"xt{c}", "rmag", "decs"], writes=[f"g{c}"])
                for ch in range(nch):
                    sl = slice(ch * 512, ch * 512 + cw)
                    if si < 2:
                        dre, dim = Dt[0][:, sl], Dt[1][:, sl]
                        g_re, g_im = gg[0][:, sl], gg[1][:, sl]
                        o_re, o_im = xt[0][:, sl], xt[1][:, sl]
                        t_a, t_b = tmp[0][:, 0:cw], tmp[1][:, 0:cw]
                    else:
                        v3 = lambda ap: ap.rearrange("p (b t) -> p b t", t=8)
                        dre = Dt[0][:, 0:8].unsqueeze(1).to_broadcast([128, 16, 8])
                        dim = Dt[1][:, 0:8].unsqueeze(1).to_broadcast([128, 16, 8])
                        g_re, g_im = v3(gg[0][:, 0:128]), v3(gg[1][:, 0:128])
                        o_re, o_im = v3(xt[0][:, 0:128]), v3(xt[1][:, 0:128])
                        t_a, t_b = v3(tmp[0][:, 0:128]), v3(tmp[1][:, 0:128])
                    P.op(V, lambda e, t_a=t_a, dim=dim, g_im=g_im: e.tensor_tensor(t_a, dim, g_im, ALU.mult),
                         reads=["D", "g1"], writes=["tmp0"])
                    P.op(V, lambda e, o_re=o_re, dre=dre, g_re=g_re: e.tensor_tensor(o_re, dre, g_re, ALU.mult),
                         reads=["D", "g0"], writes=["xt0"])
                    P.op(V, lambda e, o_re=o_re, t_a=t_a: e.tensor_tensor(o_re, o_re, t_a, ALU.subtract),
                         reads=["tmp0"], writes=["xt0"])
                    P.op(V, lambda e, t_b=t_b, dim=dim, g_re=g_re: e.tensor_tensor(t_b, dim, g_re, ALU.mult),
                         reads=["D", "g0"], writes=["tmp1"])
                    P.op(V, lambda e, o_im=o_im, dre=dre, g_im=g_im: e.tensor_tensor(o_im, dre, g_im, ALU.mult),
                         reads=["D", "g1"], writes=["xt1"])
                    P.op(V, lambda e, o_im=o_im, t_b=t_b: e.tensor_tensor(o_im, o_im, t_b, ALU.add),
                         reads=["tmp1"], writes=["xt1"])
                for c in range(2):
                    P.op("act", lambda e, c=c, j=j, t0=t0, ln=ln: e.activation(
                        hbuf[:, j, c, t0:t0 + ln], xt[c][:, 0:ln], AF.Copy),
                        reads=[f"xt{c}"], writes=[("hbuf", j, c, si)])
                    if si < 2:
                        P.op("act", lambda e, c=c, k=k, si=si: e.activation(
                            hfp[c][:, si, k:k + 1], xt[c][:, SEQ - 1:SEQ], AF.Copy),
                            reads=[f"xt{c}"], writes=[("hfp", c)])
                    else:
                        P.op("act", lambda e, c=c, k=k: e.activation(
                            hfs[c][:, :, k], xt[c][:, 0:128].rearrange("p (b t) -> p b t", t=8)[:, :, 7], AF.Copy),
                            reads=[f"xt{c}"], writes=[("hfs", c)])
            if j == 3:
                for ch in range(9):
                    t0 = ch * 512
                    cw = 512 if ch < 8 else 128
                    si = 0 if ch < 4 else (1 if ch < 8 else 2)
                    yi = cnt["y"] % 2
                    cnt["y"] += 1
                    n = 0
                    for jj in range(4):
                        for c in range(2):
                            P.op("pe", lambda e, yi=yi, jj=jj, c=c, t0=t0, cw=cw, n=n: e.matmul(
                                yps[yi][:, 0:cw], yl[c][jj][:], hbuf[:, jj, c, t0:t0 + cw],
                                start=(n == 0), stop=(n == 7)),
                                reads=[("yl", c, jj), ("hbuf", jj, c, si)], writes=[("yps", yi)])
                            n += 1
                    bi = cnt["ysb"] % 2
                    cnt["ysb"] += 1
                    P.op(V, lambda e, yi=yi, bi=bi, ub=ub, F=F, t0=t0, cw=cw: e.scalar_tensor_tensor(
                        ysb[bi][:, 0:cw], uT[ub][:, t0:t0 + cw], dsk[:, F:F + 1], yps[yi][:, 0:cw],
                        ALU.mult, ALU.add),
                        reads=[("uT", ub), "dsk"], writes=[("yps", yi), ("ysb", bi)])
                    P.op("pool", lambda e, bi=bi, cw=cw: e.tensor_tensor(yw[bi][:, 0:cw], ysb[bi][:, 0:cw], ysb[bi][:, 0:cw], ALU.mult),
                         reads=[("ysb", bi)], writes=[("yw", bi)])
                    P.op("pool", lambda e, bi=bi, cw=cw: e.tensor_scalar(yw[bi][:, 0:cw], yw[bi][:, 0:cw], 0.044715, 1.0, ALU.mult, ALU.add),
                         reads=[("yw", bi)], writes=[("yw", bi)])
                    P.op("pool", lambda e, bi=bi, cw=cw: e.tensor_tensor(yw[bi][:, 0:cw], yw[bi][:, 0:cw], ysb[bi][:, 0:cw], ALU.mult),
                         reads=[("yw", bi), ("ysb", bi)], writes=[("yw", bi)])
                    P.op("act", lambda e, bi=bi, cw=cw: e.activation(yw[bi][:, 0:cw], yw[bi][:, 0:cw], AF.Sigmoid, scale=1.5957691216057308),
                         reads=[("yw", bi)], writes=[("yw", bi)])
                    P.op("pool", lambda e, bi=bi, cw=cw, t0=t0: e.tensor_tensor(hsT[:, t0:t0 + cw], yw[bi][:, 0:cw], ysb[bi][:, 0:cw], ALU.mult),
                         reads=[("yw", bi), ("ysb", bi)], writes=["hsT"])
                P.dma("sp", T.hsT_s.ap()[F * 128:(F + 1) * 128, :], hsT[:], reads=["hsT"], writes=[("hsT_s", F)])
        for c, dstp, dsts in ((0, T.ssm_p_re, T.ssm_s_re), (1, T.ssm_p_im, T.ssm_s_im)):
            P.op("pe", lambda e, c=c: e.transpose(yps[0][0:64, 0:128], hfp[c][:].rearrange("p s k -> p (s k)"), C.ident_f[:]),
                 reads=[("hfp", c), "ident_f"], writes=[("yps", 0)])
            P.op("act", lambda e: e.activation(hfo[0:64, :], yps[0][0:64, 0:128], AF.Copy), writes=[("yps", 0), "hfo"])
            for s_ in range(2):
                P.dma("sp", dstp.ap()[s_].rearrange("(k p) -> k p", p=128), hfo[s_ * 32:(s_ + 1) * 32, :],
                      reads=["hfo"], writes=[("ssm_p", c, s_)])
            for q in range(4):
                P.op("pe", lambda e, c=c, q=q: e.transpose(
                    yps[1][:, 0:128], hfs[c][:, q * 4:(q + 1) * 4, :].rearrange("p b k -> p (b k)"), C.ident_f[:]),
                    reads=[("hfs", c), "ident_f"], writes=[("yps", 1)])
                P.op("act", lambda e: e.activation(hfo[:, :], yps[1][:, 0:128], AF.Copy), writes=[("yps", 1), "hfo"])
                for bl in range(4):
                    P.dma("sp", dsts.ap()[q * 4 + bl].rearrange("(k p) -> k p", p=128), hfo[bl * 32:(bl + 1) * 32, :],
                          reads=["hfo"], writes=[("ssm_s", c, q, bl)])
        P.emit()


def phase3(nc, S, T, C):
    with ExitStack() as st:
        sb = lambda n, shape, dt: st.enter_context(nc.sbuf_tensor(n, shape, dt))
        ps = lambda n, shape, dt: st.enter_context(nc.psum_tensor(n, shape, dt))
        P = Prog(S)
        qTg = [sb(f"p3_qT{i}", [128, 4, SEQ], BF16) for i in range(2)]
        kTg = [sb(f"p3_kT{i}", [128, 4, SEQ], BF16) for i in range(2)]
        Vg = [sb(f"p3_V{i}", [128, 16, 512], BF16) for i in range(2)]
        qTs = sb("p3_qTs", [128, 12, 128], BF16)
        kTs = sb("p3_kTs", [128, 12, 128], BF16)
        kc32 = [sb(f"p3_kc32{i}", [128, 512], F32) for i in range(2)]
        vc32 = [sb(f"p3_vc32{i}", [128, 512], F32) for i in range(2)]
        kcb = [sb(f"p3_kcb{i}", [128, 512], BF16) for i in range(2)]
        vcb = [sb(f"p3_vcb{i}", [128, 512], BF16) for i in range(2)]
        kcT = [sb(f"p3_kcT{i}", [128, 4, 128], BF16) for i in range(2)]
        vnew = [sb(f"p3_vnew{i}", [8, 512], BF16) for i in range(2)]
        Pb = [sb(f"p3_Pb{i}", [128, 256], BF16) for i in range(4)]
        PTs = [sb(f"p3_PTs{i}", [128, 2, 128], BF16) for i in range(4)]
        stat = [sb(f"p3_stat{i}", [128, 16], F32) for i in range(2)]
        Osb = [sb(f"p3_Osb{i}", [128, 512], F32) for i in range(2)]
        sps = [ps(f"p3_sps{i}", [128, 2, 256], F32) for i in range(2)]
        ptp = [ps(f"p3_ptp{i}", [128, 4, 2, 128], BF16) for i in range(2)]
        ops_ = [ps(f"p3_ops{i}", [128, 8, 64], F32) for i in range(2)]
        kps = ps("p3_kps", [128, 4, 128], BF16)

        cnt = {"unit": 0, "head": 0}
        scale = 0.125

        def unit(nq, blocks, q_of, out_rows_U, out_rows_md, extra_reads):
            ui = cnt["unit"] % 2
            cnt["unit"] += 1
            c_lo = blocks[0][0]
            c_hi = blocks[-1][0] + blocks[-1][1]
            def res(h):
                hi = hbase + h
                sp = sps[hi % 2]
                sslot = (hi // 2) % 2
                skey = ("sps", hi % 2)
                pb = hi % 4
                tp = ptp[hi % 2]
                tslot = (hi // 2) % 4
                tkey = ("ptp", hi % 2)
                return sp, sslot, skey, pb, tp, tslot, tkey

            def stage_s(h):
                sp, sslot, skey, pb, tp, tslot, tkey = res(h)
                first = True
                for (col0, n, kT_of, v_of, rk) in blocks:
                    P.op("pe", lambda e, sp=sp, sslot=sslot, col0=col0, n=n, kT_of=kT_of, h=h, first=first: e.matmul(
                        sp[0:nq, sslot, col0:col0 + n], q_of(h), kT_of(h), start=first, stop=False,
                        skip_group_check=True),
                        reads=list(extra_reads) + list(rk), writes=[skey])
                    first = False
                P.op("pe", lambda e, sp=sp, sslot=sslot: e.matmul(
                    sp[0:nq, sslot, c_lo:c_hi], C.ident_b[0:nq, 0:nq], C.maskadd[0:nq, c_lo:c_hi],
                    start=False, stop=True, skip_group_check=True),
                    reads=["ident_b", "maskadd"], writes=[skey])
                P.op("dve", lambda e, sp=sp, sslot=sslot, h=h: e.tensor_reduce(
                    stat[ui][0:nq, h:h + 1], sp[0:nq, sslot, c_lo:c_hi], AX.X, ALU.max, negate=True),
                    writes=[skey, ("stat", ui, h)])
                P.op("act", lambda e, sp=sp, sslot=sslot, h=h, pb=pb: e.activation(
                    Pb[pb][0:nq, c_lo:c_hi], sp[0:nq, sslot, c_lo:c_hi], AF.Exp,
                    bias=stat[ui][0:nq, h:h + 1], accum_out=stat[ui][0:nq, 8 + h:9 + h]),
                    writes=[skey, ("stat", ui, h), ("Pb", pb)])

            def stage_t(h):
                sp, sslot, skey, pb, tp, tslot, tkey = res(h)
                for bi, (col0, n, kT_of, v_of, rk) in enumerate(blocks):
                    P.op("pe", lambda e, tp=tp, tslot=tslot, bi=bi, col0=col0, n=n, pb=pb: e.transpose(
                        tp[0:n, tslot, bi, 0:nq], Pb[pb][0:nq, col0:col0 + n], C.ident_b[0:nq, 0:nq]),
                        reads=[("Pb", pb), "ident_b"], writes=[tkey])
                for bi, (col0, n, kT_of, v_of, rk) in enumerate(blocks):
                    if h % 2 == 0:
                        P.op("dve", lambda e, tp=tp, tslot=tslot, bi=bi, n=n, pb=pb: e.tensor_copy(
                            PTs[pb][0:n, bi, 0:nq], tp[0:n, tslot, bi, 0:nq]),
                            writes=[tkey, ("PTs", pb, bi)])
                    else:
                        P.op("act", lambda e, tp=tp, tslot=tslot, bi=bi, n=n, pb=pb: e.activation(
                            PTs[pb][0:n, bi, 0:nq], tp[0:n, tslot, bi, 0:nq], AF.Copy),
                            writes=[tkey, ("PTs", pb, bi)])

            def stage_v(h):
                sp, sslot, skey, pb, tp, tslot, tkey = res(h)
                for bi, (col0, n, kT_of, v_of, rk) in enumerate(blocks):
                    P.op("pe", lambda e, h=h, bi=bi, n=n, pb=pb, v_of=v_of: e.matmul(
                        ops_[ui][0:nq, h, :], PTs[pb][0:n, bi, 0:nq], v_of(h),
                        start=(bi == 0), stop=(bi == len(blocks) - 1), skip_group_check=True),
                        reads=[("PTs", pb, bi)] + list(rk), writes=[("ops", ui)])

            hbase = cnt["head"]
            cnt["head"] += 8
            for i in range(10):
                if i < 8:
                    stage_s(i)
                if 1 <= i <= 8:
                    stage_t(i - 1)
                if 2 <= i <= 9:
                    stage_v(i - 2)
            P.op("act", lambda e, ui=ui: e.activation(
                Osb[ui][0:nq, :], ops_[ui][0:nq, :, :].rearrange("p h e -> p (h e)"), AF.Copy),
                writes=[("ops", ui), ("Osb", ui)])
            P.dma("sp", out_rows_U, Osb[ui][0:nq, :], reads=[("Osb", ui)], writes=[("U_s", cnt["unit"])])
            P.dma("sp", out_rows_md, stat[ui][0:nq, :], reads=[("stat", ui, h) for h in range(8)], writes=[("md_s", cnt["unit"])])

        gi = 0
        for seq in DBG.get("p3_seqs", range(NPS)):
            for g in range(3):
                d = DILS[g]
                bsel = gi % 2
                gi += 1
                c0 = seq * SEQ
                P.dma("sp", qTg[bsel][:], T.qT_s.ap()[g * 512:(g + 1) * 512, c0:c0 + SEQ].rearrange("(hp p) t -> p hp t", p=128),
                      reads=[("q", 4 * g + f, ch) for f in range(4) for ch in range(8)], writes=[("qTg", bsel)])
                P.dma("sp", kTg[bsel][:], T.kT_s.ap()[g * 512:(g + 1) * 512, c0:c0 + SEQ].rearrange("(hp p) t -> p hp t", p=128),
                      reads=[("k", 4 * g + f, ch) for f in range(4) for ch in range(8)], writes=[("kTg", bsel)])
                nb = SEQ // d // 128
                for r in range(d):
                    for blk in range(nb):
                        ux = r * nb + blk
                        row0 = c0 + r + d * blk * 128
                        P.dma("sp", Vg[bsel][:, ux, :], T.vtok_s.ap()[ss(row0, 128, d), g * 512:(g + 1) * 512],
                              reads=[("vtok_s", ti, g) for ti in range(33)], writes=[("Vg", bsel, ux)])
                for r in range(d):
                    for blk in [b_ for b_ in DBG.get("p3_blks", range(nb)) if b_ < nb]:
                        ux = r * nb + blk
                        tq0 = r + d * blk * 128

                        def q_of(h, bsel=bsel, tq0=tq0, d=d):
                            return qTg[bsel][64 * (h % 2):64 * (h % 2) + 64, h // 2, ss(tq0, 128, d)]

                        blocks = []
                        if blk > 0:
                            tk0 = r + d * (blk - 1) * 128
                            blocks.append((0, 128,
                                           lambda h, bsel=bsel, tk0=tk0, d=d: kTg[bsel][64 * (h % 2):64 * (h % 2) + 64, h // 2, ss(tk0, 128, d)],
                                           lambda h, bsel=bsel, ux=ux: Vg[bsel][:, ux - 1, h * 64:(h + 1) * 64],
                                           [("kTg", bsel), ("Vg", bsel, ux - 1)]))
                        blocks.append((128, 128,
                                       lambda h, bsel=bsel, tq0=tq0, d=d: kTg[bsel][64 * (h % 2):64 * (h % 2) + 64, h // 2, ss(tq0, 128, d)],
                                       lambda h, bsel=bsel, ux=ux: Vg[bsel][:, ux, h * 64:(h + 1) * 64],
                                       [("kTg", bsel), ("Vg", bsel, ux)]))
                        rows = ss(c0 + tq0, 128, d)
                        unit(128, blocks, q_of, T.U_s[g].ap()[rows, :], T.md_s[g].ap()[rows, :], [("qTg", bsel)])
        P.dma("sp", qTs[:], T.qT_s.ap()[:, TOKP:TOK].rearrange("(f p) t -> p f t", p=128),
              reads=[("q", f, 8) for f in range(12)], writes=["qTs"])
        P.dma("sp", kTs[:], T.kT_s.ap()[:, TOKP:TOK].rearrange("(f p) t -> p f t", p=128),
              reads=[("k", f, 8) for f in range(12)], writes=["kTs"])
        caches = (T.c128, T.c512, T.c2048)
        ci = 0
        for b in DBG.get("p3_bs", range(NSS)):
            for g in range(3):
                d = DILS[g]
                nq = max(1, DEC // d)
                for r in range(min(d, DEC)):
                    cb = ci % 2
                    ci += 1
                    csrc = caches[g].ap()[b, ss(r, 128, d), :, :]
                    P.dma("sp", kc32[cb][:], csrc[:, 0, :], writes=[("kc32", cb)])
                    P.dma("sp", vc32[cb][:], csrc[:, 1, :], writes=[("vc32", cb)])
                    tok0 = TOKP + b * DEC + r
                    P.dma("sp", vnew[cb][0:nq, :], T.vtok_s.ap()[ss(tok0, nq, d), g * 512:(g + 1) * 512],
                          reads=[("vtok_s", 32, g)], writes=[("vnew", cb)])
                    P.op("pool", lambda e, cb=cb: e.tensor_copy(kcb[cb][:], kc32[cb][:]), reads=[("kc32", cb)], writes=[("kcb", cb)])
                    P.op("pool", lambda e, cb=cb: e.tensor_copy(vcb[cb][:], vc32[cb][:]), reads=[("vc32", cb)], writes=[("vcb", cb)])
                    for hp in range(4):
                        P.op("pe", lambda e, cb=cb, hp=hp: e.transpose(kps[:, hp, :], kcb[cb][:, hp * 128:(hp + 1) * 128], C.ident_b[:]),
                             reads=[("kcb", cb), "ident_b"], writes=["kps"])
                    P.op("dve", lambda e, cb=cb: e.tensor_copy(kcT[cb][:], kps[:]), writes=["kps", ("kcT", cb)])
                    col = b * DEC + r

                    def q_of(h, g=g, col=col, nq=nq, d=d):
                        return qTs[64 * (h % 2):64 * (h % 2) + 64, 4 * g + h // 2, ss(col, nq, d)]

                    blocks = [
                        (0, 128,
                         lambda h, cb=cb: kcT[cb][64 * (h % 2):64 * (h % 2) + 64, h // 2, :],
                         lambda h, cb=cb: vcb[cb][:, h * 64:(h + 1) * 64],
                         [("kcT", cb), ("vcb", cb)]),
                        (128, nq,
                         lambda h, g=g, col=col, nq=nq, d=d: kTs[64 * (h % 2):64 * (h % 2) + 64, 4 * g + h // 2, ss(col, nq, d)],
                         lambda h, cb=cb, nq=nq: vnew[cb][0:nq, h * 64:(h + 1) * 64],
                         ["kTs", ("vnew", cb)]),
                    ]
                    rows = ss(tok0, nq, d)
                    unit(nq, blocks, q_of, T.U_s[g].ap()[rows, :], T.md_s[g].ap()[rows, :], ["qTs"])
        P.emit()


def load_weight_bf16(P, nc, dst, src_ap, kch, ncols, wst, tag, scale_ap=None, engs=("dve", "pool")):
    src = src_ap.rearrange("(k p) f -> p k f", p=128)
    cw = 2048 // kch
    n = 0
    for c in range(0, ncols, cw):
        b = n % 2
        P.dma("sp", wst[b][:, 0:kch * cw].rearrange("p (k f) -> p k f", k=kch), src[:, :, c:c + cw],
              writes=[("wst", b)])
        for k in range(kch):
            eng = engs[n % len(engs)]
            n2 = n
            if scale_ap is None:
                P.op(eng, lambda e, b=b, k=k, c=c: e.tensor_copy(
                    dst[:, k, c:c + cw], wst[b][:, k * cw:(k + 1) * cw]),
                    reads=[("wst", b)], writes=[(tag, c)])
            else:
                P.op(eng, lambda e, b=b, k=k, c=c: e.tensor_scalar(
                    dst[:, k, c:c + cw], wst[b][:, k * cw:(k + 1) * cw], scale_ap[:, k:k + 1], None, ALU.mult),
                    reads=[("wst", b), tag + "_scale"], writes=[(tag, c)])
        n += 1
    return [(tag, c) for c in range(0, ncols, cw)]


def phase4(nc, S, T, C):
    with ExitStack() as st:
        sb = lambda n, shape, dt: st.enter_context(nc.sbuf_tensor(n, shape, dt))
        ps = lambda n, shape, dt: st.enter_context(nc.psum_tensor(n, shape, dt))
        P = Prog(S)
        Wa = sb("p4_Wa", [128, 8, D], BF16)
        Wb = sb("p4_Wb", [128, 8, D], BF16)
        Wp = sb("p4_Wp", [128, 4, D], BF16)
        Wo = sb("p4_Wo", [128, 8, D], BF16)
        wst = [sb(f"p4_wst{i}", [128, 2048], F32) for i in range(2)]
        U = [sb(f"p4_U{i}", [128, 3, 512], F32) for i in range(2)]
        md = [sb(f"p4_md{i}", [128, 3, 16], F32) for i in range(2)]
        nM = sb("p4_nM", [128, 8], F32)
        tdf = sb("p4_tdf", [128, 3, 8], F32)
        aw = sb("p4_aw", [128, 3, 8], F32)
        ad = sb("p4_ad", [128, 3, 8], F32)
        Z = sb("p4_Z", [128, 8], F32)
        acc = sb("p4_acc", [128, 512], F32)
        acc2 = sb("p4_acc2", [128, 512], F32)
        attn_b = sb("p4_attn_b", [128, 512], BF16)
        attnT = [sb(f"p4_attnT{i}", [128, 4, 128], BF16) for i in range(2)]
        hsT = [sb(f"p4_hsT{i}", [128, 8, 128], BF16) for i in range(2)]
        sga = [sb(f"p4_sga{i}", [128, 8, 128], F32) for i in range(2)]
        sgb = [sb(f"p4_sgb{i}", [128, 8, 128], F32) for i in range(2)]
        sig = [sb(f"p4_sig{i}", [128, 128], F32) for i in range(2)]
        t1 = [sb(f"p4_t1{i}", [128, 128], F32) for i in range(2)]
        t2 = [sb(f"p4_t2{i}", [128, 128], F32) for i in range(2)]
        mixT = [sb(f"p4_mixT{i}", [128, 8, 128], BF16) for i in range(2)]
        xs = [sb(f"p4_xs{i}", [128, D], F32) for i in range(2)]
        x1 = [sb(f"p4_x1{i}", [128, D], F32) for i in range(2)]
        ptp = ps("p4_ptp", [128, 4, 128], BF16)
        pabc = [ps(f"p4_pabc{i}", [128, 3, 128], F32) for i in range(2)]
        pout = [ps(f"p4_pout{i}", [128, 512], F32) for i in range(4)]

        ka = load_weight_bf16(P, nc, Wa, T.w_glu_a.ap(), 8, D, wst, "Wa")
        kb = load_weight_bf16(P, nc, Wb, T.w_glu_b.ap(), 8, D, wst, "Wb")
        kp = load_weight_bf16(P, nc, Wp, T.w_attn.ap(), 4, D, wst, "Wp")
        ko = load_weight_bf16(P, nc, Wo, T.w_out.ap(), 8, D, wst, "Wo")

        V = "dve"
        cnt = {"f": 0, "po": 0}
        for ti in DBG.get("p4_tiles", range(NTILE)):
            b = ti % 2
            t0 = ti * 128
            for g in range(3):
                P.dma("sp", U[b][:, g, :], T.U_s[g].ap()[t0:t0 + 128, :], writes=[("U", b)])
                P.dma("sp", md[b][:, g, :], T.md_s[g].ap()[t0:t0 + 128, :], writes=[("md", b)])
            P.dma("sp", hsT[b][:], T.hsT_s.ap()[:, t0:t0 + 128].rearrange("(k p) t -> p k t", p=128), writes=[("hsT", b)])
            P.dma("sp", sga[b][:], T.sga_s.ap()[:, t0:t0 + 128].rearrange("(k p) t -> p k t", p=128), writes=[("sga", b)])
            P.dma("sp", sgb[b][:], T.sgb_s.ap()[:, t0:t0 + 128].rearrange("(k p) t -> p k t", p=128), writes=[("sgb", b)])
            P.dma("sp", xs[b][:], T.x.ap()[t0:t0 + 128, :], writes=[("xs", b)])
            P.op(V, lambda e, b=b: e.tensor_tensor(nM[:], md[b][:, 0, 0:8], md[b][:, 1, 0:8], ALU.min), reads=[("md", b)], writes=["nM"])
            P.op(V, lambda e, b=b: e.tensor_tensor(nM[:], nM[:], md[b][:, 2, 0:8], ALU.min), reads=[("md", b), "nM"], writes=["nM"])
            P.op(V, lambda e, b=b: e.tensor_tensor(tdf[:], md[b][:, :, 0:8], nM[:].unsqueeze(1).to_broadcast([128, 3, 8]), ALU.subtract),
                 reads=[("md", b), "nM"], writes=["tdf"])
            P.op("act", lambda e: e.activation(aw[:], tdf[:], AF.Exp, scale=-1.0), reads=["tdf"], writes=["aw"])
            P.op(V, lambda e, b=b: e.tensor_tensor(ad[:], aw[:], md[b][:, :, 8:16], ALU.mult), reads=["aw", ("md", b)], writes=["ad"])
            P.op(V, lambda e: e.tensor_tensor(Z[:], ad[:, 0, :], ad[:, 1, :], ALU.add), reads=["ad"], writes=["Z"])
            P.op(V, lambda e: e.tensor_tensor(Z[:], Z[:], ad[:, 2, :], ALU.add), reads=["ad", "Z"], writes=["Z"])
            P.op(V, lambda e: e.reciprocal(Z[:], Z[:]), reads=["Z"], writes=["Z"])
            P.op(V, lambda e: e.tensor_tensor(aw[:], aw[:], Z[:].unsqueeze(1).to_broadcast([128, 3, 8]), ALU.mult),
                 reads=["aw", "Z"], writes=["aw"])
            wbs = [aw[:, g, :].unsqueeze(2).to_broadcast([128, 8, 64]) for g in range(3)]
            u3s = [U[b][:, g, :].rearrange("p (h e) -> p h e", e=64) for g in range(3)]
            acc3 = acc[:].rearrange("p (h e) -> p h e", e=64)
            acc23 = acc2[:].rearrange("p (h e) -> p h e", e=64)
            P.op(V, lambda e, u=u3s[0], w=wbs[0]: e.tensor_tensor(acc3, u, w, ALU.mult), reads=[("U", b), "aw"], writes=["acc"])
            P.op(V, lambda e, u=u3s[1], w=wbs[1]: e.tensor_tensor(acc23, u, w, ALU.mult), reads=[("U", b), "aw"], writes=["acc2"])
            P.op(V, lambda e: e.tensor_tensor(acc[:], acc[:], acc2[:], ALU.add), reads=["acc", "acc2"], writes=["acc"])
            P.op(V, lambda e, u=u3s[2], w=wbs[2]: e.tensor_tensor(acc23, u, w, ALU.mult), reads=[("U", b), "aw"], writes=["acc2"])
            P.op(V, lambda e: e.tensor_tensor(attn_b[:], acc[:], acc2[:], ALU.add), reads=["acc", "acc2"], writes=["attn_b"])
            for c in range(4):
                P.op("pe", lambda e, c=c: e.transpose(ptp[:, c, :], attn_b[:, c * 128:(c + 1) * 128], C.ident_b[:]),
                     reads=["attn_b", "ident_b"], writes=["ptp"])
            P.op("act", lambda e, b=b: e.activation(attnT[b][:], ptp[:], AF.Copy), writes=["ptp", ("attnT", b)])
            for F in range(8):
                fi = cnt["f"] % 2
                cnt["f"] += 1
                pk = ("pabc", fi)
                for k in range(8):
                    P.op("pe", lambda e, fi=fi, k=k, F=F, b=b: e.matmul(
                        pabc[fi][:, 0, :], Wa[:, k, F * 128:(F + 1) * 128], hsT[b][:, k, :], start=(k == 0), stop=(k == 7),
                        skip_group_check=True), reads=ka + [("hsT", b)], writes=[pk])
                for k in range(8):
                    P.op("pe", lambda e, fi=fi, k=k, F=F, b=b: e.matmul(
                        pabc[fi][:, 1, :], Wb[:, k, F * 128:(F + 1) * 128], hsT[b][:, k, :], start=False, stop=(k == 7),
                        skip_group_check=True), reads=kb + [("hsT", b)], writes=[pk])
                for k in range(4):
                    P.op("pe", lambda e, fi=fi, k=k, F=F, b=b: e.matmul(
                        pabc[fi][:, 2, :], Wp[:, k, F * 128:(F + 1) * 128], attnT[b][:, k, :], start=False, stop=(k == 3),
                        skip_group_check=True), reads=kp + [("attnT", b)], writes=[pk])
                P.op("act", lambda e, fi=fi: e.activation(sig[fi][:], pabc[fi][:, 1, :], AF.Sigmoid), writes=[pk, ("sig", fi)])
                P.op(V, lambda e, fi=fi: e.tensor_tensor(t1[fi][:], pabc[fi][:, 0, :], sig[fi][:], ALU.mult),
                     reads=[("sig", fi)], writes=[pk, ("t1", fi)])
                P.op(V, lambda e, fi=fi, F=F, b=b: e.tensor_tensor(t2[fi][:], pabc[fi][:, 2, :], sgb[b][:, F, :], ALU.mult),
                     reads=[("sgb", b)], writes=[pk, ("t2", fi)])
                P.op("pool", lambda e, fi=fi, F=F, b=b: e.tensor_tensor(t1[fi][:], t1[fi][:], sga[b][:, F, :], ALU.mult),
                     reads=[("sga", b)], writes=[("t1", fi)])
                P.op("pool", lambda e, fi=fi, F=F, b=b: e.tensor_tensor(mixT[b][:, F, :], t1[fi][:], t2[fi][:], ALU.add),
                     reads=[("t1", fi), ("t2", fi)], writes=[("mixT", b)])
            for half in range(2):
                po = cnt["po"] % 4
                cnt["po"] += 1
                for k in range(8):
                    P.op("pe", lambda e, po=po, k=k, half=half, b=b: e.matmul(
                        pout[po][:], mixT[b][:, k, :], Wo[:, k, half * 512:(half + 1) * 512], start=(k == 0), stop=(k == 7)),
                        reads=ko + [("mixT", b)], writes=[("pout", po)])
                P.op(V, lambda e, po=po, half=half, b=b: e.tensor_tensor(
                    x1[b][:, half * 512:(half + 1) * 512], pout[po][:], xs[b][:, half * 512:(half + 1) * 512], ALU.add),
                    reads=[("xs", b)], writes=[("pout", po), ("x1", b)])
            P.dma("sp", T.x1_s.ap()[t0:t0 + 128, :], x1[b][:], reads=[("x1", b)], writes=[("x1_s", ti)])
        P.emit()


def phase5a(nc, S, T, C):
    with ExitStack() as st:
        sb = lambda n, shape, dt: st.enter_context(nc.sbuf_tensor(n, shape, dt))
        P = Prog(S)
        R = 4
        NBUF = 4
        cin = [sb(f"p5a_in{i}", [128, R, D], F32) for i in range(NBUF)]
        cout = [sb(f"p5a_out{i}", [128, R, D], BF16) for i in range(NBUF)]
        nexp = T.u_tab.shape[0]
        nblk = nexp // (128 * R)
        engs = ("act", "dve", "pool")
        n = 0
        for blk in range(nblk):
            r0 = blk * 128 * R
            for c, tab in ((0, T.u_tab), (1, T.v_tab)):
                i = n % NBUF
                P.dma("sp", cin[i][:], tab.ap()[r0:r0 + 128 * R, :].rearrange("(p r) d -> p r d", r=R), writes=[("cin", i)])
                eng = engs[n % 3]
                if eng == "act":
                    P.op("act", lambda e, i=i: e.activation(cout[i][:], cin[i][:], AF.Copy), reads=[("cin", i)], writes=[("cout", i)])
                else:
                    P.op(eng, lambda e, i=i: e.tensor_copy(cout[i][:], cin[i][:]), reads=[("cin", i)], writes=[("cout", i)])
                P.dma("sp", T.uv_s.ap()[r0:r0 + 128 * R, c, :].rearrange("(p r) d -> p r d", r=R), cout[i][:],
                      reads=[("cout", i)], writes=[("uv_s", blk, c)])
                n += 1
        P.emit()


def phase5(nc, S, T, C):
    NEG = -1.0e30
    with ExitStack() as st:
        sb = lambda n, shape, dt: st.enter_context(nc.sbuf_tensor(n, shape, dt))
        ps = lambda n, shape, dt: st.enter_context(nc.psum_tensor(n, shape, dt))
        P = Prog(S)
        V = "dve"
        Wq = sb("p5_Wq", [128, 8, 2048], BF16)
        wst = [sb(f"p5_wst{i}", [128, 2048], F32) for i in range(2)]
        gk = sb("p5_gk", [128, 8], F32)
        skT = sb("p5_skT", [128, 16, 128], BF16)
        skb = [sb(f"p5_skb{i}", [128, 128], BF16) for i in range(2)]
        gffn = sb("p5_gffn", [128, D], F32)
        gfin = sb("p5_gfin", [128, D], F32)
        io16 = sb("p5_io16", [128, 256], I32)
        io256 = sb("p5_io256", [128, 256], F32)
        x1 = [sb(f"p5_x1{i}", [128, D], F32) for i in range(2)]
        xn2s = [sb(f"p5_xn2s{i}", [128, D], BF16) for i in range(2)]
        eidx_is = [sb(f"p5_eidx_is{i}", [128, 128], I32) for i in range(2)]
        gates = [sb(f"p5_gates{i}", [128, 128], F32) for i in range(2)]
        NYIELD = 2
        ss = sb("p5_ss", [128, 4], F32)
        xn2b = sb("p5_xn2b", [128, D], BF16)
        xn2T = sb("p5_xn2T", [128, 8, 128], BF16)
        qpT = sb("p5_qpT", [128, 16, 128], BF16)
        sc = sb("p5_sc", [128, 16, 128], F32)
        scw = sb("p5_scw", [128, 16, 128], F32)
        sv = sb("p5_sv", [128, 16, 16], F32)
        si = sb("p5_si", [128, 16, 16], U32)
        sif = sb("p5_sif", [128, 16, 16], F32)
        cand = sb("p5_cand", [128, 8, 256], F32)
        candw = sb("p5_candw", [128, 8, 256], F32)
        cidx = sb("p5_cidx", [128, 8, 256], F32)
        best = sb("p5_best", [128, 8, 16], F32)
        pos = sb("p5_pos", [128, 8, 16], U32)
        posf = sb("p5_posf", [128, 8, 16], F32)
        eqb = sb("p5_eqb", [128, 8, 256], F32)
        eidx = sb("p5_eidx", [128, 128], F32)
        ex = sb("p5_ex", [128, 8, 16], F32)
        gs = sb("p5_gs", [128, 8], F32)
        dots = sb("p5_dots", [128, 128], F32)
        gw = [sb(f"p5_gw{i}", [128, 8], F32) for i in range(2)]
        wgt = sb("p5_wgt", [128, 128], F32)
        NB = 16
        gb = [sb(f"p5_gb{i}", [128, 2 * D], BF16) for i in range(8)]
        gb_extra = {}
        for i_ in range(2):
            wv = wst[i_][:].bitcast(BF16)
            gb.append(wv[:, 0:2 * D])
            gb.append(wv[:, 2 * D:4 * D])
            gb_extra[8 + 2 * i_] = ("wst", i_)
            gb_extra[9 + 2 * i_] = ("wst", i_)
        for i_ in range(4):
            gb.append(sb(f"p5_gbx{i_}", [128, 2 * D], BF16)[:])
        gbv = lambda i: (gb[i] if i >= 8 else gb[i][:])
        gbk = lambda i: [("gb", i)] + ([gb_extra[i]] if i in gb_extra else [])
        prod = [sb(f"p5_prod{i}", [128, D], BF16) for i in range(4)]
        junk = sb("p5_junk", [128, D], BF16)
        diag = [sb(f"p5_diag{i}", [128, 128], BF16) for i in range(4)]
        yb = sb("p5_yb", [128, D], F32)
        pacc = [ps(f"p5_pacc{i}", [128, 512], F32) for i in range(2)]
        uv2d = T.uv_s.ap().rearrange("n c d -> n (c d)")
        ptp = ps("p5_ptp", [128, 8, 128], BF16)
        pq = [ps(f"p5_pq{i}", [128, 4, 128], F32) for i in range(2)]
        psc = [ps(f"p5_psc{i}", [128, 4, 128], F32) for i in range(2)]

        bc_reg = nc.gpsimd.alloc_register("p5_bc")
        nc.gpsimd.reg_mov(bc_reg, T.u_tab.shape[0] - 1)
        P.dma("sp", gk[:], T.g_ffn.ap().rearrange("(k p) -> p k", p=128), writes=["Wq_scale"], allow_slow_non_contiguous=True)
        kq = load_weight_bf16(P, nc, Wq, T.w_qp.ap(), 8, 2048, wst, "Wq", scale_ap=gk)
        P.dma("sp", gffn[:], T.g_ffn.ap().partition_broadcast(128), writes=["gffn"])
        P.dma("sp", gfin[:], T.g_final.ap().partition_broadcast(128), writes=["gfin"])
        P.op("pool", lambda e: e.iota(io16[:], [[1, 256]], base=0, channel_multiplier=0), writes=["io16"])
        P.op(V, lambda e: e.tensor_copy(io256[:], io16[:]), reads=["io16"], writes=["io256"])
        for hp in range(16):
            b = hp % 2
            P.dma("sp", wst[b][:, 0:128], T.sub_keys.ap()[hp], writes=[("wst", b)])
            P.op(V, lambda e, b=b: e.tensor_copy(skb[b][:], wst[b][:, 0:128]), reads=[("wst", b)], writes=[("skb", b)])
            P.op("pe", lambda e, b=b: e.transpose(ptp[:, 0, :], skb[b][:], C.ident_b[:]), reads=[("skb", b), "ident_b"], writes=["ptp"])
            P.op("act", lambda e, hp=hp: e.activation(skT[:, hp, :], ptp[:, 0, :], AF.Copy), writes=["ptp", "skT"])

        def rms(src, col, reads):
            P.op("act", lambda e: e.activation(junk[:], src, AF.Square, accum_out=ss[:, col:col + 1]),
                 reads=reads, writes=["junk", ("ss", col)])
            P.op(V, lambda e: e.tensor_scalar(ss[:, col:col + 1], ss[:, col:col + 1], 1.0 / D, EPS, ALU.mult, ALU.add),
                 reads=[("ss", col)], writes=[("ss", col)])
            P.op("act", lambda e: e.activation(ss[:, col:col + 1], ss[:, col:col + 1], AF.Sqrt), reads=[("ss", col)], writes=[("ss", col)])
            P.op(V, lambda e: e.reciprocal(ss[:, col:col + 1], ss[:, col:col + 1]), reads=[("ss", col)], writes=[("ss", col)])

        def route(ti):
            b = ti % 2
            t0 = ti * 128
            P.dma("sp", x1[b][:], T.x1_s.ap()[t0:t0 + 128, :], reads=[("x1_s", ti)], writes=[("x1", b)])
            rms(x1[b][:], 0, [("x1", b)])
            P.op(V, lambda e, b=b: e.scalar_tensor_tensor(xn2s[b][:], x1[b][:], ss[:, 0:1], gffn[:], ALU.mult, ALU.mult),
                 reads=[("x1", b), ("ss", 0), "gffn"], writes=[("xn2", b)])
            P.op("pool", lambda e, b=b: e.tensor_scalar(xn2b[:], x1[b][:], ss[:, 0:1], None, ALU.mult),
                 reads=[("x1", b), ("ss", 0)], writes=["xn2b"])
            for k in range(8):
                P.op("pe", lambda e, k=k: e.transpose(ptp[:, k, :], xn2b[:, k * 128:(k + 1) * 128], C.ident_b[:]),
                     reads=["xn2b", "ident_b"], writes=["ptp"])
            P.op("act", lambda e: e.activation(xn2T[:], ptp[:], AF.Copy), writes=["ptp", "xn2T"])
            yield
            for j in range(4):
                pj = j % 2
                for hh in range(4):
                    hp = 4 * j + hh
                    for k in range(8):
                        P.op("pe", lambda e, pj=pj, hh=hh, hp=hp, k=k: e.matmul(
                            pq[pj][:, hh, :], Wq[:, k, hp * 128:(hp + 1) * 128], xn2T[:, k, :],
                            start=(k == 0 and hh == 0), stop=(k == 7), skip_group_check=True),
                            reads=kq + ["xn2T"], writes=[("pq", pj)])
                eng = "act" if j % 2 == 0 else V
                if eng == "act":
                    P.op("act", lambda e, pj=pj, j=j: e.activation(qpT[:, 4 * j:4 * j + 4, :], pq[pj][:], AF.Copy),
                         writes=[("pq", pj), ("qpT", j)])
                else:
                    P.op(V, lambda e, pj=pj, j=j: e.tensor_copy(qpT[:, 4 * j:4 * j + 4, :], pq[pj][:]),
                         writes=[("pq", pj), ("qpT", j)])
                yield
            for j in range(4):
                pj = j % 2
                for hh in range(4):
                    hp = 4 * j + hh
                    P.op("pe", lambda e, pj=pj, hh=hh, hp=hp: e.matmul(
                        psc[pj][:, hh, :], qpT[:, hp, :], skT[:, hp, :], start=(hh == 0), stop=True, skip_group_check=True),
                        reads=[("qpT", j), "skT"], writes=[("psc", pj)])
                P.op("act", lambda e, pj=pj, j=j: e.activation(sc[:, 4 * j:4 * j + 4, :], psc[pj][:], AF.Copy),
                     writes=[("psc", pj), ("sc", j)])
                yield
            for step in range(5):
                for hp in range(16):
                    j = hp // 4
                    if step == 0:
                        P.op(V, lambda e, hp=hp: e.max(sv[:, hp, 0:8], sc[:, hp, :]), reads=[("sc", j)], writes=[("sv", hp)])
                    elif step == 1:
                        P.op(V, lambda e, hp=hp: e.max_index(si[:, hp, 0:8], sv[:, hp, 0:8], sc[:, hp, :]),
                             reads=[("sc", j), ("sv", hp)], writes=[("si", hp)])
                    elif step == 2:
                        P.op(V, lambda e, hp=hp: e.match_replace(scw[:, hp, :], sv[:, hp, 0:8], sc[:, hp, :], NEG),
                             reads=[("sc", j), ("sv", hp)], writes=[("scw", hp)])
                    elif step == 3:
                        P.op(V, lambda e, hp=hp: e.max(sv[:, hp, 8:16], scw[:, hp, :]), reads=[("scw", hp)], writes=[("sv", hp)])
                    else:
                        P.op(V, lambda e, hp=hp: e.max_index(si[:, hp, 8:16], sv[:, hp, 8:16], scw[:, hp, :]),
                             reads=[("scw", hp), ("sv", hp)], writes=[("si", hp)])
                    if hp % 4 == 3:
                        yield
            svk = [("sv", hp) for hp in range(16)]
            sik = [("si", hp) for hp in range(16)]
            P.op(V, lambda e: e.tensor_copy(sif[:], si[:]), reads=sik, writes=["sif"])
            sv4 = sv[:].rearrange("p (h two) k -> p h two k", two=2)
            sif4 = sif[:].rearrange("p (h two) k -> p h two k", two=2)
            c4 = lambda t: t[:].rearrange("p h (a b) -> p h a b", b=16)
            P.op(V, lambda e: e.tensor_tensor(c4(cand), sv4[:, :, 0, :].unsqueeze(3).to_broadcast([128, 8, 16, 16]),
                                              sv4[:, :, 1, :].unsqueeze(2).to_broadcast([128, 8, 16, 16]), ALU.add),
                 reads=svk, writes=["cand"])
            P.op(V, lambda e: e.tensor_scalar(c4(cidx), sif4[:, :, 0, :].unsqueeze(3).to_broadcast([128, 8, 16, 16]), 128.0, None, ALU.mult),
                 reads=["sif"], writes=["cidx"])
            P.op(V, lambda e: e.tensor_tensor(c4(cidx), c4(cidx), sif4[:, :, 1, :].unsqueeze(2).to_broadcast([128, 8, 16, 16]), ALU.add),
                 reads=["sif", "cidx"], writes=["cidx"])
            for step in range(5):
                for h in range(8):
                    if step == 0:
                        P.op(V, lambda e, h=h: e.max(best[:, h, 0:8], cand[:, h, :]), reads=["cand"], writes=[("best", h)])
                    elif step == 1:
                        P.op(V, lambda e, h=h: e.max_index(pos[:, h, 0:8], best[:, h, 0:8], cand[:, h, :]),
                             reads=["cand", ("best", h)], writes=[("pos", h)])
                    elif step == 2:
                        P.op(V, lambda e, h=h: e.match_replace(candw[:, h, :], best[:, h, 0:8], cand[:, h, :], NEG),
                             reads=["cand", ("best", h)], writes=[("candw", h)])
                    elif step == 3:
                        P.op(V, lambda e, h=h: e.max(best[:, h, 8:16], candw[:, h, :]), reads=[("candw", h)], writes=[("best", h)])
                    else:
                        P.op(V, lambda e, h=h: e.max_index(pos[:, h, 8:16], best[:, h, 8:16], candw[:, h, :]),
                             reads=[("candw", h), ("best", h)], writes=[("pos", h)])
                    if h % 4 == 3:
                        yield
            bk = [("best", h) for h in range(8)]
            pk = [("pos", h) for h in range(8)]
            P.op(V, lambda e: e.tensor_copy(posf[:], pos[:]), reads=pk, writes=["posf"])
            for h in range(8):
                for hk in range(2):
                    bkey = ["eqb"]
                    e3 = eqb[:]
                    ks = slice(hk * 8, hk * 8 + 8)
                    P.op(V, lambda e, h=h, e3=e3, ks=ks: e.tensor_tensor(
                        e3, io256[:].unsqueeze(1).to_broadcast([128, 8, 256]),
                        posf[:, h, ks].unsqueeze(2).to_broadcast([128, 8, 256]), ALU.is_equal),
                        reads=["io256", "posf"], writes=bkey)
                    P.op(V, lambda e, h=h, e3=e3: e.tensor_tensor(
                        e3, e3, cidx[:, h, :].unsqueeze(1).to_broadcast([128, 8, 256]), ALU.mult),
                        reads=["cidx"], writes=bkey)
                    P.op(V, lambda e, h=h, e3=e3, hk=hk: e.tensor_reduce(eidx[:, h * 16 + hk * 8:h * 16 + hk * 8 + 8], e3, AX.X, ALU.add),
                         reads=bkey, writes=[("eidx", h)])
                    yield
            ek = [("eidx", h) for h in range(8)]
            P.op(V, lambda e, b=b: e.tensor_copy(eidx_is[b][:], eidx[:]), reads=ek, writes=[("eidx_i", b)])
            P.op(V, lambda e: e.tensor_tensor(ex[:], best[:], best[:, :, 0:1].to_broadcast([128, 8, 16]), ALU.subtract),
                 reads=bk, writes=["ex"])
            P.op("act", lambda e: e.activation(ex[:], ex[:], AF.Exp), reads=["ex"], writes=["ex"])
            P.op(V, lambda e: e.tensor_reduce(gs[:], ex[:], AX.X, ALU.add), reads=["ex"], writes=["gs"])
            P.op(V, lambda e: e.reciprocal(gs[:], gs[:]), reads=["gs"], writes=["gs"])
            P.op(V, lambda e, b=b: e.tensor_tensor(gates[b][:].rearrange("p (h k) -> p h k", k=16), ex[:],
                                                   gs[:].unsqueeze(2).to_broadcast([128, 8, 16]), ALU.mult),
                 reads=["ex", "gs"], writes=[("gate", b)])
            yield

        def gather(ti, nxt):
            b = ti % 2
            t0 = ti * 128
            GS = 4
            for k0 in range(0, 128, GS):
                for k in range(k0, k0 + GS):
                    gbi = k % NB
                    pi = k % 4
                    P.op("pool", lambda e, k=k, gbi=gbi, b=b: e.indirect_dma_start(
                        out=gbv(gbi)[:, :], out_offset=None, in_=uv2d,
                        in_offset=bass.IndirectOffsetOnAxis(ap=eidx_is[b][:, k:k + 1], axis=0),
                        bounds_check=bc_reg, oob_is_err=False),
                        reads=[("eidx_i", b)], writes=gbk(gbi), dma=True)
                    P.op(V, lambda e, gbi=gbi, pi=pi, b=b: e.tensor_tensor(prod[pi][:], gbv(gbi)[:, 0:D], xn2s[b][:], ALU.mult),
                         reads=[("gb", gbi), ("xn2", b)], writes=[("prod", pi)])
                    P.op("act", lambda e, k=k, pi=pi: e.activation(junk[:], prod[pi][:], AF.Copy, accum_out=dots[:, k:k + 1]),
                         reads=[("prod", pi)], writes=["junk", ("dots", k0)])
                dk = [("dots", k0)]
                sl = slice(k0, k0 + GS)
                gk_ = ("gw", (k0 // GS) % 2)
                gwb = gw[(k0 // GS) % 2]
                P.op("act", lambda e, sl=sl, gwb=gwb: e.activation(gwb[:, 0:GS], dots[:, sl], AF.Gelu_apprx_tanh), reads=dk, writes=[gk_])
                P.op(V, lambda e, sl=sl, gwb=gwb, b=b: e.tensor_tensor(wgt[:, sl], gwb[:, 0:GS], gates[b][:, sl], ALU.mult), reads=[gk_, ("gate", b)], writes=[("wgt", k0)])
                for k in range(k0, k0 + GS):
                    gbi = k % NB
                    di = k % 4
                    P.op(V, lambda e, k=k, di=di: e.tensor_scalar(diag[di][:], C.ident_b[:], wgt[:, k:k + 1], None, ALU.mult),
                         reads=["ident_b", ("wgt", k0)], writes=[("diag", di)])
                    for hf in range(2):
                        P.op("pe", lambda e, k=k, di=di, gbi=gbi, hf=hf: e.matmul(
                            pacc[hf][:], diag[di][:], gbv(gbi)[:, D + hf * 512:D + (hf + 1) * 512],
                            start=(k == 0), stop=(k == 127)),
                            reads=[("diag", di), ("gb", gbi)], writes=[("pacc", hf)])
                if nxt is not None:
                    for _ in range(NYIELD):
                        next(nxt, None)
            for hf in range(2):
                P.op(V, lambda e, hf=hf, b=b: e.tensor_tensor(
                    yb[:, hf * 512:(hf + 1) * 512], pacc[hf][:], x1[b][:, hf * 512:(hf + 1) * 512], ALU.add),
                    reads=[("x1", b)], writes=[("pacc", hf), "yb"])
            rms(yb[:], 1, ["yb"])
            P.op(V, lambda e: e.scalar_tensor_tensor(yb[:], yb[:], ss[:, 1:2], gfin[:], ALU.mult, ALU.mult),
                 reads=["yb", ("ss", 1), "gfin"], writes=["yb"])
            P.dma("sp", T.y.ap()[t0:t0 + 128, :], yb[:], reads=["yb"], writes=[("y", ti)])

        tiles = list(DBG.get("p5_tiles", range(NTILE)))
        gens = {ti: route(ti) for ti in tiles}
        for _ in gens[tiles[0]]:
            pass
        for n_, ti in enumerate(tiles):
            nxt = gens[tiles[n_ + 1]] if n_ + 1 < len(tiles) else None
            gather(ti, nxt)
            if nxt is not None:
                for _ in nxt:
                    pass
        P.emit()
```

```python
from contextlib import ExitStack
import math
import numpy as np
import concourse.bass as bass
import concourse.mybir as mybir
from concourse.bass_utils import run_bass_kernel_spmd

F32 = mybir.dt.float32
BF16 = mybir.dt.bfloat16
I32 = mybir.dt.int32
U32 = mybir.dt.uint32
ALU = mybir.AluOpType
AF = mybir.ActivationFunctionType
AX = mybir.AxisListType

NCORES = 8
D = 1024
SEQ = 2048
NPS = 2
NSS = 16
DEC = 8
TOKP = NPS * SEQ
TOK = TOKP + 128
NTILE = TOK // 128
PROJ = 7680
OFF_U, OFF_Q, OFF_K, OFF_V, OFF_GA, OFF_GB = 0, 1024, 2560, 4096, 5632, 6656
WINS = (128, 512, 2048)
DILS = (1, 4, 16)
EPS = 1e-6
NEXP = 16384
DBG = {}


class Op:
    __slots__ = ("eng", "fn", "reads", "writes", "dma", "deps", "needs_inc", "sem", "val")

    def __init__(self, eng, fn, reads, writes, dma):
        self.eng = eng
        self.fn = fn
        self.reads = reads
        self.writes = writes
        self.dma = dma
        self.deps = ()
        self.needs_inc = False
        self.sem = None
        self.val = 0


class Sync:
    def __init__(self, nc, stack, dma_pool=None):
        self.nc = nc
        self.engs = {"pe": nc.tensor, "act": nc.scalar, "dve": nc.vector,
                     "pool": nc.gpsimd, "sp": nc.sync}
        dma_pool = dma_pool or {"sp": 24, "pool": 8, "act": 4}
        self.csem = {}
        self.ccount = {}
        for e in ("pe", "act", "dve", "pool"):
            self.csem[e] = stack.enter_context(nc.semaphore("cs_" + e))
            self.ccount[e] = 0
        self.pools = {}
        for q, n in dma_pool.items():
            self.pools[q] = {
                "sems": [stack.enter_context(nc.semaphore(f"ds_{q}_{i}")) for i in range(n)],
                "vals": [0] * n, "next": 0}
        self.waited = {e: {} for e in self.engs}
        self.n_inst = 0

    def wait(self, eng_name, sem, val):
        w = self.waited[eng_name]
        key = id(sem)
        if w.get(key, 0) >= val:
            return
        self.engs[eng_name].wait_ge(sem, val)
        w[key] = val

    def barrier(self, engines=("pe", "act", "dve", "pool", "sp")):
        for E in engines:
            for q, p in self.pools.items():
                for sem, v in zip(p["sems"], p["vals"]):
                    if v > 0:
                        self.wait(E, sem, v)
            for e in ("pe", "act", "dve", "pool"):
                if self.ccount[e] > 0 and e != E:
                    self.wait(E, self.csem[e], self.ccount[e])


class Prog:
    def __init__(self, sync):
        self.S = sync
        self.ops = []

    def op(self, eng, fn, reads=(), writes=(), dma=False):
        self.ops.append(Op(eng, fn, tuple(reads), tuple(writes), dma))

    def dma(self, eng, out, in_, reads=(), writes=(), **kw):
        self.op(eng, lambda e: e.dma_start(out=out, in_=in_, **kw), reads, writes, dma=True)

    def emit(self, barrier=True):
        S = self.S
        ops = self.ops
        last_w = {}
        readers = {}
        last_on_eng = {}
        for i, o in enumerate(ops):
            deps = set()
            for k in o.reads:
                if k in last_w:
                    deps.add(last_w[k])
            for k in o.writes:
                if k in last_w:
                    deps.add(last_w[k])
                deps.update(readers.get(k, ()))
            deps.discard(i)
            for k in o.reads:
                readers.setdefault(k, []).append(i)
            for k in o.writes:
                last_w[k] = i
                readers[k] = []
            o.deps = sorted(deps)
            for j in o.deps:
                ops[j].needs_inc = True
            if not o.dma:
                last_on_eng[o.eng] = i
        for i in last_on_eng.values():
            ops[i].needs_inc = True
        for o in ops:
            E = o.eng
            eng = S.engs[E]
            for j in o.deps:
                d = ops[j]
                if (not d.dma) and d.eng == "pe" and E == "pe" and not o.dma:
                    continue
                S.wait(E, d.sem, d.val)
            if o.dma:
                p = S.pools[E]
                k = p["next"]
                p["next"] = (k + 1) % len(p["sems"])
                sem = p["sems"][k]
                if p["vals"][k] > 0:
                    S.wait(E, sem, p["vals"][k])
                p["vals"][k] += 16
                ins = o.fn(eng)
                ins.then_inc(sem, 16)
                o.sem = sem
                o.val = p["vals"][k]
            else:
                ins = o.fn(eng)
                if o.needs_inc:
                    S.ccount[E] += 1
                    ins.then_inc(S.csem[E], 1)
                    o.sem = S.csem[E]
                    o.val = S.ccount[E]
            S.n_inst += 1
        if barrier:
            S.barrier()


class Ctx:
    pass


def ss(start, n, step):
    return slice(start, start + (n - 1) * step + 1, step)


def declare_io(nc):
    T = Ctx()
    di = lambda n, s, dt=F32: nc.dram_tensor(n, list(s), dt, kind="ExternalInput")
    do = lambda n, s, dt=F32: nc.dram_tensor(n, list(s), dt, kind="ExternalOutput")
    ds = lambda n, s, dt: nc.dram_tensor(n, list(s), dt, kind=("ExternalOutput" if n in DBG.get("expose", ()) else ("ExternalInput" if n in DBG.get("inject", ()) else "Internal")))
    T.x = di("x", [TOK, D])
    T.st_re = di("st_re", [NSS, 4096])
    T.st_im = di("st_im", [NSS, 4096])
    nss = 1 if DBG.get("small") else NSS
    nexp = 512 if DBG.get("small_tab") else NEXP
    T.c128 = di("c128", [nss, 128, 2, 512])
    T.c512 = di("c512", [nss, 512, 2, 512])
    T.c2048 = di("c2048", [nss, 2048, 2, 512])
    T.g_mix = di("g_mix", [D])
    T.w_in = di("w_in", [D, PROJ])
    T.lam_re = di("lam_re", [64, 64])
    T.lam_im = di("lam_im", [64, 64])
    T.log_dt = di("log_dt", [64])
    T.b_re = di("b_re", [4096, 16])
    T.b_im = di("b_im", [4096, 16])
    T.c_re = di("c_re", [1024, 64])
    T.c_im = di("c_im", [1024, 64])
    T.d_skip = di("d_skip", [1024])
    T.w_glu_a = di("w_glu_a", [D, D])
    T.w_glu_b = di("w_glu_b", [D, D])
    T.w_attn = di("w_attn", [512, D])
    T.w_out = di("w_out", [D, D])
    T.g_ffn = di("g_ffn", [D])
    T.w_qp = di("w_qp", [D, 2048])
    T.sub_keys = di("sub_keys", [16, 128, 128])
    T.u_tab = di("u_tab", [nexp, D])
    T.v_tab = di("v_tab", [nexp, D])
    T.g_final = di("g_final", [D])
    T.y = do("y", [TOK, D])
    T.ssm_p_re = do("ssm_p_re", [NPS, 4096])
    T.ssm_p_im = do("ssm_p_im", [NPS, 4096])
    T.ssm_s_re = do("ssm_s_re", [NSS, 4096])
    T.ssm_s_im = do("ssm_s_im", [NSS, 4096])
    T.kvp = [do(f"kvp{w}", [NPS, w, 1024]) for w in WINS]
    T.kvs = [do(f"kvs{w}", [128, 1024]) for w in WINS]
    T.uT_s = ds("uT_s", [1024, TOK], BF16)
    T.qT_s = ds("qT_s", [1536, TOK], BF16)
    T.kT_s = ds("kT_s", [1536, TOK], BF16)
    T.sga_s = ds("sga_s", [1024, TOK], F32)
    T.sgb_s = ds("sgb_s", [1024, TOK], F32)
    T.vtok_s = ds("vtok_s", [TOK, 1536], BF16)
    T.hsT_s = ds("hsT_s", [1024, TOK], BF16)
    T.x1_s = ds("x1_s", [TOK, D], F32)
    T.uv_s = ds("uv_s", [nexp, 2, D], BF16)
    T.U_s = [ds(f"U_s{g}", [TOK, 512], F32) for g in range(3)]
    T.md_s = [ds(f"md_s{g}", [TOK, 16], F32) for g in range(3)]
    return T


def phase_consts(nc, S, T, C, st):
    sb = lambda n, shape, dt: st.enter_context(nc.sbuf_tensor(n, shape, dt))
    P = Prog(S)
    C.ident_b = sb("ident_b", [128, 128], BF16)
    C.ident_f = sb("ident_f", [128, 128], F32)
    C.maskadd = sb("maskadd", [128, 256], BF16)
    iot = sb("c_iot", [128, 256], I32)
    t1 = sb("c_t1", [128, 256], F32)
    t2 = sb("c_t2", [128, 256], F32)
    P.op("pool", lambda e: e.iota(iot[:], [[1, 256]], base=0, channel_multiplier=-1), writes=["iot"])
    P.op("dve", lambda e: e.tensor_scalar(C.ident_b[:], iot[:, 0:128], 0.0, None, ALU.is_equal),
         reads=["iot"], writes=["ident_b"])
    P.op("dve", lambda e: e.tensor_scalar(C.ident_f[:], iot[:, 0:128], 0.0, None, ALU.is_equal),
         reads=["iot"], writes=["ident_f"])
    P.op("dve", lambda e: e.tensor_scalar(t1[:], iot[:], 0.0, None, ALU.is_ge), reads=["iot"], writes=["t1"])
    P.op("dve", lambda e: e.tensor_scalar(t2[:], iot[:], 128.0, None, ALU.is_le), reads=["iot"], writes=["t2"])
    P.op("dve", lambda e: e.tensor_tensor(t1[:], t1[:], t2[:], ALU.mult), reads=["t1", "t2"], writes=["t1"])
    P.op("dve", lambda e: e.tensor_scalar(C.maskadd[:], t1[:], -1.0, 30000.0, ALU.add, ALU.mult),
         reads=["t1"], writes=["maskadd"])
    P.emit()


def phase1(nc, S, T, C):
    with ExitStack() as st:
        sb = lambda n, shape, dt: st.enter_context(nc.sbuf_tensor(n, shape, dt))
        ps = lambda n, shape, dt: st.enter_context(nc.psum_tensor(n, shape, dt))
        P = Prog(S)
        win_b = sb("win_b", [128, 8, PROJ], BF16)
        wst = [sb(f"wst{i}", [128, 8, 256], F32) for i in range(2)]
        gmix = sb("gmix", [128, 8], F32)
        xs = [sb(f"xs{i}", [128, D], F32) for i in range(2)]
        junk = sb("junk", [128, D], BF16)
        ss = [sb(f"ss{i}", [128, 1], F32) for i in range(2)]
        rstd = [sb(f"rstd{i}", [128, 1], F32) for i in range(2)]
        xnb = [sb(f"xnb{i}", [128, D], BF16) for i in range(2)]
        xnT = [sb(f"xnT{i}", [128, 8, 512], BF16) for i in range(2)]
        kvst = [sb(f"kvst{i}", [128, 1024], F32) for i in range(3)]
        vb = [sb(f"vb{i}", [128, 512], BF16) for i in range(3)]
        fsb = [sb(f"fsb{i}", [128, 512], BF16) for i in range(4)]
        fsf = [sb(f"fsf{i}", [128, 512], F32) for i in range(3)]
        pT = [ps(f"pT{i}", [128, 8, 128], BF16) for i in range(2)]
        pM = [ps(f"pM{i}", [128, 512], F32) for i in range(5)]

        x = T.x.ap()
        P.dma("sp", gmix[:], T.g_mix.ap().rearrange("(k p) -> p k", p=128), writes=["gmix"],
              allow_slow_non_contiguous=True)
        w_in = T.w_in.ap().rearrange("(k p) f -> p k f", p=128)
        for c in range(PROJ // 256):
            b = c % 2
            P.dma("sp", wst[b][:], w_in[:, :, c * 256:(c + 1) * 256], writes=[("wst", b)])
            for k in range(8):
                eng = "dve" if k % 2 == 0 else "pool"
                P.op(eng, lambda e, b=b, k=k, c=c: e.tensor_scalar(
                    win_b[:, k, c * 256:(c + 1) * 256], wst[b][:, k, :], gmix[:, k:k + 1], None, ALU.mult),
                    reads=[("wst", b), "gmix"], writes=[("win", c)])
        wkeys = [("win", c) for c in range(PROJ // 256)]

        cnt = {"pm": 0, "kv": 0, "fsb": 0, "fsf": 0, "ev": 0}

        def next_pm():
            i = cnt["pm"] % len(pM)
            cnt["pm"] += 1
            return i

        nchunks = 9
        for ch in DBG.get("chunks", range(nchunks)):
            ntl = 4 if ch < 8 else 1
            cb = ch % 2
            ntok = ntl * 128
            for j in range(ntl):
                ti = ch * 4 + j
                b = ti % 2
                t0 = ti * 128
                P.dma("sp", xs[b][:], x[t0:t0 + 128, :], writes=[("xs", b)])
                P.op("act", lambda e, b=b: e.activation(junk[:], xs[b][:], AF.Square, accum_out=ss[b][:]),
                     reads=[("xs", b)], writes=["junk", ("ss", b)])
                P.op("dve", lambda e, b=b: e.tensor_scalar(rstd[b][:], ss[b][:], 1.0 / D, EPS, ALU.mult, ALU.add),
                     reads=[("ss", b)], writes=[("rstd", b)])
                P.op("act", lambda e, b=b: e.activation(rstd[b][:], rstd[b][:], AF.Sqrt),
                     reads=[("rstd", b)], writes=[("rstd", b)])
                P.op("dve", lambda e, b=b: e.reciprocal(rstd[b][:], rstd[b][:]),
                     reads=[("rstd", b)], writes=[("rstd", b)])
                P.op("dve", lambda e, b=b: e.tensor_scalar(xnb[b][:], xs[b][:], rstd[b][:], None, ALU.mult),
                     reads=[("xs", b), ("rstd", b)], writes=[("xnb", b)])
                for k in range(8):
                    P.op("pe", lambda e, b=b, k=k: e.transpose(pT[b][:, k, :], xnb[b][:, k * 128:(k + 1) * 128],
                                                               C.ident_b[:]),
                         reads=[("xnb", b), "ident_b"], writes=[("pT", b)])
                P.op("dve", lambda e, b=b, cb=cb, j=j: e.tensor_copy(xnT[cb][:, :, j * 128:(j + 1) * 128], pT[b][:]),
                     writes=[("pT", b), ("xnT", cb, j)])
                if ti < 32:
                    seq = ti // 16
                    tin = (ti % 16) * 128
                else:
                    seq, tin = None, 0
                for g in range(3):
                    need_k = True
                    if seq is not None and tin < SEQ - WINS[g]:
                        need_k = False
                    kb = cnt["kv"] % 3
                    cnt["kv"] += 1
                    for part, off in ((0, OFF_K + 512 * g), (1, OFF_V + 512 * g)):
                        if part == 0 and not need_k:
                            continue
                        pi = next_pm()
                        for k in range(8):
                            P.op("pe", lambda e, pi=pi, k=k, cb=cb, j=j, off=off: e.matmul(
                                pM[pi][:], xnT[cb][:, k, j * 128:(j + 1) * 128], win_b[:, k, off:off + 512],
                                start=(k == 0), stop=(k == 7)),
                                reads=[("xnT", cb, j)] + wkeys[off // 256: off // 256 + 2], writes=[("pM", pi)])
                        P.op("act", lambda e, pi=pi, kb=kb, part=part: e.activation(
                            kvst[kb][:, part * 512:(part + 1) * 512], pM[pi][:], AF.Copy),
                            writes=[("pM", pi), ("kvst", kb, part)])
                        if part == 1:
                            P.op("pool", lambda e, kb=kb: e.tensor_copy(vb[kb][:], kvst[kb][:, 512:1024]),
                                 reads=[("kvst", kb, 1)], writes=[("vb", kb)])
                    P.dma("sp", T.vtok_s.ap()[t0:t0 + 128, g * 512:(g + 1) * 512], vb[kb][:],
                          reads=[("vb", kb)], writes=[("vtok_s", ti, g)])
                    if need_k:
                        if seq is None:
                            dst = T.kvs[g].ap()[:, :]
                        else:
                            r0 = tin - (SEQ - WINS[g])
                            dst = T.kvp[g].ap()[seq, r0:r0 + 128, :]
                        P.dma("sp", dst, kvst[kb][:], reads=[("kvst", kb, 0), ("kvst", kb, 1)],
                              writes=[("kvout", ti, g)])
            tok0 = ch * 512
            xkeys = [("xnT", cb, j) for j in range(ntl)]
            jobs = []
            for f in range(8):
                jobs.append(("u", OFF_U + 128 * f, T.uT_s, f))
            for f in range(12):
                jobs.append(("q", OFF_Q + 128 * f, T.qT_s, f))
            for f in range(12):
                jobs.append(("k", OFF_K + 128 * f, T.kT_s, f))
            for f in range(8):
                jobs.append(("ga", OFF_GA + 128 * f, T.sga_s, f))
            for f in range(8):
                jobs.append(("gb", OFF_GB + 128 * f, T.sgb_s, f))
            for kind, off, dst_t, f in jobs:
                pi = next_pm()
                for k in range(8):
                    P.op("pe", lambda e, pi=pi, k=k, cb=cb, off=off, ntok=ntok: e.matmul(
                        pM[pi][:, 0:ntok], win_b[:, k, off:off + 128], xnT[cb][:, k, 0:ntok],
                        start=(k == 0), stop=(k == 7)),
                        reads=xkeys + [wkeys[off // 256]], writes=[("pM", pi)])
                dst = dst_t.ap()[f * 128:(f + 1) * 128, tok0:tok0 + ntok]
                if kind in ("ga", "gb"):
                    bi = cnt["fsf"] % len(fsf)
                    cnt["fsf"] += 1
                    P.op("act", lambda e, pi=pi, bi=bi, ntok=ntok: e.activation(
                        fsf[bi][:, 0:ntok], pM[pi][:, 0:ntok], AF.Sigmoid),
                        writes=[("pM", pi), ("fsf", bi)])
                    P.dma("sp", dst, fsf[bi][:, 0:ntok], reads=[("fsf", bi)], writes=[(kind, f, ch)])
                else:
                    bi = cnt["fsb"] % len(fsb)
                    cnt["fsb"] += 1
                    eng = "dve" if cnt["ev"] % 2 == 0 else "act"
                    cnt["ev"] += 1
                    qs = 0.125 if kind == "q" else 1.0
                    if eng == "dve":
                        P.op("dve", lambda e, pi=pi, bi=bi, ntok=ntok, qs=qs: e.tensor_scalar(
                            fsb[bi][:, 0:ntok], pM[pi][:, 0:ntok], qs, None, ALU.mult),
                            writes=[("pM", pi), ("fsb", bi)])
                    else:
                        P.op("act", lambda e, pi=pi, bi=bi, ntok=ntok, qs=qs: e.activation(
                            fsb[bi][:, 0:ntok], pM[pi][:, 0:ntok], AF.Copy, scale=qs),
                            writes=[("pM", pi), ("fsb", bi)])
                    P.dma("sp", dst, fsb[bi][:, 0:ntok], reads=[("fsb", bi)], writes=[(kind, f, ch)])
        P.emit()


def build_program(upto=99):
    nc = bass.Bass("TRN2", target_bir_lowering=False)
    T = declare_io(nc)
    C = Ctx()
    with ExitStack() as gst:
        S = Sync(nc, gst)
        phase_consts(nc, S, T, C, gst)
        only = DBG.get("only")
        run = lambda i: (upto >= i) if only is None else (i in only)
        if run(1):
            phase1(nc, S, T, C)
        if run(2) and not DBG.get("skip2"):
            phase2(nc, S, T, C)
        if run(3):
            phase3(nc, S, T, C)
        if run(4):
            phase4(nc, S, T, C)
        if run(5):
            phase5a(nc, S, T, C)
            phase5(nc, S, T, C)
        print("instructions:", S.n_inst, "counts:", S.ccount)
    return nc


def make_in_maps(inputs):
    f = lambda a: np.ascontiguousarray(np.asarray(a, dtype=np.float32))
    xp = f(inputs["x_prompt"])
    xsm = f(inputs["x_sample"])
    shared = {
        "g_mix": f(inputs["g_mix"]).reshape(D),
        "w_in": f(inputs["w_in"]).reshape(D, PROJ),
        "lam_re": f(inputs["lam_re"]).reshape(64, 64),
        "lam_im": f(inputs["lam_im"]).reshape(64, 64),
        "log_dt": f(inputs["log_dt"]).reshape(64),
        "b_re": f(inputs["b_re"]).reshape(4096, 16),
        "b_im": f(inputs["b_im"]).reshape(4096, 16),
        "c_re": f(inputs["c_re"]).reshape(1024, 64),
        "c_im": f(inputs["c_im"]).reshape(1024, 64),
        "d_skip": f(inputs["d_skip"]).reshape(1024),
        "w_glu_a": f(inputs["w_glu_a"]).reshape(D, D),
        "w_glu_b": f(inputs["w_glu_b"]).reshape(D, D),
        "w_attn": f(inputs["w_attn_proj"]).reshape(512, D),
        "w_out": f(inputs["w_out"]).reshape(D, D),
        "g_ffn": f(inputs["g_ffn"]).reshape(D),
        "w_qp": f(inputs["w_qp"]).reshape(D, 2048),
        "sub_keys": f(inputs["sub_keys"]).reshape(16, 128, 128),
        "u_tab": f(inputs["u_tab"]).reshape(NEXP, D),
        "v_tab": f(inputs["v_tab"]).reshape(NEXP, D),
        "g_final": f(inputs["g_final"]).reshape(D),
    }
    st_re = f(inputs["state_ssm_re"]).reshape(128, 4096)
    st_im = f(inputs["state_ssm_im"]).reshape(128, 4096)
    c128 = f(inputs["cache_kv_w128"]).reshape(128, 128, 2, 512)
    c512 = f(inputs["cache_kv_w512"]).reshape(128, 512, 2, 512)
    c2048 = f(inputs["cache_kv_w2048"]).reshape(128, 2048, 2, 512)
    maps = []
    for c in range(NCORES):
        m = dict(shared)
        m["x"] = np.concatenate([xp[NPS * c:NPS * (c + 1)].reshape(TOKP, D),
                                 xsm[NSS * c:NSS * (c + 1)].reshape(128, D)], axis=0)
        sl = slice(NSS * c, NSS * (c + 1))
        m["st_re"] = st_re[sl]
        m["st_im"] = st_im[sl]
        m["c128"] = c128[sl]
        m["c512"] = c512[sl]
        m["c2048"] = c2048[sl]
        maps.append(m)
    return maps


def gather_outputs(results):
    cat = lambda name: np.concatenate([np.asarray(r[name]) for r in results], axis=0)
    y = np.stack([np.asarray(r["y"]) for r in results], axis=0)
    y_prompt = y[:, :TOKP].reshape(16, SEQ, D)
    y_sample = y[:, TOKP:].reshape(128, DEC, D)
    outs = [y_prompt, y_sample,
            cat("ssm_p_re").reshape(1, 16, 64, 64), cat("ssm_p_im").reshape(1, 16, 64, 64)]
    for w in WINS:
        outs.append(cat(f"kvp{w}").reshape(1, 16, w, 2, 8, 64))
    outs.append(cat("ssm_s_re").reshape(1, 128, 64, 64))
    outs.append(cat("ssm_s_im").reshape(1, 128, 64, 64))
    for w in WINS:
        outs.append(cat(f"kvs{w}").reshape(1, 128, DEC, 2, 8, 64))
    return tuple(np.ascontiguousarray(o, dtype=np.float32) for o in outs)


def kernel(**inputs):
    nc = build_program()
    maps = make_in_maps(inputs)
    res = run_bass_kernel_spmd(nc, maps, core_ids=list(range(NCORES)))
    return gather_outputs(res.results)


TWO_PI = 2.0 * math.pi


def phase2(nc, S, T, C):
    with ExitStack() as st:
        sb = lambda n, shape, dt: st.enter_context(nc.sbuf_tensor(n, shape, dt))
        ps = lambda n, shape, dt: st.enter_context(nc.psum_tensor(n, shape, dt))
        P = Prog(S)
        V = "dve"

        def small(name):
            return sb("p2_" + name, [128, 32], F32)

        lr, li, ldt, dtt, rmag, ang = (small(n) for n in ("lr", "li", "ldt", "dt", "rmag", "ang"))
        a1, kq, red, m1 = (small(n) for n in ("a1", "kq", "red", "m1"))
        kqi = sb("p2_kqi", [128, 32], I32)
        sin_t, cos_t, abre, abim, am1 = (small(n) for n in ("sin", "cos", "abre", "abim", "am1"))
        den, fre, fim, tq = (small(n) for n in ("den", "fre", "fim", "tq"))
        Wre = sb("p2_Wre", [128, 11, 32], F32)
        Wim = sb("p2_Wim", [128, 11, 32], F32)
        bre = sb("p2_bre", [128, 32, 16], F32)
        bim = sb("p2_bim", [128, 32, 16], F32)
        bbre = sb("p2_bbre", [128, 32, 16], F32)
        bbim = sb("p2_bbim", [128, 32, 16], F32)
        btmp = sb("p2_btmp", [128, 32, 16], F32)
        bbpad = [[sb(f"p2_bbpad{c}{j}", [128, 128], BF16) for j in range(4)] for c in range(2)]
        Cn = [sb(f"p2_Cn{c}", [128, 8, 64], F32) for c in range(2)]
        CT = [sb(f"p2_CT{c}", [128, 128], BF16) for c in range(2)]
        xl = [sb(f"p2_xl{c}", [128, 128], BF16) for c in range(2)]
        yl = [[sb(f"p2_yl{c}{j}", [128, 128], BF16) for j in range(4)] for c in range(2)]
        bmi = sb("p2_bmi", [128, 8], I32)
        bm = sb("p2_bm", [128, 8], F32)
        bm2 = sb("p2_bm2", [128, 8], F32)
        nbm = sb("p2_nbm", [128, 8], F32)
        dsk = sb("p2_dsk", [128, 8], F32)
        h0st = [sb(f"p2_h0st{i}", [16, 1024], F32) for i in range(2)]
        h0T = [sb(f"p2_h0T{c}", [128, 32, 16], F32) for c in range(2)]
        Dt = [sb(f"p2_D{c}", [128, 2048], F32) for c in range(2)]
        dtmp = [sb(f"p2_dtmp{i}", [128, 1024], F32) for i in range(2)]
        uT = [sb(f"p2_uT{i}", [128, TOK], BF16) for i in range(2)]
        xt = [sb(f"p2_xt{c}", [128, 2048], F32) for c in range(2)]
        gg = [sb(f"p2_g{c}", [128, 2048], F32) for c in range(2)]
        tmp = [sb(f"p2_tmp{i}", [128, 512], F32) for i in range(2)]
        hbuf = sb("p2_hbuf", [128, 4, 2, TOK], BF16)
        hsT = sb("p2_hsT", [128, TOK], BF16)
        ysb = [sb(f"p2_ysb{i}", [128, 512], F32) for i in range(2)]
        yw = [sb(f"p2_yw{i}", [128, 512], F32) for i in range(2)]
        pat = sb("p2_pat", [128, 16, 8], F32)
        decs = sb("p2_decs", [128, 128], F32)
        hfp = [sb(f"p2_hfp{c}", [128, 2, 32], F32) for c in range(2)]
        hfs = [sb(f"p2_hfs{c}", [128, 16, 32], F32) for c in range(2)]
        hfo = sb("p2_hfo", [128, 128], F32)

        xps = [[ps(f"p2_xps{i}{c}", [128, 512], F32) for c in range(2)] for i in range(2)]
        tps = ps("p2_tps", [128, 128], BF16)
        yps = [ps(f"p2_yps{i}", [128, 512], F32) for i in range(2)]
        mps = ps("p2_mps", [128, 32, 16], F32)

        P.dma("sp", lr[:], T.lam_re.ap().rearrange("(k a) n -> (a n) k", a=2), writes=["lr"],
              allow_slow_non_contiguous=True)
        P.dma("sp", li[:], T.lam_im.ap().rearrange("(k a) n -> (a n) k", a=2), writes=["li"],
              allow_slow_non_contiguous=True)
        for a in range(2):
            P.dma("sp", ldt[a * 64:(a + 1) * 64, :], bass.AP(T.log_dt, a, [[0, 64], [2, 32]]), writes=["ldt"],
                  allow_slow_non_contiguous=True)
        P.dma("sp", bre[:], T.b_re.ap().rearrange("(k p) c -> p k c", p=128), writes=["bre"])
        P.dma("sp", bim[:], T.b_im.ap().rearrange("(k p) c -> p k c", p=128), writes=["bim"])
        P.dma("sp", Cn[0][:], T.c_re.ap().rearrange("(f r) n -> r f n", r=128), writes=["Cn0"])
        P.dma("sp", Cn[1][:], T.c_im.ap().rearrange("(f r) n -> r f n", r=128), writes=["Cn1"])
        P.dma("sp", dsk[:], T.d_skip.ap().rearrange("(f r) -> r f", r=128), writes=["dsk"],
              allow_slow_non_contiguous=True)

        def tt(out, a, b, op, reads, writes, eng=V):
            P.op(eng, lambda e: e.tensor_tensor(out, a, b, op), reads=reads, writes=writes)

        def ts(out, a, s1, s2, op0, op1, reads, writes, eng=V):
            if op1 is None:
                P.op(eng, lambda e: e.tensor_scalar(out, a, s1, None, op0), reads=reads, writes=writes)
            else:
                P.op(eng, lambda e: e.tensor_scalar(out, a, s1, s2, op0, op1), reads=reads, writes=writes)

        def stt(out, a, sc, b, op0, op1, reads, writes, eng=V):
            P.op(eng, lambda e: e.scalar_tensor_tensor(out, a, sc, b, op0, op1), reads=reads, writes=writes)

        def act(out, a, func, reads, writes, **kw):
            P.op("act", lambda e: e.activation(out, a, func, **kw), reads=reads, writes=writes)

        act(dtt[:], ldt[:], AF.Exp, ["ldt"], ["dt"])
        tt(a1[:], lr[:], dtt[:], ALU.mult, ["lr", "dt"], ["a1"])
        act(rmag[:], a1[:], AF.Exp, ["a1"], ["rmag"])
        tt(ang[:], li[:], dtt[:], ALU.mult, ["li", "dt"], ["ang"])
        for off, dst, nm in ((0.0, sin_t, "sin"), (math.pi / 2, cos_t, "cos")):
            ts(a1[:], ang[:], off, None, ALU.add, None, ["ang"], ["a1"])
            ts(kq[:], a1[:], 1.0 / TWO_PI, None, ALU.mult, None, ["a1"], ["kq"])
            P.op(V, lambda e: e.tensor_copy(kqi[:], kq[:]), reads=["kq"], writes=["kqi"])
            P.op(V, lambda e: e.tensor_copy(kq[:], kqi[:]), reads=["kqi"], writes=["kq"])
            stt(red[:], kq[:], -TWO_PI, a1[:], ALU.mult, ALU.add, ["kq", "a1"], ["red"])
            ts(m1[:], red[:], math.pi, None, ALU.is_gt, None, ["red"], ["m1"])
            stt(red[:], m1[:], -TWO_PI, red[:], ALU.mult, ALU.add, ["m1", "red"], ["red"])
            ts(m1[:], red[:], -math.pi, None, ALU.is_lt, None, ["red"], ["m1"])
            stt(red[:], m1[:], TWO_PI, red[:], ALU.mult, ALU.add, ["m1", "red"], ["red"])
            ts(red[:], red[:], -math.pi, math.pi, ALU.max, ALU.min, ["red"], ["red"])
            act(dst[:], red[:], AF.Sin, ["red"], [nm])
        tt(abre[:], rmag[:], cos_t[:], ALU.mult, ["rmag", "cos"], ["abre"])
        tt(abim[:], rmag[:], sin_t[:], ALU.mult, ["rmag", "sin"], ["abim"])
        P.op(V, lambda e: e.tensor_copy(Wre[:, 0, :], cos_t[:]), reads=["cos"], writes=["W"])
        P.op(V, lambda e: e.tensor_copy(Wim[:, 0, :], sin_t[:]), reads=["sin"], writes=["W"])
        for L in range(10):
            tt(a1[:], Wre[:, L, :], Wre[:, L, :], ALU.mult, ["W"], ["a1"])
            tt(kq[:], Wim[:, L, :], Wim[:, L, :], ALU.mult, ["W"], ["kq"])
            tt(red[:], Wre[:, L, :], Wim[:, L, :], ALU.mult, ["W"], ["red"])
            tt(Wre[:, L + 1, :], a1[:], kq[:], ALU.subtract, ["a1", "kq"], ["W"])
            ts(Wim[:, L + 1, :], red[:], 2.0, None, ALU.mult, None, ["red"], ["W"])
        tt(den[:], lr[:], lr[:], ALU.mult, ["lr"], ["den"])
        tt(tq[:], li[:], li[:], ALU.mult, ["li"], ["tq"])
        tt(den[:], den[:], tq[:], ALU.add, ["den", "tq"], ["den"])
        P.op(V, lambda e: e.reciprocal(den[:], den[:]), reads=["den"], writes=["den"])
        ts(am1[:], abre[:], -1.0, None, ALU.add, None, ["abre"], ["am1"])
        tt(fre[:], am1[:], lr[:], ALU.mult, ["am1", "lr"], ["fre"])
        tt(tq[:], abim[:], li[:], ALU.mult, ["abim", "li"], ["tq"])
        tt(fre[:], fre[:], tq[:], ALU.add, ["fre", "tq"], ["fre"])
        tt(fre[:], fre[:], den[:], ALU.mult, ["fre", "den"], ["fre"])
        tt(fim[:], abim[:], lr[:], ALU.mult, ["abim", "lr"], ["fim"])
        tt(tq[:], am1[:], li[:], ALU.mult, ["am1", "li"], ["tq"])
        tt(fim[:], fim[:], tq[:], ALU.subtract, ["fim", "tq"], ["fim"])
        tt(fim[:], fim[:], den[:], ALU.mult, ["fim", "den"], ["fim"])
        fre_b = fre[:].unsqueeze(2).to_broadcast([128, 32, 16])
        fim_b = fim[:].unsqueeze(2).to_broadcast([128, 32, 16])
        tt(bbre[:], bre[:], fre_b, ALU.mult, ["bre", "fre"], ["bbre"])
        tt(btmp[:], bim[:], fim_b, ALU.mult, ["bim", "fim"], ["btmp"])
        tt(bbre[:], bbre[:], btmp[:], ALU.subtract, ["bbre", "btmp"], ["bbre"])
        tt(bbim[:], bim[:], fre_b, ALU.mult, ["bim", "fre"], ["bbim"])
        tt(btmp[:], bre[:], fim_b, ALU.mult, ["bre", "fim"], ["btmp"])
        tt(bbim[:], bbim[:], btmp[:], ALU.add, ["bbim", "btmp"], ["bbim"])
        P.op("pool", lambda e: e.iota(bmi[:], [[-16, 8]], base=0, channel_multiplier=1), writes=["bmi"])
        ts(bm[:], bmi[:], 0.0, None, ALU.is_ge, None, ["bmi"], ["bm"])
        ts(bm2[:], bmi[:], 15.0, None, ALU.is_le, None, ["bmi"], ["bm2"])
        tt(bm[:], bm[:], bm2[:], ALU.mult, ["bm", "bm2"], ["bm"])
        ts(nbm[:], bm[:], -1.0, None, ALU.mult, None, ["bm"], ["nbm"])
        for c in range(2):
            for j in range(4):
                P.op("pool", lambda e, c=c, j=j: e.memset(bbpad[c][j][:], 0.0), writes=[("bbpad", c, j)])
        P.op("pool", lambda e: e.memset(pat[:], 1.0), writes=["pat"])
        P.op("pool", lambda e: e.memset(pat[:, :, 0:1], 0.0), writes=["pat"])
        for c, src in ((0, T.st_re), (1, T.st_im)):
            for pc in range(4):
                hb = (c * 4 + pc) % 2
                P.dma("sp", h0st[hb][:], src.ap()[:, pc * 1024:(pc + 1) * 1024], writes=[("h0st", hb)])
                for kk in range(8):
                    k = pc * 8 + kk
                    P.op("pe", lambda e, hb=hb, kk=kk, k=k: e.transpose(
                        mps[:, k, :], h0st[hb][:, kk * 128:(kk + 1) * 128], C.ident_f[0:16, 0:16]),
                        reads=[("h0st", hb), "ident_f"], writes=["mps"])
            P.op("act", lambda e, c=c: e.activation(h0T[c][:], mps[:], AF.Copy), writes=["mps", ("h0T", c)])

        segs = [(0, SEQ), (SEQ, SEQ), (TOKP, 128)]
        cnt = {"x": 0, "y": 0, "ysb": 0}
        for k in range(32):
            F, j = divmod(k, 4)
            r0 = j * 32
            if j == 0:
                ub = F % 2
                P.dma("sp", uT[ub][:], T.uT_s.ap()[F * 128:(F + 1) * 128, :], reads=[("u", F, ch) for ch in range(9)],
                      writes=[("uT", ub)])
            for c, bb in ((0, bbre), (1, bbim)):
                for a in range(2):
                    P.op(V, lambda e, c=c, a=a, bb=bb, k=k, j=j, r0=r0: e.tensor_copy(
                        bbpad[c][j][a * 64:(a + 1) * 64, r0 + 16 * a:r0 + 16 * a + 16], bb[a * 64:(a + 1) * 64, k, :]),
                        reads=["bbre" if c == 0 else "bbim"], writes=[("bbpad", c, j)])
                P.op("pe", lambda e, c=c, j=j: e.transpose(tps[:], bbpad[c][j][:], C.ident_b[:]),
                     reads=[("bbpad", c, j), "ident_b"], writes=["tps"])
                P.op("act", lambda e, c=c: e.activation(xl[c][:], tps[:], AF.Copy), writes=["tps", ("xl", c)])
            for c in range(2):
                msk = bm if c == 0 else nbm
                for a in range(2):
                    P.op(V, lambda e, c=c, a=a, F=F, j=j, msk=msk: e.tensor_scalar(
                        CT[c][:, a * 64:(a + 1) * 64], Cn[c][:, F, :], msk[:, 2 * j + a:2 * j + a + 1], None, ALU.mult),
                        reads=[f"Cn{c}", "bm", "nbm"], writes=[("CT", c)])
                P.op("pe", lambda e, c=c: e.transpose(tps[:], CT[c][:], C.ident_b[:]),
                     reads=[("CT", c), "ident_b"], writes=["tps"])
                P.op("act", lambda e, c=c, j=j: e.activation(yl[c][j][:], tps[:], AF.Copy),
                     writes=["tps", ("yl", c, j)])
            P.op(V, lambda e, k=k: e.tensor_copy(Dt[0][:, 0:1], Wre[:, 0, k:k + 1]), reads=["W"], writes=["D"])
            P.op(V, lambda e, k=k: e.tensor_copy(Dt[1][:, 0:1], Wim[:, 0, k:k + 1]), reads=["W"], writes=["D"])
            for L in range(11):
                n = 1 << L
                wr = Wre[:, L, k:k + 1]
                wi = Wim[:, L, k:k + 1]
                ts(dtmp[0][:, 0:n], Dt[1][:, 0:n], wi, None, ALU.mult, None, ["D", "W"], ["dtmp0"])
                ts(dtmp[1][:, 0:n], Dt[1][:, 0:n], wr, None, ALU.mult, None, ["D", "W"], ["dtmp1"])
                stt(Dt[0][:, n:2 * n], Dt[0][:, 0:n], wr, dtmp[0][:, 0:n], ALU.mult, ALU.subtract,
                    ["D", "W", "dtmp0"], ["D"])
                stt(Dt[1][:, n:2 * n], Dt[0][:, 0:n], wi, dtmp[1][:, 0:n], ALU.mult, ALU.add,
                    ["D", "W", "dtmp1"], ["D"])
            ts(decs[:], pat[:].rearrange("p b t -> p (b t)"), rmag[:, k:k + 1], None, ALU.mult, None,
               ["pat", "rmag"], ["decs"])
            ub = F % 2
            for si, (t0, ln) in enumerate(segs):
                nch = max(1, ln // 512)
                cw = min(ln, 512)
                for ch in range(nch):
                    xi = cnt["x"] % 2
                    cnt["x"] += 1
                    for c in range(2):
                        P.op("pe", lambda e, xi=xi, c=c, ub=ub, t0=t0, ch=ch, cw=cw: e.matmul(
                            xps[xi][c][:, 0:cw], xl[c][:], uT[ub][:, t0 + ch * 512:t0 + ch * 512 + cw],
                            start=True, stop=True),
                            reads=[("xl", c), ("uT", ub)], writes=[("xps", xi, c)])
                    sl = slice(ch * 512, ch * 512 + cw)
                    if si < 2:
                        dre, dim = Dt[0][:, sl], Dt[1][:, sl]
                        xr, xim = xps[xi][0][:, 0:cw], xps[xi][1][:, 0:cw]
                        o_re, o_im = xt[0][:, sl], xt[1][:, sl]
                        t_a, t_b = tmp[0][:, 0:cw], tmp[1][:, 0:cw]
                    else:
                        v3 = lambda ap: ap.rearrange("p (b t) -> p b t", t=8)
                        dre = Dt[0][:, 0:8].unsqueeze(1).to_broadcast([128, 16, 8])
                        dim = Dt[1][:, 0:8].unsqueeze(1).to_broadcast([128, 16, 8])
                        xr, xim = v3(xps[xi][0][:, 0:128]), v3(xps[xi][1][:, 0:128])
                        o_re, o_im = v3(xt[0][:, 0:128]), v3(xt[1][:, 0:128])
                        t_a, t_b = v3(tmp[0][:, 0:128]), v3(tmp[1][:, 0:128])
                    kx = [("xps", xi, 0), ("xps", xi, 1)]
                    P.op(V, lambda e, t_a=t_a, dim=dim, xim=xim: e.tensor_tensor(t_a, dim, xim, ALU.mult),
                         reads=["D"], writes=["tmp0"] + kx)
                    P.op(V, lambda e, o_re=o_re, dre=dre, xr=xr: e.tensor_tensor(o_re, dre, xr, ALU.mult),
                         reads=["D"], writes=["xt0"] + kx)
                    P.op(V, lambda e, o_re=o_re, t_a=t_a: e.tensor_tensor(o_re, o_re, t_a, ALU.add),
                         reads=["tmp0"], writes=["xt0"])
                    P.op(V, lambda e, t_b=t_b, dim=dim, xr=xr: e.tensor_tensor(t_b, dim, xr, ALU.mult),
                         reads=["D"], writes=["tmp1"] + kx)
                    P.op(V, lambda e, o_im=o_im, dre=dre, xim=xim: e.tensor_tensor(o_im, dre, xim, ALU.mult),
                         reads=["D"], writes=["xt1"] + kx)
                    P.op(V, lambda e, o_im=o_im, t_b=t_b: e.tensor_tensor(o_im, o_im, t_b, ALU.subtract),
                         reads=["tmp1"], writes=["xt1"])
                if si == 2:
                    for c in range(2):
                        xv = xt[c][:, 0:128].rearrange("p (b t) -> p b t", t=8)[:, :, 0]
                        P.op(V, lambda e, c=c, k=k, xv=xv: e.scalar_tensor_tensor(
                            xv, h0T[c][:, k, :], rmag[:, k:k + 1], xv, ALU.mult, ALU.add),
                            reads=[("h0T", c), "rmag"], writes=[f"xt{c}"])
                    dec = decs[:]
                else:
                    dec = rmag[:, k:k + 1].to_broadcast([128, ln])
                for c in range(2):
                    P.op(V, lambda e, c=c, dec=dec, ln=ln: e.tensor_tensor_scan(
                        gg[c][:, 0:ln], dec, xt[c][:, 0:ln], 0.0, ALU.mult, ALU.add),
                        reads=[f"xt{c}", "rmag", "decs"], writes=[f"g{c}"])
                for ch in range(nch):
                    sl = slice(ch * 512, ch * 512 + cw)
                    if si < 2:
                        dre, dim = Dt[0][:, sl], Dt[1][:, sl]
                        g_re, g_im = gg[0][:, sl], gg[1][:, sl]
                        o_re, o_im = xt[0][:, sl], xt[1][:, sl]
                        t_a, t_b = tmp[0][:, 0:cw], tmp[1][:, 0:cw]
                    else:
                        v3 = lambda ap: ap.rearrange("p (b t) -> p b t", t=8)
                        dre = Dt[0][:, 0:8].unsqueeze(1).to_broadcast([128, 16, 8])
                        dim = Dt[1][:, 0:8].unsqueeze(1).to_broadcast([128, 16, 8])
                        g_re, g_im = v3(gg[0][:, 0:128]), v3(gg[1][:, 0:128])
                        o_re, o_im = v3(xt[0][:, 0:128]), v3(xt[1][:, 0:128])
                        t_a, t_b = v3(tmp[0][:, 0:128]), v3(tmp[1][:, 0:128])
                    P.op(V, lambda e, t_a=t_a, dim=dim, g_im=g_im: e.tensor_tensor(t_a, dim, g_im, ALU.mult),
                         reads=["D", "g1"], writes=["tmp0"])
                    P.op(V, lambda e, o_re=o_re, dre=dre, g_re=g_re: e.tensor_tensor(o_re, dre, g_re, ALU.mult),
                         reads=["D", "g0"], writes=["xt0"])
                    P.op(V, lambda e, o_re=o_re, t_a=t_a: e.tensor_tensor(o_re, o_re, t_a, ALU.subtract),
                         reads=["tmp0"], writes=["xt0"])
                    P.op(V, lambda e, t_b=t_b, dim=dim, g_re=g_re: e.tensor_tensor(t_b, dim, g_re, ALU.mult),
                         reads=["D", "g0"], writes=["tmp1"])
                    P.op(V, lambda e, o_im=o_im, dre=dre, g_im=g_im: e.tensor_tensor(o_im, dre, g_im, ALU.mult),
                         reads=["D", "g1"], writes=["xt1"])
                    P.op(V, lambda e, o_im=o_im, t_b=t_b: e.tensor_tensor(o_im, o_im, t_b, ALU.add),
                         reads=["tmp1"], writes=["xt1"])
                for c in range(2):
                    P.op("act", lambda e, c=c, j=j, t0=t0, ln=ln: e.activation(
                        hbuf[:, j, c, t0:t0 + ln], xt[c][:, 0:ln], AF.Copy),
                        reads=[f"xt{c}"], writes=[("hbuf", j, c, si)])
                    if si < 2:
                        P.op("act", lambda e, c=c, k=k, si=si: e.activation(
                            hfp[c][:, si, k:k + 1], xt[c][:, SEQ - 1:SEQ], AF.Copy),
                            reads=[f"xt{c}"], writes=[("hfp", c)])
                    else:
                        P.op("act", lambda e, c=c, k=k: e.activation(
                            hfs[c][:, :, k], xt[c][:, 0:128].rearrange("p (b t) -> p b t", t=8)[:, :, 7], AF.Copy),
                            reads=[f"xt{c}"], writes=[("hfs", c)])
            if j == 3:
                for ch in range(9):
                    t0 = ch * 512
                    cw = 512 if ch < 8 else 128
                    si = 0 if ch < 4 else (1 if ch < 8 else 2)
                    yi = cnt["y"] % 2
                    cnt["y"] += 1
                    n = 0
                    for jj in range(4):
                        for c in range(2):
                            P.op("pe", lambda e, yi=yi, jj=jj, c=c, t0=t0, cw=cw, n=n: e.matmul(
                                yps[yi][:, 0:cw], yl[c][jj][:], hbuf[:, jj, c, t0:t0 + cw],
                                start=(n == 0), stop=(n == 7)),
                                reads=[("yl", c, jj), ("hbuf", jj, c, si)], writes=[("yps", yi)])
                            n += 1
                    bi = cnt["ysb"] % 2
                    cnt["ysb"] += 1
                    P.op(V, lambda e, yi=yi, bi=bi, ub=ub, F=F, t0=t0, cw=cw: e.scalar_tensor_tensor(
                        ysb[bi][:, 0:cw], uT[ub][:, t0:t0 + cw], dsk[:, F:F + 1], yps[yi][:, 0:cw],
                        ALU.mult, ALU.add),
                        reads=[("uT", ub), "dsk"], writes=[("yps", yi), ("ysb", bi)])
                    P.op("pool", lambda e, bi=bi, cw=cw: e.tensor_tensor(yw[bi][:, 0:cw], ysb[bi][:, 0:cw], ysb[bi][:, 0:cw], ALU.mult),
                         reads=[("ysb", bi)], writes=[("yw", bi)])
                    P.op("pool", lambda e, bi=bi, cw=cw: e.tensor_scalar(yw[bi][:, 0:cw], yw[bi][:, 0:cw], 0.044715, 1.0, ALU.mult, ALU.add),
                         reads=[("yw", bi)], writes=[("yw", bi)])
                    P.op("pool", lambda e, bi=bi, cw=cw: e.tensor_tensor(yw[bi][:, 0:cw], yw[bi][:, 0:cw], ysb[bi][:, 0:cw], ALU.mult),
                         reads=[("yw", bi), ("ysb", bi)], writes=[("yw", bi)])
                    P.op("act", lambda e, bi=bi, cw=cw: e.activation(yw[bi][:, 0:cw], yw[bi][:, 0:cw], AF.Sigmoid, scale=1.5957691216057308),
                         reads=[("yw", bi)], writes=[("yw", bi)])
                    P.op("pool", lambda e, bi=bi, cw=cw, t0=t0: e.tensor_tensor(hsT[:, t0:t0 + cw], yw[bi][:, 0:cw], ysb[bi][:, 0:cw], ALU.mult),
                         reads=[("yw", bi), ("ysb", bi)], writes=["hsT"])
                P.dma("sp", T.hsT_s.ap()[F * 128:(F + 1) * 128, :], hsT[:], reads=["hsT"], writes=[("hsT_s", F)])
        for c, dstp, dsts in ((0, T.ssm_p_re, T.ssm_s_re), (1, T.ssm_p_im, T.ssm_s_im)):
            P.op("pe", lambda e, c=c: e.transpose(yps[0][0:64, 0:128], hfp[c][:].rearrange("p s k -> p (s k)"), C.ident_f[:]),
                 reads=[("hfp", c), "ident_f"], writes=[("yps", 0)])
            P.op("act", lambda e: e.activation(hfo[0:64, :], yps[0][0:64, 0:128], AF.Copy), writes=[("yps", 0), "hfo"])
            for s_ in range(2):
                P.dma("sp", dstp.ap()[s_].rearrange("(k p) -> k p", p=128), hfo[s_ * 32:(s_ + 1) * 32, :],
                      reads=["hfo"], writes=[("ssm_p", c, s_)])
            for q in range(4):
                P.op("pe", lambda e, c=c, q=q: e.transpose(
                    yps[1][:, 0:128], hfs[c][:, q * 4:(q + 1) * 4, :].rearrange("p b k -> p (b k)"), C.ident_f[:]),
                    reads=[("hfs", c), "ident_f"], writes=[("yps", 1)])
                P.op("act", lambda e: e.activation(hfo[:, :], yps[1][:, 0:128], AF.Copy), writes=[("yps", 1), "hfo"])
                for bl in range(4):
                    P.dma("sp", dsts.ap()[q * 4 + bl].rearrange("(k p) -> k p", p=128), hfo[bl * 32:(bl + 1) * 32, :],
                          reads=["hfo"], writes=[("ssm_s", c, q, bl)])
        P.emit()


def phase3(nc, S, T, C):
    with ExitStack() as st:
        sb = lambda n, shape, dt: st.enter_context(nc.sbuf_tensor(n, shape, dt))
        ps = lambda n, shape, dt: st.enter_context(nc.psum_tensor(n, shape, dt))
        P = Prog(S)
        qTg = [sb(f"p3_qT{i}", [128, 4, SEQ], BF16) for i in range(2)]
        kTg = [sb(f"p3_kT{i}", [128, 4, SEQ], BF16) for i in range(2)]
        Vg = [sb(f"p3_V{i}", [128, 16, 512], BF16) for i in range(2)]
        qTs = sb("p3_qTs", [128, 12, 128], BF16)
        kTs = sb("p3_kTs", [128, 12, 128], BF16)
        kc32 = [sb(f"p3_kc32{i}", [128, 512], F32) for i in range(2)]
        vc32 = [sb(f"p3_vc32{i}", [128, 512], F32) for i in range(2)]
        kcb = [sb(f"p3_kcb{i}", [128, 512], BF16) for i in range(2)]
        vcb = [sb(f"p3_vcb{i}", [128, 512], BF16) for i in range(2)]
        kcT = [sb(f"p3_kcT{i}", [128, 4, 128], BF16) for i in range(2)]
        vnew = [sb(f"p3_vnew{i}", [8, 512], BF16) for i in range(2)]
        Pb = [sb(f"p3_Pb{i}", [128, 256], BF16) for i in range(4)]
        PTs = [sb(f"p3_PTs{i}", [128, 2, 128], BF16) for i in range(4)]
        stat = [sb(f"p3_stat{i}", [128, 16], F32) for i in range(2)]
        Osb = [sb(f"p3_Osb{i}", [128, 512], F32) for i in range(2)]
        sps = [ps(f"p3_sps{i}", [128, 2, 256], F32) for i in range(2)]
        ptp = [ps(f"p3_ptp{i}", [128, 4, 2, 128], BF16) for i in range(2)]
        ops_ = [ps(f"p3_ops{i}", [128, 8, 64], F32) for i in range(2)]
        kps = ps("p3_kps", [128, 4, 128], BF16)

        cnt = {"unit": 0, "head": 0}
        scale = 0.125

        def unit(nq, blocks, q_of, out_rows_U, out_rows_md, extra_reads):
            ui = cnt["unit"] % 2
            cnt["unit"] += 1
            c_lo = blocks[0][0]
            c_hi = blocks[-1][0] + blocks[-1][1]
            def res(h):
                hi = hbase + h
                sp = sps[hi % 2]
                sslot = (hi // 2) % 2
                skey = ("sps", hi % 2)
                pb = hi % 4
                tp = ptp[hi % 2]
                tslot = (hi // 2) % 4
                tkey = ("ptp", hi % 2)
                return sp, sslot, skey, pb, tp, tslot, tkey

            def stage_s(h):
                sp, sslot, skey, pb, tp, tslot, tkey = res(h)
                first = True
                for (col0, n, kT_of, v_of, rk) in blocks:
                    P.op("pe", lambda e, sp=sp, sslot=sslot, col0=col0, n=n, kT_of=kT_of, h=h, first=first: e.matmul(
                        sp[0:nq, sslot, col0:col0 + n], q_of(h), kT_of(h), start=first, stop=False,
                        skip_group_check=True),
                        reads=list(extra_reads) + list(rk), writes=[skey])
                    first = False
                P.op("pe", lambda e, sp=sp, sslot=sslot: e.matmul(
                    sp[0:nq, sslot, c_lo:c_hi], C.ident_b[0:nq, 0:nq], C.maskadd[0:nq, c_lo:c_hi],
                    start=False, stop=True, skip_group_check=True),
                    reads=["ident_b", "maskadd"], writes=[skey])
                P.op("dve", lambda e, sp=sp, sslot=sslot, h=h: e.tensor_reduce(
                    stat[ui][0:nq, h:h + 1], sp[0:nq, sslot, c_lo:c_hi], AX.X, ALU.max, negate=True),
                    writes=[skey, ("stat", ui, h)])
                P.op("act", lambda e, sp=sp, sslot=sslot, h=h, pb=pb: e.activation(
                    Pb[pb][0:nq, c_lo:c_hi], sp[0:nq, sslot, c_lo:c_hi], AF.Exp,
                    bias=stat[ui][0:nq, h:h + 1], accum_out=stat[ui][0:nq, 8 + h:9 + h]),
                    writes=[skey, ("stat", ui, h), ("Pb", pb)])

            def stage_t(h):
                sp, sslot, skey, pb, tp, tslot, tkey = res(h)
                for bi, (col0, n, kT_of, v_of, rk) in enumerate(blocks):
                    P.op("pe", lambda e, tp=tp, tslot=tslot, bi=bi, col0=col0, n=n, pb=pb: e.transpose(
                        tp[0:n, tslot, bi, 0:nq], Pb[pb][0:nq, col0:col0 + n], C.ident_b[0:nq, 0:nq]),
                        reads=[("Pb", pb), "ident_b"], writes=[tkey])
                for bi, (col0, n, kT_of, v_of, rk) in enumerate(blocks):
                    if h % 2 == 0:
                        P.op("dve", lambda e, tp=tp, tslot=tslot, bi=bi, n=n, pb=pb: e.tensor_copy(
                            PTs[pb][0:n, bi, 0:nq], tp[0:n, tslot, bi, 0:nq]),
                            writes=[tkey, ("PTs", pb, bi)])
                    else:
                        P.op("act", lambda e, tp=tp, tslot=tslot, bi=bi, n=n, pb=pb: e.activation(
                            PTs[pb][0:n, bi, 0:nq], tp[0:n, tslot, bi, 0:nq], AF.Copy),
                            writes=[tkey, ("PTs", pb, bi)])

            def stage_v(h):
                sp, sslot, skey, pb, tp, tslot, tkey = res(h)
                for bi, (col0, n, kT_of, v_of, rk) in enumerate(blocks):
                    P.op("pe", lambda e, h=h, bi=bi, n=n, pb=pb, v_of=v_of: e.matmul(
                        ops_[ui][0:nq, h, :], PTs[pb][0:n, bi, 0:nq], v_of(h),
                        start=(bi == 0), stop=(bi == len(blocks) - 1), skip_group_check=True),
                        reads=[("PTs", pb, bi)] + list(rk), writes=[("ops", ui)])

            hbase = cnt["head"]
            cnt["head"] += 8

            def fin():
                P.op("act", lambda e: e.activation(
                    Osb[ui][0:nq, :], ops_[ui][0:nq, :, :].rearrange("p h e -> p (h e)"), AF.Copy),
                    writes=[("ops", ui), ("Osb", ui)])
                P.dma("sp", out_rows_U, Osb[ui][0:nq, :], reads=[("Osb", ui)], writes=[("U_s", uid)])
                P.dma("sp", out_rows_md, stat[ui][0:nq, :], reads=[("stat", ui, h) for h in range(8)], writes=[("md_s", uid)])

            uid = cnt["unit"]
            for h in range(8):
                stage_s(h)
                tick = hbase + h
                pending.append((tick + 2, 0, lambda h=h: stage_t(h)))
                pending.append((tick + 4, 1, lambda h=h: stage_v(h)))
                if h == 7:
                    pending.append((tick + 4, 2, fin))
                flush(tick)

        pending = []

        def flush(tick):
            pending.sort(key=lambda t: (t[0], t[1]))
            while pending and pending[0][0] <= tick:
                _, _, fn = pending.pop(0)
                fn()

        gi = 0
        for seq in DBG.get("p3_seqs", range(NPS)):
            for g in range(3):
                d = DILS[g]
                bsel = gi % 2
                gi += 1
                c0 = seq * SEQ
                P.dma("sp", qTg[bsel][:], T.qT_s.ap()[g * 512:(g + 1) * 512, c0:c0 + SEQ].rearrange("(hp p) t -> p hp t", p=128),
                      reads=[("q", 4 * g + f, ch) for f in range(4) for ch in range(8)], writes=[("qTg", bsel)])
                P.dma("sp", kTg[bsel][:], T.kT_s.ap()[g * 512:(g + 1) * 512, c0:c0 + SEQ].rearrange("(hp p) t -> p hp t", p=128),
                      reads=[("k", 4 * g + f, ch) for f in range(4) for ch in range(8)], writes=[("kTg", bsel)])
                nb = SEQ // d // 128
                for r in range(d):
                    for blk in range(nb):
                        ux = r * nb + blk
                        row0 = c0 + r + d * blk * 128
                        P.dma("sp", Vg[bsel][:, ux, :], T.vtok_s.ap()[ss(row0, 128, d), g * 512:(g + 1) * 512],
                              reads=[("vtok_s", ti, g) for ti in range(33)], writes=[("Vg", bsel, ux)])
                for r in range(d):
                    for blk in [b_ for b_ in DBG.get("p3_blks", range(nb)) if b_ < nb]:
                        ux = r * nb + blk
                        tq0 = r + d * blk * 128

                        def q_of(h, bsel=bsel, tq0=tq0, d=d):
                            return qTg[bsel][64 * (h % 2):64 * (h % 2) + 64, h // 2, ss(tq0, 128, d)]

                        blocks = []
                        if blk > 0:
                            tk0 = r + d * (blk - 1) * 128
                            blocks.append((0, 128,
                                           lambda h, bsel=bsel, tk0=tk0, d=d: kTg[bsel][64 * (h % 2):64 * (h % 2) + 64, h // 2, ss(tk0, 128, d)],
                                           lambda h, bsel=bsel, ux=ux: Vg[bsel][:, ux - 1, h * 64:(h + 1) * 64],
                                           [("kTg", bsel), ("Vg", bsel, ux - 1)]))
                        blocks.append((128, 128,
                                       lambda h, bsel=bsel, tq0=tq0, d=d: kTg[bsel][64 * (h % 2):64 * (h % 2) + 64, h // 2, ss(tq0, 128, d)],
                                       lambda h, bsel=bsel, ux=ux: Vg[bsel][:, ux, h * 64:(h + 1) * 64],
                                       [("kTg", bsel), ("Vg", bsel, ux)]))
                        rows = ss(c0 + tq0, 128, d)
                        unit(128, blocks, q_of, T.U_s[g].ap()[rows, :], T.md_s[g].ap()[rows, :], [("qTg", bsel)])
        P.dma("sp", qTs[:], T.qT_s.ap()[:, TOKP:TOK].rearrange("(f p) t -> p f t", p=128),
              reads=[("q", f, 8) for f in range(12)], writes=["qTs"])
        P.dma("sp", kTs[:], T.kT_s.ap()[:, TOKP:TOK].rearrange("(f p) t -> p f t", p=128),
              reads=[("k", f, 8) for f in range(12)], writes=["kTs"])
        caches = (T.c128, T.c512, T.c2048)
        ci = 0
        for b in DBG.get("p3_bs", range(NSS)):
            for g in range(3):
                d = DILS[g]
                nq = max(1, DEC // d)
                for r in range(min(d, DEC)):
                    cb = ci % 2
                    ci += 1
                    csrc = caches[g].ap()[b, ss(r, 128, d), :, :]
                    P.dma("sp", kc32[cb][:], csrc[:, 0, :], writes=[("kc32", cb)])
                    P.dma("sp", vc32[cb][:], csrc[:, 1, :], writes=[("vc32", cb)])
                    tok0 = TOKP + b * DEC + r
                    P.dma("sp", vnew[cb][0:nq, :], T.vtok_s.ap()[ss(tok0, nq, d), g * 512:(g + 1) * 512],
                          reads=[("vtok_s", 32, g)], writes=[("vnew", cb)])
                    P.op("pool", lambda e, cb=cb: e.tensor_copy(kcb[cb][:], kc32[cb][:]), reads=[("kc32", cb)], writes=[("kcb", cb)])
                    P.op("pool", lambda e, cb=cb: e.tensor_copy(vcb[cb][:], vc32[cb][:]), reads=[("vc32", cb)], writes=[("vcb", cb)])
                    for hp in range(4):
                        P.op("pe", lambda e, cb=cb, hp=hp: e.transpose(kps[:, hp, :], kcb[cb][:, hp * 128:(hp + 1) * 128], C.ident_b[:]),
                             reads=[("kcb", cb), "ident_b"], writes=["kps"])
                    P.op("dve", lambda e, cb=cb: e.tensor_copy(kcT[cb][:], kps[:]), writes=["kps", ("kcT", cb)])
                    col = b * DEC + r

                    def q_of(h, g=g, col=col, nq=nq, d=d):
                        return qTs[64 * (h % 2):64 * (h % 2) + 64, 4 * g + h // 2, ss(col, nq, d)]

                    blocks = [
                        (0, 128,
                         lambda h, cb=cb: kcT[cb][64 * (h % 2):64 * (h % 2) + 64, h // 2, :],
                         lambda h, cb=cb: vcb[cb][:, h * 64:(h + 1) * 64],
                         [("kcT", cb), ("vcb", cb)]),
                        (128, nq,
                         lambda h, g=g, col=col, nq=nq, d=d: kTs[64 * (h % 2):64 * (h % 2) + 64, 4 * g + h // 2, ss(col, nq, d)],
                         lambda h, cb=cb, nq=nq: vnew[cb][0:nq, h * 64:(h + 1) * 64],
                         ["kTs", ("vnew", cb)]),
                    ]
                    rows = ss(tok0, nq, d)
                    unit(nq, blocks, q_of, T.U_s[g].ap()[rows, :], T.md_s[g].ap()[rows, :], ["qTs"])
        flush(10 ** 9)
        P.emit()


def load_weight_bf16(P, nc, dst, src_ap, kch, ncols, wst, tag, scale_ap=None, engs=("dve", "pool")):
    src = src_ap.rearrange("(k p) f -> p k f", p=128)
    cw = 2048 // kch
    n = 0
    for c in range(0, ncols, cw):
        b = n % 2
        P.dma("sp", wst[b][:, 0:kch * cw].rearrange("p (k f) -> p k f", k=kch), src[:, :, c:c + cw],
              writes=[("wst", b)])
        for k in range(kch):
            eng = engs[n % len(engs)]
            n2 = n
            if scale_ap is None:
                P.op(eng, lambda e, b=b, k=k, c=c: e.tensor_copy(
                    dst[:, k, c:c + cw], wst[b][:, k * cw:(k + 1) * cw]),
                    reads=[("wst", b)], writes=[(tag, c)])
            else:
                P.op(eng, lambda e, b=b, k=k, c=c: e.tensor_scalar(
                    dst[:, k, c:c + cw], wst[b][:, k * cw:(k + 1) * cw], scale_ap[:, k:k + 1], None, ALU.mult),
                    reads=[("wst", b), tag + "_scale"], writes=[(tag, c)])
        n += 1
    return [(tag, c) for c in range(0, ncols, cw)]


def phase4(nc, S, T, C):
    with ExitStack() as st:
        sb = lambda n, shape, dt: st.enter_context(nc.sbuf_tensor(n, shape, dt))
        ps = lambda n, shape, dt: st.enter_context(nc.psum_tensor(n, shape, dt))
        P = Prog(S)
        Wa = sb("p4_Wa", [128, 8, D], BF16)
        Wb = sb("p4_Wb", [128, 8, D], BF16)
        Wp = sb("p4_Wp", [128, 4, D], BF16)
        Wo = sb("p4_Wo", [128, 8, D], BF16)
        wst = [sb(f"p4_wst{i}", [128, 2048], F32) for i in range(2)]
        U = [sb(f"p4_U{i}", [128, 3, 512], F32) for i in range(2)]
        md = [sb(f"p4_md{i}", [128, 3, 16], F32) for i in range(2)]
        nM = sb("p4_nM", [128, 8], F32)
        tdf = sb("p4_tdf", [128, 3, 8], F32)
        aw = sb("p4_aw", [128, 3, 8], F32)
        ad = sb("p4_ad", [128, 3, 8], F32)
        Z = sb("p4_Z", [128, 8], F32)
        acc = sb("p4_acc", [128, 512], F32)
        acc2 = sb("p4_acc2", [128, 512], F32)
        attn_b = sb("p4_attn_b", [128, 512], BF16)
        attnT = [sb(f"p4_attnT{i}", [128, 4, 128], BF16) for i in range(2)]
        hsT = [sb(f"p4_hsT{i}", [128, 8, 128], BF16) for i in range(2)]
        sga = [sb(f"p4_sga{i}", [128, 8, 128], F32) for i in range(2)]
        sgb = [sb(f"p4_sgb{i}", [128, 8, 128], F32) for i in range(2)]
        sig = [sb(f"p4_sig{i}", [128, 128], F32) for i in range(2)]
        t1 = [sb(f"p4_t1{i}", [128, 128], F32) for i in range(2)]
        t2 = [sb(f"p4_t2{i}", [128, 128], F32) for i in range(2)]
        mixT = [sb(f"p4_mixT{i}", [128, 8, 128], BF16) for i in range(2)]
        xs = [sb(f"p4_xs{i}", [128, D], F32) for i in range(2)]
        x1 = [sb(f"p4_x1{i}", [128, D], F32) for i in range(2)]
        ptp = ps("p4_ptp", [128, 4, 128], BF16)
        pabc = [ps(f"p4_pabc{i}", [128, 3, 128], F32) for i in range(2)]
        pout = [ps(f"p4_pout{i}", [128, 512], F32) for i in range(4)]

        ka = load_weight_bf16(P, nc, Wa, T.w_glu_a.ap(), 8, D, wst, "Wa")
        kb = load_weight_bf16(P, nc, Wb, T.w_glu_b.ap(), 8, D, wst, "Wb")
        kp = load_weight_bf16(P, nc, Wp, T.w_attn.ap(), 4, D, wst, "Wp")
        ko = load_weight_bf16(P, nc, Wo, T.w_out.ap(), 8, D, wst, "Wo")

        V = "dve"
        cnt = {"f": 0, "po": 0}
        for ti in DBG.get("p4_tiles", range(NTILE)):
            b = ti % 2
            t0 = ti * 128
            for g in range(3):
                P.dma("sp", U[b][:, g, :], T.U_s[g].ap()[t0:t0 + 128, :], writes=[("U", b)])
                P.dma("sp", md[b][:, g, :], T.md_s[g].ap()[t0:t0 + 128, :], writes=[("md", b)])
            P.dma("sp", hsT[b][:], T.hsT_s.ap()[:, t0:t0 + 128].rearrange("(k p) t -> p k t", p=128), writes=[("hsT", b)])
            P.dma("sp", sga[b][:], T.sga_s.ap()[:, t0:t0 + 128].rearrange("(k p) t -> p k t", p=128), writes=[("sga", b)])
            P.dma("sp", sgb[b][:], T.sgb_s.ap()[:, t0:t0 + 128].rearrange("(k p) t -> p k t", p=128), writes=[("sgb", b)])
            P.dma("sp", xs[b][:], T.x.ap()[t0:t0 + 128, :], writes=[("xs", b)])
            P.op(V, lambda e, b=b: e.tensor_tensor(nM[:], md[b][:, 0, 0:8], md[b][:, 1, 0:8], ALU.min), reads=[("md", b)], writes=["nM"])
            P.op(V, lambda e, b=b: e.tensor_tensor(nM[:], nM[:], md[b][:, 2, 0:8], ALU.min), reads=[("md", b), "nM"], writes=["nM"])
            P.op(V, lambda e, b=b: e.tensor_tensor(tdf[:], md[b][:, :, 0:8], nM[:].unsqueeze(1).to_broadcast([128, 3, 8]), ALU.subtract),
                 reads=[("md", b), "nM"], writes=["tdf"])
            P.op("act", lambda e: e.activation(aw[:], tdf[:], AF.Exp, scale=-1.0), reads=["tdf"], writes=["aw"])
            P.op(V, lambda e, b=b: e.tensor_tensor(ad[:], aw[:], md[b][:, :, 8:16], ALU.mult), reads=["aw", ("md", b)], writes=["ad"])
            P.op(V, lambda e: e.tensor_tensor(Z[:], ad[:, 0, :], ad[:, 1, :], ALU.add), reads=["ad"], writes=["Z"])
            P.op(V, lambda e: e.tensor_tensor(Z[:], Z[:], ad[:, 2, :], ALU.add), reads=["ad", "Z"], writes=["Z"])
            P.op(V, lambda e: e.reciprocal(Z[:], Z[:]), reads=["Z"], writes=["Z"])
            P.op(V, lambda e: e.tensor_tensor(aw[:], aw[:], Z[:].unsqueeze(1).to_broadcast([128, 3, 8]), ALU.mult),
                 reads=["aw", "Z"], writes=["aw"])
            wbs = [aw[:, g, :].unsqueeze(2).to_broadcast([128, 8, 64]) for g in range(3)]
            u3s = [U[b][:, g, :].rearrange("p (h e) -> p h e", e=64) for g in range(3)]
            acc3 = acc[:].rearrange("p (h e) -> p h e", e=64)
            acc23 = acc2[:].rearrange("p (h e) -> p h e", e=64)
            P.op(V, lambda e, u=u3s[0], w=wbs[0]: e.tensor_tensor(acc3, u, w, ALU.mult), reads=[("U", b), "aw"], writes=["acc"])
            P.op(V, lambda e, u=u3s[1], w=wbs[1]: e.tensor_tensor(acc23, u, w, ALU.mult), reads=[("U", b), "aw"], writes=["acc2"])
            P.op(V, lambda e: e.tensor_tensor(acc[:], acc[:], acc2[:], ALU.add), reads=["acc", "acc2"], writes=["acc"])
            P.op(V, lambda e, u=u3s[2], w=wbs[2]: e.tensor_tensor(acc23, u, w, ALU.mult), reads=[("U", b), "aw"], writes=["acc2"])
            P.op(V, lambda e: e.tensor_tensor(attn_b[:], acc[:], acc2[:], ALU.add), reads=["acc", "acc2"], writes=["attn_b"])
            for c in range(4):
                P.op("pe", lambda e, c=c: e.transpose(ptp[:, c, :], attn_b[:, c * 128:(c + 1) * 128], C.ident_b[:]),
                     reads=["attn_b", "ident_b"], writes=["ptp"])
            P.op("act", lambda e, b=b: e.activation(attnT[b][:], ptp[:], AF.Copy), writes=["ptp", ("attnT", b)])
            for F in range(8):
                fi = cnt["f"] % 2
                cnt["f"] += 1
                pk = ("pabc", fi)
                for k in range(8):
                    P.op("pe", lambda e, fi=fi, k=k, F=F, b=b: e.matmul(
                        pabc[fi][:, 0, :], Wa[:, k, F * 128:(F + 1) * 128], hsT[b][:, k, :], start=(k == 0), stop=(k == 7),
                        skip_group_check=True), reads=ka + [("hsT", b)], writes=[pk])
                for k in range(8):
                    P.op("pe", lambda e, fi=fi, k=k, F=F, b=b: e.matmul(
                        pabc[fi][:, 1, :], Wb[:, k, F * 128:(F + 1) * 128], hsT[b][:, k, :], start=False, stop=(k == 7),
                        skip_group_check=True), reads=kb + [("hsT", b)], writes=[pk])
                for k in range(4):
                    P.op("pe", lambda e, fi=fi, k=k, F=F, b=b: e.matmul(
                        pabc[fi][:, 2, :], Wp[:, k, F * 128:(F + 1) * 128], attnT[b][:, k, :], start=False, stop=(k == 3),
                        skip_group_check=True), reads=kp + [("attnT", b)], writes=[pk])
                P.op("act", lambda e, fi=fi: e.activation(sig[fi][:], pabc[fi][:, 1, :], AF.Sigmoid), writes=[pk, ("sig", fi)])
                P.op(V, lambda e, fi=fi: e.tensor_tensor(t1[fi][:], pabc[fi][:, 0, :], sig[fi][:], ALU.mult),
                     reads=[("sig", fi)], writes=[pk, ("t1", fi)])
                P.op(V, lambda e, fi=fi, F=F, b=b: e.tensor_tensor(t2[fi][:], pabc[fi][:, 2, :], sgb[b][:, F, :], ALU.mult),
                     reads=[("sgb", b)], writes=[pk, ("t2", fi)])
                P.op("pool", lambda e, fi=fi, F=F, b=b: e.tensor_tensor(t1[fi][:], t1[fi][:], sga[b][:, F, :], ALU.mult),
                     reads=[("sga", b)], writes=[("t1", fi)])
                P.op("pool", lambda e, fi=fi, F=F, b=b: e.tensor_tensor(mixT[b][:, F, :], t1[fi][:], t2[fi][:], ALU.add),
                     reads=[("t1", fi), ("t2", fi)], writes=[("mixT", b)])
            for half in range(2):
                po = cnt["po"] % 4
                cnt["po"] += 1
                for k in range(8):
                    P.op("pe", lambda e, po=po, k=k, half=half, b=b: e.matmul(
                        pout[po][:], mixT[b][:, k, :], Wo[:, k, half * 512:(half + 1) * 512], start=(k == 0), stop=(k == 7)),
                        reads=ko + [("mixT", b)], writes=[("pout", po)])
                P.op(V, lambda e, po=po, half=half, b=b: e.tensor_tensor(
                    x1[b][:, half * 512:(half + 1) * 512], pout[po][:], xs[b][:, half * 512:(half + 1) * 512], ALU.add),
                    reads=[("xs", b)], writes=[("pout", po), ("x1", b)])
            P.dma("sp", T.x1_s.ap()[t0:t0 + 128, :], x1[b][:], reads=[("x1", b)], writes=[("x1_s", ti)])
        P.emit()


def phase5a(nc, S, T, C):
    with ExitStack() as st:
        sb = lambda n, shape, dt: st.enter_context(nc.sbuf_tensor(n, shape, dt))
        P = Prog(S)
        R = 4
        NBUF = 4
        cin = [sb(f"p5a_in{i}", [128, R, D], F32) for i in range(NBUF)]
        cout = [sb(f"p5a_out{i}", [128, R, D], BF16) for i in range(NBUF)]
        nexp = T.u_tab.shape[0]
        nblk = nexp // (128 * R)
        engs = ("act", "dve", "pool")
        n = 0
        for blk in range(nblk):
            r0 = blk * 128 * R
            for c, tab in ((0, T.u_tab), (1, T.v_tab)):
                i = n % NBUF
                P.dma("sp", cin[i][:], tab.ap()[r0:r0 + 128 * R, :].rearrange("(p r) d -> p r d", r=R), writes=[("cin", i)])
                eng = engs[n % 3]
                if eng == "act":
                    P.op("act", lambda e, i=i: e.activation(cout[i][:], cin[i][:], AF.Copy), reads=[("cin", i)], writes=[("cout", i)])
                else:
                    P.op(eng, lambda e, i=i: e.tensor_copy(cout[i][:], cin[i][:]), reads=[("cin", i)], writes=[("cout", i)])
                P.dma("sp", T.uv_s.ap()[r0:r0 + 128 * R, c, :].rearrange("(p r) d -> p r d", r=R), cout[i][:],
                      reads=[("cout", i)], writes=[("uv_s", blk, c)])
                n += 1
        P.emit()


def phase5(nc, S, T, C):
    NEG = -1.0e30
    with ExitStack() as st:
        sb = lambda n, shape, dt: st.enter_context(nc.sbuf_tensor(n, shape, dt))
        ps = lambda n, shape, dt: st.enter_context(nc.psum_tensor(n, shape, dt))
        P = Prog(S)
        V = "dve"
        Wq = sb("p5_Wq", [128, 8, 2048], BF16)
        wst = [sb(f"p5_wst{i}", [128, 2048], F32) for i in range(2)]
        gk = sb("p5_gk", [128, 8], F32)
        skT = sb("p5_skT", [128, 16, 128], BF16)
        skb = [sb(f"p5_skb{i}", [128, 128], BF16) for i in range(2)]
        gffn = sb("p5_gffn", [128, D], F32)
        gfin = sb("p5_gfin", [128, D], F32)
        io16 = sb("p5_io16", [128, 256], I32)
        io256 = sb("p5_io256", [128, 256], F32)
        x1 = [sb(f"p5_x1{i}", [128, D], F32) for i in range(2)]
        xn2s = [sb(f"p5_xn2s{i}", [128, D], BF16) for i in range(2)]
        eidx_is = [sb(f"p5_eidx_is{i}", [128, 128], I32) for i in range(2)]
        gates = [sb(f"p5_gates{i}", [128, 128], F32) for i in range(2)]
        NYIELD = 2
        ss = sb("p5_ss", [128, 4], F32)
        xn2b = sb("p5_xn2b", [128, D], BF16)
        xn2T = sb("p5_xn2T", [128, 8, 128], BF16)
        qpT = sb("p5_qpT", [128, 16, 128], BF16)
        sc = sb("p5_sc", [128, 16, 128], F32)
        scw = sb("p5_scw", [128, 16, 128], F32)
        sv = sb("p5_sv", [128, 16, 16], F32)
        si = sb("p5_si", [128, 16, 16], U32)
        sif = sb("p5_sif", [128, 16, 16], F32)
        cand = sb("p5_cand", [128, 8, 256], F32)
        candw = sb("p5_candw", [128, 8, 256], F32)
        cidx = sb("p5_cidx", [128, 8, 256], F32)
        best = sb("p5_best", [128, 8, 16], F32)
        pos = sb("p5_pos", [128, 8, 16], U32)
        posf = sb("p5_posf", [128, 8, 16], F32)
        eqb = sb("p5_eqb", [128, 8, 256], F32)
        eidx = sb("p5_eidx", [128, 128], F32)
        ex = sb("p5_ex", [128, 8, 16], F32)
        gs = sb("p5_gs", [128, 8], F32)
        dots = sb("p5_dots", [128, 128], F32)
        gw = [sb(f"p5_gw{i}", [128, 8], F32) for i in range(2)]
        wgt = sb("p5_wgt", [128, 128], F32)
        NB = 16
        gb = [sb(f"p5_gb{i}", [128, 2 * D], BF16) for i in range(8)]
        gb_extra = {}
        for i_ in range(2):
            wv = wst[i_][:].bitcast(BF16)
            gb.append(wv[:, 0:2 * D])
            gb.append(wv[:, 2 * D:4 * D])
            gb_extra[8 + 2 * i_] = ("wst", i_)
            gb_extra[9 + 2 * i_] = ("wst", i_)
        for i_ in range(4):
            gb.append(sb(f"p5_gbx{i_}", [128, 2 * D], BF16)[:])
        gbv = lambda i: (gb[i] if i >= 8 else gb[i][:])
        gbk = lambda i: [("gb", i)] + ([gb_extra[i]] if i in gb_extra else [])
        prod = [sb(f"p5_prod{i}", [128, D], BF16) for i in range(4)]
        junk = sb("p5_junk", [128, D], BF16)
        diag = [sb(f"p5_diag{i}", [128, 128], BF16) for i in range(4)]
        yb = sb("p5_yb", [128, D], F32)
        pacc = [ps(f"p5_pacc{i}", [128, 512], F32) for i in range(2)]
        uv2d = T.uv_s.ap().rearrange("n c d -> n (c d)")
        ptp = ps("p5_ptp", [128, 8, 128], BF16)
        pq = [ps(f"p5_pq{i}", [128, 4, 128], F32) for i in range(2)]
        psc = [ps(f"p5_psc{i}", [128, 4, 128], F32) for i in range(2)]

        bc_reg = nc.gpsimd.alloc_register("p5_bc")
        nc.gpsimd.reg_mov(bc_reg, T.u_tab.shape[0] - 1)
        P.dma("sp", gk[:], T.g_ffn.ap().rearrange("(k p) -> p k", p=128), writes=["Wq_scale"], allow_slow_non_contiguous=True)
        kq = load_weight_bf16(P, nc, Wq, T.w_qp.ap(), 8, 2048, wst, "Wq", scale_ap=gk)
        P.dma("sp", gffn[:], T.g_ffn.ap().partition_broadcast(128), writes=["gffn"])
        P.dma("sp", gfin[:], T.g_final.ap().partition_broadcast(128), writes=["gfin"])
        P.op("pool", lambda e: e.iota(io16[:], [[1, 256]], base=0, channel_multiplier=0), writes=["io16"])
        P.op(V, lambda e: e.tensor_copy(io256[:], io16[:]), reads=["io16"], writes=["io256"])
        for hp in range(16):
            b = hp % 2
            P.dma("sp", wst[b][:, 0:128], T.sub_keys.ap()[hp], writes=[("wst", b)])
            P.op(V, lambda e, b=b: e.tensor_copy(skb[b][:], wst[b][:, 0:128]), reads=[("wst", b)], writes=[("skb", b)])
            P.op("pe", lambda e, b=b: e.transpose(ptp[:, 0, :], skb[b][:], C.ident_b[:]), reads=[("skb", b), "ident_b"], writes=["ptp"])
            P.op("act", lambda e, hp=hp: e.activation(skT[:, hp, :], ptp[:, 0, :], AF.Copy), writes=["ptp", "skT"])

        def rms(src, col, reads):
            P.op("act", lambda e: e.activation(junk[:], src, AF.Square, accum_out=ss[:, col:col + 1]),
                 reads=reads, writes=["junk", ("ss", col)])
            P.op(V, lambda e: e.tensor_scalar(ss[:, col:col + 1], ss[:, col:col + 1], 1.0 / D, EPS, ALU.mult, ALU.add),
                 reads=[("ss", col)], writes=[("ss", col)])
            P.op("act", lambda e: e.activation(ss[:, col:col + 1], ss[:, col:col + 1], AF.Sqrt), reads=[("ss", col)], writes=[("ss", col)])
            P.op(V, lambda e: e.reciprocal(ss[:, col:col + 1], ss[:, col:col + 1]), reads=[("ss", col)], writes=[("ss", col)])

        def route(ti):
            b = ti % 2
            t0 = ti * 128
            P.dma("sp", x1[b][:], T.x1_s.ap()[t0:t0 + 128, :], reads=[("x1_s", ti)], writes=[("x1", b)])
            rms(x1[b][:], 0, [("x1", b)])
            P.op(V, lambda e, b=b: e.scalar_tensor_tensor(xn2s[b][:], x1[b][:], ss[:, 0:1], gffn[:], ALU.mult, ALU.mult),
                 reads=[("x1", b), ("ss", 0), "gffn"], writes=[("xn2", b)])
            P.op("pool", lambda e, b=b: e.tensor_scalar(xn2b[:], x1[b][:], ss[:, 0:1], None, ALU.mult),
                 reads=[("x1", b), ("ss", 0)], writes=["xn2b"])
            for k in range(8):
                P.op("pe", lambda e, k=k: e.transpose(ptp[:, k, :], xn2b[:, k * 128:(k + 1) * 128], C.ident_b[:]),
                     reads=["xn2b", "ident_b"], writes=["ptp"])
            P.op("act", lambda e: e.activation(xn2T[:], ptp[:], AF.Copy), writes=["ptp", "xn2T"])
            yield
            for j in range(4):
                pj = j % 2
                for hh in range(4):
                    hp = 4 * j + hh
                    for k in range(8):
                        P.op("pe", lambda e, pj=pj, hh=hh, hp=hp, k=k: e.matmul(
                            pq[pj][:, hh, :], Wq[:, k, hp * 128:(hp + 1) * 128], xn2T[:, k, :],
                            start=(k == 0 and hh == 0), stop=(k == 7), skip_group_check=True),
                            reads=kq + ["xn2T"], writes=[("pq", pj)])
                eng = "act" if j % 2 == 0 else V
                if eng == "act":
                    P.op("act", lambda e, pj=pj, j=j: e.activation(qpT[:, 4 * j:4 * j + 4, :], pq[pj][:], AF.Copy),
                         writes=[("pq", pj), ("qpT", j)])
                else:
                    P.op(V, lambda e, pj=pj, j=j: e.tensor_copy(qpT[:, 4 * j:4 * j + 4, :], pq[pj][:]),
                         writes=[("pq", pj), ("qpT", j)])
                yield
            for j in range(4):
                pj = j % 2
                for hh in range(4):
                    hp = 4 * j + hh
                    P.op("pe", lambda e, pj=pj, hh=hh, hp=hp: e.matmul(
                        psc[pj][:, hh, :], qpT[:, hp, :], skT[:, hp, :], start=(hh == 0), stop=True, skip_group_check=True),
                        reads=[("qpT", j), "skT"], writes=[("psc", pj)])
                P.op("act", lambda e, pj=pj, j=j: e.activation(sc[:, 4 * j:4 * j + 4, :], psc[pj][:], AF.Copy),
                     writes=[("psc", pj), ("sc", j)])
                yield
            for step in range(5):
                for hp in range(16):
                    j = hp // 4
                    if step == 0:
                        P.op(V, lambda e, hp=hp: e.max(sv[:, hp, 0:8], sc[:, hp, :]), reads=[("sc", j)], writes=[("sv", hp)])
                    elif step == 1:
                        P.op(V, lambda e, hp=hp: e.max_index(si[:, hp, 0:8], sv[:, hp, 0:8], sc[:, hp, :]),
                             reads=[("sc", j), ("sv", hp)], writes=[("si", hp)])
                    elif step == 2:
                        P.op(V, lambda e, hp=hp: e.match_replace(scw[:, hp, :], sv[:, hp, 0:8], sc[:, hp, :], NEG),
                             reads=[("sc", j), ("sv", hp)], writes=[("scw", hp)])
                    elif step == 3:
                        P.op(V, lambda e, hp=hp: e.max(sv[:, hp, 8:16], scw[:, hp, :]), reads=[("scw", hp)], writes=[("sv", hp)])
                    else:
                        P.op(V, lambda e, hp=hp: e.max_index(si[:, hp, 8:16], sv[:, hp, 8:16], scw[:, hp, :]),
                             reads=[("scw", hp), ("sv", hp)], writes=[("si", hp)])
                    if hp % 4 == 3:
                        yield
            svk = [("sv", hp) for hp in range(16)]
            sik = [("si", hp) for hp in range(16)]
            P.op(V, lambda e: e.tensor_copy(sif[:], si[:]), reads=sik, writes=["sif"])
            sv4 = sv[:].rearrange("p (h two) k -> p h two k", two=2)
            sif4 = sif[:].rearrange("p (h two) k -> p h two k", two=2)
            c4 = lambda t: t[:].rearrange("p h (a b) -> p h a b", b=16)
            P.op(V, lambda e: e.tensor_tensor(c4(cand), sv4[:, :, 0, :].unsqueeze(3).to_broadcast([128, 8, 16, 16]),
                                              sv4[:, :, 1, :].unsqueeze(2).to_broadcast([128, 8, 16, 16]), ALU.add),
                 reads=svk, writes=["cand"])
            P.op(V, lambda e: e.tensor_scalar(c4(cidx), sif4[:, :, 0, :].unsqueeze(3).to_broadcast([128, 8, 16, 16]), 128.0, None, ALU.mult),
                 reads=["sif"], writes=["cidx"])
            P.op(V, lambda e: e.tensor_tensor(c4(cidx), c4(cidx), sif4[:, :, 1, :].unsqueeze(2).to_broadcast([128, 8, 16, 16]), ALU.add),
                 reads=["sif", "cidx"], writes=["cidx"])
            for step in range(5):
                for h in range(8):
                    if step == 0:
                        P.op(V, lambda e, h=h: e.max(best[:, h, 0:8], cand[:, h, :]), reads=["cand"], writes=[("best", h)])
                    elif step == 1:
                        P.op(V, lambda e, h=h: e.max_index(pos[:, h, 0:8], best[:, h, 0:8], cand[:, h, :]),
                             reads=["cand", ("best", h)], writes=[("pos", h)])
                    elif step == 2:
                        P.op(V, lambda e, h=h: e.match_replace(candw[:, h, :], best[:, h, 0:8], cand[:, h, :], NEG),
                             reads=["cand", ("best", h)], writes=[("candw", h)])
                    elif step == 3:
                        P.op(V, lambda e, h=h: e.max(best[:, h, 8:16], candw[:, h, :]), reads=[("candw", h)], writes=[("best", h)])
                    else:
                        P.op(V, lambda e, h=h: e.max_index(pos[:, h, 8:16], best[:, h, 8:16], candw[:, h, :]),
                             reads=[("candw", h), ("best", h)], writes=[("pos", h)])
                    if h % 4 == 3:
                        yield
            bk = [("best", h) for h in range(8)]
            pk = [("pos", h) for h in range(8)]
            P.op(V, lambda e: e.tensor_copy(posf[:], pos[:]), reads=pk, writes=["posf"])
            for h in range(8):
                for hk in range(2):
                    bkey = ["eqb"]
                    e3 = eqb[:]
                    ks = slice(hk * 8, hk * 8 + 8)
                    P.op(V, lambda e, h=h, e3=e3, ks=ks: e.tensor_tensor(
                        e3, io256[:].unsqueeze(1).to_broadcast([128, 8, 256]),
                        posf[:, h, ks].unsqueeze(2).to_broadcast([128, 8, 256]), ALU.is_equal),
                        reads=["io256", "posf"], writes=bkey)
                    P.op(V, lambda e, h=h, e3=e3: e.tensor_tensor(
                        e3, e3, cidx[:, h, :].unsqueeze(1).to_broadcast([128, 8, 256]), ALU.mult),
                        reads=["cidx"], writes=bkey)
                    P.op(V, lambda e, h=h, e3=e3, hk=hk: e.tensor_reduce(eidx[:, h * 16 + hk * 8:h * 16 + hk * 8 + 8], e3, AX.X, ALU.add),
                         reads=bkey, writes=[("eidx", h)])
                    yield
            ek = [("eidx", h) for h in range(8)]
            P.op(V, lambda e, b=b: e.tensor_copy(eidx_is[b][:], eidx[:]), reads=ek, writes=[("eidx_i", b)])
            P.op(V, lambda e: e.tensor_tensor(ex[:], best[:], best[:, :, 0:1].to_broadcast([128, 8, 16]), ALU.subtract),
                 reads=bk, writes=["ex"])
            P.op("act", lambda e: e.activation(ex[:], ex[:], AF.Exp), reads=["ex"], writes=["ex"])
            P.op(V, lambda e: e.tensor_reduce(gs[:], ex[:], AX.X, ALU.add), reads=["ex"], writes=["gs"])
            P.op(V, lambda e: e.reciprocal(gs[:], gs[:]), reads=["gs"], writes=["gs"])
            P.op(V, lambda e, b=b: e.tensor_tensor(gates[b][:].rearrange("p (h k) -> p h k", k=16), ex[:],
                                                   gs[:].unsqueeze(2).to_broadcast([128, 8, 16]), ALU.mult),
                 reads=["ex", "gs"], writes=[("gate", b)])
            yield

        def gather(ti, nxt):
            b = ti % 2
            t0 = ti * 128
            GS = 4
            for k0 in range(0, 128, GS):
                for k in range(k0, k0 + GS):
                    gbi = k % NB
                    pi = k % 4
                    P.op("pool", lambda e, k=k, gbi=gbi, b=b: e.indirect_dma_start(
                        out=gbv(gbi)[:, :], out_offset=None, in_=uv2d,
                        in_offset=bass.IndirectOffsetOnAxis(ap=eidx_is[b][:, k:k + 1], axis=0),
                        bounds_check=bc_reg, oob_is_err=False),
                        reads=[("eidx_i", b)], writes=gbk(gbi), dma=True)
                    P.op(V, lambda e, gbi=gbi, pi=pi, b=b: e.tensor_tensor(prod[pi][:], gbv(gbi)[:, 0:D], xn2s[b][:], ALU.mult),
                         reads=[("gb", gbi), ("xn2", b)], writes=[("prod", pi)])
                    P.op("act", lambda e, k=k, pi=pi: e.activation(junk[:], prod[pi][:], AF.Copy, accum_out=dots[:, k:k + 1]),
                         reads=[("prod", pi)], writes=["junk", ("dots", k0)])
                dk = [("dots", k0)]
                sl = slice(k0, k0 + GS)
                gk_ = ("gw", (k0 // GS) % 2)
                gwb = gw[(k0 // GS) % 2]
                P.op("act", lambda e, sl=sl, gwb=gwb: e.activation(gwb[:, 0:GS], dots[:, sl], AF.Gelu_apprx_tanh), reads=dk, writes=[gk_])
                P.op(V, lambda e, sl=sl, gwb=gwb, b=b: e.tensor_tensor(wgt[:, sl], gwb[:, 0:GS], gates[b][:, sl], ALU.mult), reads=[gk_, ("gate", b)], writes=[("wgt", k0)])
                for k in range(k0, k0 + GS):
                    gbi = k % NB
                    di = k % 4
                    P.op(V, lambda e, k=k, di=di: e.tensor_scalar(diag[di][:], C.ident_b[:], wgt[:, k:k + 1], None, ALU.mult),
                         reads=["ident_b", ("wgt", k0)], writes=[("diag", di)])
                    for hf in range(2):
                        P.op("pe", lambda e, k=k, di=di, gbi=gbi, hf=hf: e.matmul(
                            pacc[hf][:], diag[di][:], gbv(gbi)[:, D + hf * 512:D + (hf + 1) * 512],
                            start=(k == 0), stop=(k == 127)),
                            reads=[("diag", di), ("gb", gbi)], writes=[("pacc", hf)])
                if nxt is not None:
                    for _ in range(NYIELD):
                        next(nxt, None)
            for hf in range(2):
                P.op(V, lambda e, hf=hf, b=b: e.tensor_tensor(
                    yb[:, hf * 512:(hf + 1) * 512], pacc[hf][:], x1[b][:, hf * 512:(hf + 1) * 512], ALU.add),
                    reads=[("x1", b)], writes=[("pacc", hf), "yb"])
            rms(yb[:], 1, ["yb"])
            P.op(V, lambda e: e.scalar_tensor_tensor(yb[:], yb[:], ss[:, 1:2], gfin[:], ALU.mult, ALU.mult),
                 reads=["yb", ("ss", 1), "gfin"], writes=["yb"])
            P.dma("sp", T.y.ap()[t0:t0 + 128, :], yb[:], reads=["yb"], writes=[("y", ti)])

        tiles = list(DBG.get("p5_tiles", range(NTILE)))
        gens = {ti: route(ti) for ti in tiles}
        for _ in gens[tiles[0]]:
            pass
        for n_, ti in enumerate(tiles):
            nxt = gens[tiles[n_ + 1]] if n_ + 1 < len(tiles) else None
            gather(ti, nxt)
            if nxt is not None:
                for _ in nxt:
                    pass
        P.emit()
```
